# Optimizing a Trainium2 kernel written in Bass

```python
import jax, jax.numpy as jnp
from jax import lax
import numpy as np

D_MODEL = 1024
BATCH = 16
SEQ = 2048
DEPTH = 1

CTX_LEN = 256
GRID_W = 64
D_MIX = D_MODEL
LRU_W = D_MIX // 2
LRU_BLOCKS = 8
LRU_BD = LRU_W // LRU_BLOCKS
CONV_W = 4
CONV_PAD_L = 1
LRU_C = 8.0
HG_W = D_MIX - LRU_W
HG_HEADS = 4
HG_HD = HG_W // HG_HEADS
HG_CHUNK = 32
N_EXPERTS = 256
TOP_K = 8
N_GROUPS = 8
TOPK_GROUPS = 4
D_EXPERT = 256
D_SHARED = 256
ROUTE_SCALE = 2.5
MOE_BLOCK = 128
EPS = 1e-6
IN_COLS = 2 * LRU_W + 5 * HG_W
IN_SPLITS = [LRU_W, 2 * LRU_W, 2 * LRU_W + HG_W, 2 * LRU_W + 2 * HG_W, 2 * LRU_W + 3 * HG_W, 2 * LRU_W + 4 * HG_W]

kernel_name = 'hybrid_rglru_hgrn2_moe_dit_block'


def _rmsnorm(x, gain):
    xf = x.astype(jnp.float32)
    y = xf * lax.rsqrt(jnp.mean(xf * xf, axis=-1, keepdims=True) + EPS)
    return (y * gain.astype(jnp.float32)).astype(x.dtype)


def _to_colmajor(t):
    b, n, ch = t.shape
    rows = n // GRID_W
    return t.reshape(b, rows, GRID_W, ch).transpose(0, 2, 1, 3).reshape(b, n, ch)


def _from_colmajor(t):
    b, n, ch = t.shape
    rows = n // GRID_W
    return t.reshape(b, GRID_W, rows, ch).transpose(0, 2, 1, 3).reshape(b, n, ch)


def _dwconv_centred(t, w, bias):
    n = t.shape[1]
    tp = jnp.pad(t, ((0, 0), (CONV_PAD_L, CONV_W - 1 - CONV_PAD_L), (0, 0)))
    out = bias
    for k in range(CONV_W):
        out = out + tp[:, k:k + n] * w[k]
    return out


def _rglru_coeffs(u, wa, ba, wx, bx, lam):
    blocks = u.reshape(u.shape[:-1] + (LRU_BLOCKS, LRU_BD))
    r = jax.nn.sigmoid(jnp.einsum('blhi,hij->blhj', blocks, wa).reshape(u.shape) + ba)
    i = jax.nn.sigmoid(jnp.einsum('blhi,hij->blhj', blocks, wx).reshape(u.shape) + bx)
    log_a = -LRU_C * r * jax.nn.softplus(-lam)
    a = jnp.exp(log_a)
    b = jnp.sqrt(-jnp.expm1(2.0 * log_a)) * (i * u)
    return a, b


def _linear_scan(a, b, h0, reverse):
    if h0 is not None:
        first = b.shape[1] - 1 if reverse else 0
        b = b.at[:, first].add(a[:, first] * h0)

    def combine(e1, e2):
        a1, b1 = e1
        a2, b2 = e2
        return a1 * a2, a2 * b1 + b2

    _, h = lax.associative_scan(combine, (a, b), reverse=reverse, axis=1)
    return h


def _hgrn2_chunkwise(q, k, v, logf, s0):
    b, n, h, d = q.shape
    nc = n // HG_CHUNK

    def split(t):
        return t.reshape(b, nc, HG_CHUNK, h, d).transpose(1, 0, 3, 2, 4)

    q, k, v, logf = split(q), split(k), split(v), split(logf)
    g = jnp.cumsum(logf, axis=3)
    g_last = g[:, :, :, -1:, :]
    q_dec = q * jnp.exp(g)
    tri = jnp.tril(jnp.ones((HG_CHUNK, HG_CHUNK), dtype=bool))
    scores = jnp.einsum('nbhid,nbhjd->nbhij', q_dec, k * jnp.exp(-g))
    scores = jnp.where(tri, scores, 0.0)
    o_intra = jnp.einsum('nbhij,nbhje->nbhie', scores, v)
    kv = jnp.einsum('nbhjd,nbhje->nbhde', k * jnp.exp(g_last - g), v)
    decay = jnp.exp(g_last[:, :, :, 0, :])

    def step(s, inp):
        dec, kv_c = inp
        return dec[..., None] * s + kv_c, s

    s_final, s_prev = lax.scan(step, s0, (decay, kv))
    o = o_intra + jnp.einsum('nbhid,nbhde->nbhie', q_dec, s_prev)
    o = o.transpose(1, 0, 3, 2, 4).reshape(b, n, h, d)
    return o, s_final


def _hgrn2_direction(q, v, zf, lb, s0, reverse):
    b, n, _ = q.shape
    f32 = jnp.float32
    zf = zf.astype(f32)
    logf = jnp.log(lb + (1.0 - lb) * jax.nn.sigmoid(zf))
    k = (1.0 - lb) * jax.nn.sigmoid(-zf)

    def heads(t):
        return t.astype(f32).reshape(b, n, HG_HEADS, HG_HD)

    q, k, v, logf = heads(q), heads(k), heads(v), heads(logf)
    if reverse:
        q, k, v, logf = (jnp.flip(t, axis=1) for t in (q, k, v, logf))
    if s0 is None:
        s0 = jnp.zeros((b, HG_HEADS, HG_HD, HG_HD), f32)
    o, s = _hgrn2_chunkwise(q, k, v, logf, s0)
    if reverse:
        o = jnp.flip(o, axis=1)
    return o.reshape(b, n, HG_W), s


def _head_rmsnorm(o, gain):
    b, n, _ = o.shape
    oh = o.reshape(b, n, HG_HEADS, HG_HD)
    oh = oh * lax.rsqrt(jnp.mean(oh * oh, axis=-1, keepdims=True) + EPS)
    return oh.reshape(b, n, HG_W) * gain.astype(jnp.float32)


def _hybrid_mixer(hx, hc, w_in, w_out, conv_w, conv_b, wa, ba, wx, bx, lam, lb, head_gain, need_ctx):
    f32 = jnp.float32
    rx, rgx, qx, vx, fx_f, fx_b, gx = jnp.split(hx @ w_in, IN_SPLITS, axis=-1)
    rc, rgc, qc, vc, fc_f, fc_b, gc = jnp.split(hc @ w_in, IN_SPLITS, axis=-1)

    ul = _dwconv_centred(rx, conv_w, conv_b).astype(f32)
    uc = _dwconv_centred(rc, conv_w, conv_b).astype(f32)
    lru_l = jnp.zeros_like(ul)
    lru_c = jnp.zeros_like(uc)
    for d, rev in enumerate((False, True)):
        a, bb = _rglru_coeffs(uc, wa[d], ba[d], wx[d], bx[d], lam[d])
        h_c = _linear_scan(a, bb, None, rev)
        h0 = h_c[:, 0] if rev else h_c[:, -1]
        a, bb = _rglru_coeffs(ul, wa[d], ba[d], wx[d], bx[d], lam[d])
        lru_l = lru_l + _linear_scan(a, bb, h0, rev)
        lru_c = lru_c + h_c

    ql, vl, zl_f, zl_b = (_to_colmajor(t) for t in (qx, vx, fx_f, fx_b))
    hg_l = 0.0
    hg_c = 0.0
    for d, (zl, zc, rev) in enumerate(((zl_f, fc_f, False), (zl_b, fc_b, True))):
        o_c, s_c = _hgrn2_direction(qc, vc, zc, lb[d], None, rev)
        o_l, _ = _hgrn2_direction(ql, vl, zl, lb[d], s_c, rev)
        hg_l = hg_l + o_l
        hg_c = hg_c + o_c
    hg_l = _from_colmajor(hg_l)

    y_l = jnp.concatenate([jax.nn.gelu(rgx.astype(f32)) * lru_l,
                           _head_rmsnorm(hg_l, head_gain) * jax.nn.silu(gx.astype(f32))], axis=-1)
    y_l = y_l.astype(hx.dtype) @ w_out
    if not need_ctx:
        return y_l, None
    y_c = jnp.concatenate([jax.nn.gelu(rgc.astype(f32)) * lru_c,
                           _head_rmsnorm(hg_c, head_gain) * jax.nn.silu(gc.astype(f32))], axis=-1)
    y_c = y_c.astype(hc.dtype) @ w_out
    return y_l, y_c


def _moe(h, router_w, router_b, w13, w2, sw13, sw2):
    n, d = h.shape
    f32 = jnp.float32
    scores = jax.nn.sigmoid((h @ router_w).astype(f32))
    biased = scores + router_b.astype(f32)
    grouped = biased.reshape(n, N_GROUPS, N_EXPERTS // N_GROUPS)
    group_score = jnp.sum(lax.top_k(grouped, 2)[0], axis=-1)
    _, gidx = lax.top_k(group_score, TOPK_GROUPS)
    gmask = jnp.sum(jax.nn.one_hot(gidx, N_GROUPS, dtype=f32), axis=1) > 0
    emask = jnp.repeat(gmask, N_EXPERTS // N_GROUPS, axis=-1)
    _, eidx = lax.top_k(jnp.where(emask, biased, -jnp.inf), TOP_K)
    gate = jnp.take_along_axis(scores, eidx, axis=-1)
    gate = gate / jnp.sum(gate, axis=-1, keepdims=True) * ROUTE_SCALE

    nk = n * TOP_K
    e_flat = eidx.reshape(nk)
    order = jnp.argsort(e_flat)
    se = e_flat[order]
    st = (order // TOP_K).astype(jnp.int32)
    sw = gate.reshape(nk)[order]
    counts = jnp.bincount(e_flat, length=N_EXPERTS)
    starts = jnp.cumsum(counts) - counts
    padded = (counts + MOE_BLOCK - 1) // MOE_BLOCK * MOE_BLOCK
    pends = jnp.cumsum(padded)
    pstarts = pends - padded
    pos = pstarts[se] + jnp.arange(nk) - starts[se]
    n_blocks = -(-nk // MOE_BLOCK) + N_EXPERTS
    cap = n_blocks * MOE_BLOCK
    tok_buf = jnp.full((cap,), n, jnp.int32).at[pos].set(st)
    w_buf = jnp.zeros((cap,), f32).at[pos].set(sw)
    blk_e = jnp.minimum(jnp.searchsorted(pends, jnp.arange(n_blocks) * MOE_BLOCK, side='right'), N_EXPERTS - 1)
    h_pad = jnp.concatenate([h, jnp.zeros((1, d), h.dtype)], axis=0)

    def body(acc, inp):
        tb, wb, e = inp
        xb = h_pad[tb]
        u = xb @ w13[e]
        y = (jax.nn.silu(u[:, :D_EXPERT]) * u[:, D_EXPERT:]) @ w2[e]
        return acc.at[tb].add(y * wb[:, None].astype(y.dtype)), None

    acc, _ = lax.scan(body, jnp.zeros((n + 1, d), h.dtype),
                      (tok_buf.reshape(n_blocks, MOE_BLOCK), w_buf.reshape(n_blocks, MOE_BLOCK), blk_e))
    u = h @ sw13
    shared = (jax.nn.silu(u[:, :D_SHARED]) * u[:, D_SHARED:]) @ sw2
    return acc[:n] + shared


def setup_inputs(seed: int = 0) -> dict:
    key = jax.random.key(seed)
    ks = jax.random.split(key, 32)
    f32 = jnp.float32
    D = D_MODEL

    def nrm(k, shape, s):
        return jax.random.normal(k, shape, f32) * s

    lam_u = jax.random.uniform(ks[16], (DEPTH, 2, LRU_W), f32, 0.9, 0.999)
    sig = lam_u ** (1.0 / LRU_C)
    return {
        'x': nrm(ks[0], (BATCH, SEQ, D), 1.0),
        'c': nrm(ks[1], (BATCH, D), 1.0),
        'ctx': nrm(ks[2], (BATCH, CTX_LEN, D), 1.0),
        'c_ctx': nrm(ks[3], (D,), 1.0),
        'ada_w': nrm(ks[4], (DEPTH, D, 6 * D), 0.5 * D ** -0.5),
        'ada_b': nrm(ks[5], (DEPTH, 6 * D), 0.02),
        'norm_mix': 1.0 + nrm(ks[6], (DEPTH, D), 0.02),
        'norm_ffn': 1.0 + nrm(ks[7], (DEPTH, D), 0.02),
        'norm_final': 1.0 + nrm(ks[8], (D,), 0.02),
        'w_in': nrm(ks[9], (DEPTH, D, IN_COLS), D ** -0.5),
        'w_out': nrm(ks[10], (DEPTH, D_MIX, D), D_MIX ** -0.5),
        'lru_conv_w': nrm(ks[11], (DEPTH, CONV_W, LRU_W), CONV_W ** -0.5),
        'lru_conv_b': nrm(ks[12], (DEPTH, LRU_W), 0.01),
        'lru_wa': nrm(ks[13], (DEPTH, 2, LRU_BLOCKS, LRU_BD, LRU_BD), LRU_BD ** -0.5),
        'lru_ba': nrm(ks[14], (DEPTH, 2, LRU_W), 0.01),
        'lru_wx': nrm(ks[15], (DEPTH, 2, LRU_BLOCKS, LRU_BD, LRU_BD), LRU_BD ** -0.5),
        'lru_bx': nrm(ks[17], (DEPTH, 2, LRU_W), 0.01),
        'lru_lambda': jnp.log(sig) - jnp.log1p(-sig),
        'hgrn_lb_logits': nrm(ks[18], (2, DEPTH + 1, HG_W), 0.1),
        'hgrn_norm': 1.0 + nrm(ks[19], (DEPTH, HG_W), 0.02),
        'router_w': nrm(ks[20], (DEPTH, D, N_EXPERTS), D ** -0.5),
        'router_b': nrm(ks[21], (DEPTH, N_EXPERTS), 0.01),
        'exp_w13': nrm(ks[22], (DEPTH, N_EXPERTS, D, 2 * D_EXPERT), D ** -0.5),
        'exp_w2': nrm(ks[23], (DEPTH, N_EXPERTS, D_EXPERT, D), D_EXPERT ** -0.5),
        'shared_w13': nrm(ks[24], (DEPTH, D, 2 * D_SHARED), D ** -0.5),
        'shared_w2': nrm(ks[25], (DEPTH, D_SHARED, D), D_SHARED ** -0.5),
    }


def reference(x, c, ctx, c_ctx, ada_w, ada_b, norm_mix, norm_ffn, norm_final, w_in, w_out,
              lru_conv_w, lru_conv_b, lru_wa, lru_ba, lru_wx, lru_bx, lru_lambda,
              hgrn_lb_logits, hgrn_norm, router_w, router_b, exp_w13, exp_w2, shared_w13, shared_w2):
    b, n, d = x.shape
    lb_all = jnp.cumsum(jax.nn.softmax(hgrn_lb_logits.astype(jnp.float32), axis=1), axis=1)
    for l in range(DEPTH):
        last = l == DEPTH - 1
        mod_x = (jax.nn.silu(c) @ ada_w[l] + ada_b[l])[:, None, :]
        mod_c = jax.nn.silu(c_ctx) @ ada_w[l] + ada_b[l]
        sh1x, sc1x, g1x, sh2x, sc2x, g2x = jnp.split(mod_x, 6, axis=-1)
        sh1c, sc1c, g1c, sh2c, sc2c, g2c = jnp.split(mod_c, 6, axis=-1)

        hx = _rmsnorm(x, norm_mix[l]) * (1.0 + sc1x) + sh1x
        hc = _rmsnorm(ctx, norm_mix[l]) * (1.0 + sc1c) + sh1c
        yx, yc = _hybrid_mixer(hx, hc, w_in[l], w_out[l], lru_conv_w[l], lru_conv_b[l],
                               lru_wa[l], lru_ba[l], lru_wx[l], lru_bx[l], lru_lambda[l],
                               lb_all[:, l], hgrn_norm[l], not last)
        x = x + g1x * yx
        hx2 = _rmsnorm(x, norm_ffn[l]) * (1.0 + sc2x) + sh2x
        if last:
            y = _moe(hx2.reshape(b * n, d), router_w[l], router_b[l], exp_w13[l], exp_w2[l],
                     shared_w13[l], shared_w2[l])
            x = x + g2x * y.reshape(b, n, d)
        else:
            ctx = ctx + g1c * yc
            hc2 = _rmsnorm(ctx, norm_ffn[l]) * (1.0 + sc2c) + sh2c
            tokens = jnp.concatenate([hx2.reshape(b * n, d), hc2.reshape(-1, d)], axis=0)
            y = _moe(tokens, router_w[l], router_b[l], exp_w13[l], exp_w2[l],
                     shared_w13[l], shared_w2[l])
            x = x + g2x * y[:b * n].reshape(b, n, d)
            ctx = ctx + g2c * y[b * n:].reshape(ctx.shape)
    return _rmsnorm(x, norm_final)
```

```python
import os
import numpy as np
from contextlib import ExitStack
import concourse.bass as bass
import concourse.mybir as mybir
from concourse.bass_utils import run_bass_kernel_spmd

F32 = mybir.dt.float32
BF16 = mybir.dt.bfloat16
I32 = mybir.dt.int32
U32 = mybir.dt.uint32
AF = mybir.ActivationFunctionType
ALU = mybir.AluOpType
AX = mybir.AxisListType

N_DMA_SEMS = 16


class TB:
    def __init__(self, t, name):
        self.t = t
        self.name = name
        self.w = None
        self.r = {}

    def sub(self, name=None):
        return TB(self.t, name or self.name + "_s")


class _Scope:
    def __init__(self, S):
        self.S = S
        self.st = ExitStack()

    def __enter__(self):
        self.st.__enter__()
        return self.st

    def __exit__(self, *a):
        r = self.st.__exit__(*a)
        if a[0] is None:
            self.S.barrier()
        return r


class Sched:
    def __init__(self, nc, st):
        self.nc = nc
        self.st = st
        self.eng = {"pe": nc.tensor, "act": nc.scalar, "dve": nc.vector, "pool": nc.gpsimd, "sp": nc.sync}
        self.sem = {}
        for k in self.eng:
            self.sem[k] = st.enter_context(nc.semaphore("s_" + k))
        self.dq = {"sp": 0, "pool": 1, "act": 2}
        for i in range(3 * N_DMA_SEMS):
            self.sem[("d", i)] = st.enter_context(nc.semaphore("d%d" % i))
        self.tick = {k: 0 for k in self.eng}
        self.seen = {k: {} for k in self.eng}
        self.dval = [0] * (3 * N_DMA_SEMS)
        self.dnext = [0, 0, 0]
        self.finals = []
        self.bregs = {}
        self.ninstr = 0

    def sb(self, name, shape, dtype, st=None):
        t = (st or self.st).enter_context(self.nc.sbuf_tensor(name, list(shape), dtype))
        return TB(t, name)

    def ps(self, name, shape, dtype, st=None):
        t = (st or self.st).enter_context(self.nc.psum_tensor(name, list(shape), dtype))
        return TB(t, name)

    def dram(self, name, shape, dtype):
        t = self.nc.dram_tensor(name, list(shape), dtype, kind="Internal")
        return TB(t, name)

    def _wait(self, E, tok):
        if tok is None:
            return
        key, val = tok
        if key == E and E == "pe":
            return
        if self.seen[E].get(key, 0) >= val:
            return
        self.eng[E].wait_ge(self.sem[key], val)
        self.seen[E][key] = val

    def _deps(self, E, reads, writes, cols=()):
        for b in reads:
            self._wait(E, b.w)
        for b in writes:
            same_ok = any(b is c for c in cols)
            if not (same_ok and b.w is not None and b.w[0] == E):
                self._wait(E, b.w)
            for k, v in list(b.r.items()):
                if same_ok and k == E:
                    continue
                self._wait(E, (k, v))

    def _commit(self, tok, reads, writes):
        key, val = tok
        for b in reads:
            if b.r.get(key, 0) < val:
                b.r[key] = val
        for b in writes:
            b.w = tok
            b.r = {}

    def op(self, E, fn, reads=(), writes=(), cols=()):
        reads = [b for b in reads if isinstance(b, TB)]
        writes = [b for b in writes if isinstance(b, TB)]
        self._deps(E, reads, writes, cols)
        ins = fn(self.eng[E])
        self.tick[E] += 1
        ins.then_inc(self.sem[E], 1)
        self._commit((E, self.tick[E]), reads, writes)
        self.ninstr += 1
        return ins

    def dma(self, Q, dst, src, out, in_, final=False, indirect=None, extra_reads=(), **kw):
        reads = [b for b in [src] + list(extra_reads) if isinstance(b, TB)]
        writes = [b for b in [dst] if isinstance(b, TB)]
        self._deps(Q, reads, writes)
        qi = self.dq[Q]
        s = qi * N_DMA_SEMS + self.dnext[qi]
        self.dnext[qi] = (self.dnext[qi] + 1) % N_DMA_SEMS
        key = ("d", s)
        if self.dval[s] > 0:
            self._wait(Q, (key, self.dval[s]))
        if indirect is not None:
            indirect = dict(indirect)
            bc = indirect.get("bounds_check")
            if isinstance(bc, int):
                if bc not in self.bregs:
                    r = self.eng[Q].alloc_register("bc_%d" % bc)
                    self.eng[Q].reg_mov(r, bc)
                    self.bregs[bc] = r
                indirect["bounds_check"] = self.bregs[bc]
            ins = self.eng[Q].indirect_dma_start(out=out, in_=in_, **indirect, **kw)
        else:
            ins = self.eng[Q].dma_start(out=out, in_=in_, **kw)
        self.dval[s] += 16
        ins.then_inc(self.sem[key], 16)
        tok = (key, self.dval[s])
        self._commit(tok, reads, writes)
        if final:
            self.finals.append(tok)
        self.ninstr += 1
        return ins

    def barrier(self):
        for E in self.eng:
            for F in ("pe", "act", "dve", "pool"):
                if F != E and self.tick[F] > 0:
                    self._wait(E, (F, self.tick[F]))
            for i in range(3 * N_DMA_SEMS):
                if self.dval[i] > 0:
                    self._wait(E, (("d", i), self.dval[i]))

    def scope(self):
        return _Scope(self)

    def finish(self):
        for tok in self.finals:
            self._wait("sp", tok)
        for k in ("pe", "act", "dve", "pool"):
            if self.tick[k] > 0:
                self._wait("sp", (k, self.tick[k]))

    def make_ident(self, ident):
        self.op("pool", lambda e: e.memset(ident.t[:], 0.0), [], [ident])
        n = ident.t.shape[0]
        self.op("pool", lambda e: e.affine_select(out=ident.t[:], in_=ident.t[:], compare_op=ALU.not_equal,
                                                  fill=1.0, base=0, pattern=[[-1, n]], channel_multiplier=1),
                [ident], [ident])


D = 1024
T = 2048
TC = 256
NTOK = TC + T
NB = 2
NE = 256
CAP = 256
NSLOT = 65536
EPS = 1e-6


def build_nc(dbg=(), stop_after=None, ne_decl=NE):
    nc = bass.Bass("TRN2", target_bir_lowering=False)

    def din(name, shape, dt=F32):
        return nc.dram_tensor(name, list(shape), dt, kind="ExternalInput").ap()

    x2 = din("x2", [NB, T, D]); ctx2 = din("ctx2", [NB, TC, D]); cT = din("cT", [D, 3])
    ada_w = din("ada_w", [D, 6 * D]); ada_bc = din("ada_bc", [128, 48])
    nmix = din("nmix", [128, 8]); nffn = din("nffn", [128, 8]); nfin = din("nfin", [1, D])
    w_in = din("w_in", [D, 3584]); w_out = din("w_out", [D, D])
    convw = din("convw", [128, 16]); convb = din("convb", [128, 4])
    wa_bd = din("wa_bd", [128, 8, 128]); wx_bd = din("wx_bd", [128, 8, 128])
    ba_c = din("ba_c", [128, 8]); bx_c = din("bx_c", [128, 8]); lam_c = din("lam_c", [128, 8])
    lbl = din("lbl", [1, 2048]); hnorm = din("hnorm", [1, 512])
    router_w = din("router_w", [D, NE]); router_b = din("router_b", [1, NE])
    w13 = din("w13", [ne_decl * 128, 4096]); w2 = din("w2", [ne_decl * 128, 2048])
    sw13 = din("sw13", [D, 512]); sw2 = din("sw2", [256, D])
    out = nc.dram_tensor("out", [NB, T, D], F32, kind="ExternalOutput").ap()

    dbg_outs = {}

    with ExitStack() as st:
        S = Sched(nc, st)

        def dump(name, tb, ap, shape, dt=F32):
            if name not in dbg:
                return
            d = nc.dram_tensor("dbg_" + name, list(shape), dt, kind="ExternalOutput").ap()
            S.dma("sp", None, tb, out=d, in_=ap, final=True)
            dbg_outs[name] = (shape, dt)

        x1d = S.dram("x1d", [NB * T, D], F32)
        vsave = S.dram("vsave", [T, 512], BF16); qsave = S.dram("qsave", [T, 512], F32)
        vsave_tb = [TB(vsave.t, "vsave%d" % i) for i in range(16)]; qsave_tb = [TB(qsave.t, "qsave%d" % i) for i in range(16)]
        yshd = S.dram("yshd", [NB * T, D], F32)
        xs_pad = S.dram("xs_pad", [NSLOT, D], BF16)
        ys_pad = S.dram("ys_pad", [NSLOT, D], F32)

        ident = S.sb("ident", [128, 128], BF16); S.make_ident(ident)
        identf = S.sb("identf", [128, 128], F32); S.make_ident(identf)
        ones = S.sb("ones", [128, 128], F32)
        S.op("pool", lambda e: e.memset(ones.t[:], 1.0), [], [ones])

        def tri(name, base, cm, step, dt):
            m = S.sb(name, [128, 128], dt)
            S.op("pool", lambda e: e.memset(m.t[:], 1.0), [], [m])
            S.op("pool", lambda e: e.affine_select(out=m.t[:], in_=m.t[:], compare_op=ALU.is_ge, fill=0.0, base=base,
                                                   pattern=[[step, 128]], channel_multiplier=cm), [m], [m])
            v3 = m.t[:].rearrange("p (b i) -> p b i", i=32)
            S.op("pool", lambda e: e.affine_select(out=v3, in_=v3, compare_op=ALU.is_ge, fill=0.0, base=0,
                                                   pattern=[[-32, 4], [0, 32]], channel_multiplier=1), [m], [m])
            S.op("pool", lambda e: e.affine_select(out=v3, in_=v3, compare_op=ALU.is_ge, fill=0.0, base=31,
                                                   pattern=[[32, 4], [0, 32]], channel_multiplier=-1), [m], [m])
            return m

        M_le = tri("M_le", 0, -1, 1, F32)
        M_ge = tri("M_ge", 0, 1, -1, F32)
        M_gt = tri("M_gt", -1, 1, -1, F32)
        M_lt = tri("M_lt", -1, -1, 1, F32)
        blk1 = S.sb("blk1", [128, 4], F32)
        S.op("pool", lambda e: e.memset(blk1.t[:], 1.0), [], [blk1])
        S.op("pool", lambda e: e.affine_select(out=blk1.t[:], in_=blk1.t[:], compare_op=ALU.is_ge, fill=0.0, base=0,
                                               pattern=[[-32, 4]], channel_multiplier=1), [blk1], [blk1])
        S.op("pool", lambda e: e.affine_select(out=blk1.t[:], in_=blk1.t[:], compare_op=ALU.is_ge, fill=0.0, base=31,
                                               pattern=[[32, 4]], channel_multiplier=-1), [blk1], [blk1])
        mask_f = S.sb("mask_f", [128, 4, 128], F32); mask_b = S.sb("mask_b", [128, 4, 128], F32)
        for h in range(4):
            S.op("pool", lambda e: e.tensor_copy(out=mask_f.t[:, h, :], in_=M_le.t[:]), [M_le], [mask_f])
            S.op("pool", lambda e: e.tensor_copy(out=mask_b.t[:, h, :], in_=M_ge.t[:]), [M_ge], [mask_b])

        cmask = S.sb("cmask", [128, 4, 4, 128], BF16)
        S.op("pool", lambda e: e.memset(cmask.t[:], 0.0), [], [cmask])
        for c in range(4):
            S.op("pool", lambda e: e.memset(cmask.t[:, c, :, 32 * c:32 * c + 32], 1.0), [cmask], [cmask])
        def load(name, shape, src, q="sp", dt=F32):
            t = S.sb(name, shape, dt)
            S.dma(q, t, None, out=t.t[:], in_=src)
            return t

        cTs = load("cTs", [128, 8, 3], cT.rearrange("(k p) j -> p k j", p=128))
        adab = load("adab", [128, 48], ada_bc)
        nmix_s = load("nmix_s", [128, 8], nmix); nffn_s = load("nffn_s", [128, 8], nffn)
        nfin_b = load("nfin_b", [128, D], nfin.partition_broadcast(128))
        convw_s = load("convw_s", [128, 16], convw); convb_s = load("convb_s", [128, 4], convb)
        wabd = load("wabd", [128, 8, 128], wa_bd, q="pool", dt=BF16)
        wxbd = load("wxbd", [128, 8, 128], wx_bd, q="pool", dt=BF16)
        ba_s = load("ba_s", [128, 8], ba_c); bx_s = load("bx_s", [128, 8], bx_c); lam_s = load("lam_s", [128, 8], lam_c)
        hn_b = load("hn_b", [128, 512], hnorm.partition_broadcast(128))
        rb_b = load("rb_b", [128, NE], router_b.partition_broadcast(128))

        lbB = S.sb("lbB", [128, 2, 512], F32); omlB = S.sb("omlB", [128, 2, 512], F32)
        with S.scope() as s0:
            lbl_b = S.sb("lbl_b", [128, 2048], F32, s0)
            S.dma("sp", lbl_b, None, out=lbl_b.t[:], in_=lbl.partition_broadcast(128))
            lv = lbl_b.t[:].rearrange("p (d s c) -> p d s c", d=2, s=2)
            for d in range(2):
                S.op("dve", lambda e: e.tensor_tensor(out=lbB.t[:, d, :], in0=lv[:, d, 0, :], in1=lv[:, d, 1, :], op=ALU.subtract), [lbl_b], [lbB])
        S.op("act", lambda e: e.activation(out=lbB.t[:], in_=lbB.t[:], func=AF.Sigmoid), [lbB], [lbB])
        S.op("dve", lambda e: e.tensor_scalar(out=omlB.t[:], in0=lbB.t[:], scalar1=-1.0, scalar2=1.0, op0=ALU.mult, op1=ALU.add), [lbB], [omlB])

        c8 = S.sb("c8", [128, 8], F32); spt = S.sb("spt", [128, 8], F32)
        S.op("dve", lambda e: e.tensor_scalar(out=spt.t[:], in0=lam_s.t[:], scalar1=-1.0, scalar2=None, op0=ALU.mult), [lam_s], [spt])
        S.op("dve", lambda e: e.tensor_tensor(out=spt.t[:], in0=spt.t[:], in1=lam_s.t[:], op=ALU.max), [lam_s, spt], [spt])
        S.op("act", lambda e: e.activation(out=spt.t[:], in_=spt.t[:], func=AF.Exp, scale=-1.0), [spt], [spt])
        S.op("act", lambda e: e.activation(out=spt.t[:], in_=spt.t[:], func=AF.Ln, bias=1.0), [spt], [spt])
        S.op("dve", lambda e: e.tensor_scalar(out=c8.t[:], in0=lam_s.t[:], scalar1=-1.0, scalar2=0.0, op0=ALU.mult, op1=ALU.max), [lam_s], [c8])
        S.op("dve", lambda e: e.tensor_tensor(out=c8.t[:], in0=c8.t[:], in1=spt.t[:], op=ALU.add), [c8, spt], [c8])
        S.op("dve", lambda e: e.tensor_scalar(out=c8.t[:], in0=c8.t[:], scalar1=-8.0, scalar2=None, op0=ALU.mult), [c8], [c8])
        dump("c8", c8, c8.t[:], [128, 8])

        modc = S.sb("modc", [128, 48, 3], F32)
        scT = S.sb("scT", [128, 8, 3], F32)
        S.op("act", lambda e: e.activation(out=scT.t[:], in_=cTs.t[:], func=AF.Silu), [cTs], [scT])
        with S.scope() as sA:
            aw = [S.sb("aw%d" % i, [128, 8, 1024], F32, sA) for i in range(2)]
            pm = S.ps("pm", [128, 512], F32, sA)
            for g in range(6):
                t = aw[g % 2]
                S.dma("sp", t, None, out=t.t[:], in_=ada_w[:, g * 1024:(g + 1) * 1024].rearrange("(k p) n -> p k n", p=128))
                for fc in range(8):
                    f = g * 8 + fc
                    for k in range(8):
                        S.op("pe", lambda e: e.matmul(pm.t[:, f * 3:(f + 1) * 3], lhsT=t.t[:, k, fc * 128:(fc + 1) * 128],
                                                      rhs=scT.t[:, k, :], start=(k == 0), stop=(k == 7)), [t, scT], [pm])
            pmv = pm.t[:, 0:144].rearrange("p (f j) -> p f j", j=3)
            for j in range(3):
                S.op("dve", lambda e: e.tensor_tensor(out=modc.t[:, :, j], in0=pmv[:, :, j], in1=adab.t[:], op=ALU.add), [pm, adab], [modc])
        dump("modc", modc, modc.t[:], [128, 48, 3])
        gm1 = S.sb("gm1", [128, 8, 3], F32); gm2 = S.sb("gm2", [128, 8, 3], F32)
        for j in range(3):
            S.op("dve", lambda e: e.scalar_tensor_tensor(out=gm1.t[:, :, j], in0=modc.t[:, 8:16, j], scalar=1.0, in1=nmix_s.t[:], op0=ALU.add, op1=ALU.mult), [modc, nmix_s], [gm1])
            S.op("dve", lambda e: e.scalar_tensor_tensor(out=gm2.t[:, :, j], in0=modc.t[:, 32:40, j], scalar=1.0, in1=nffn_s.t[:], op0=ALU.add, op1=ALU.mult), [modc, nffn_s], [gm2])

        def row_bcast(dst, col_ap_fn, src_tb, pbank):
            tmp = rb_tmp
            for k in range(8):
                S.op("dve", lambda e: e.tensor_scalar(out=tmp.t[:], in0=ones.t[:], scalar1=col_ap_fn(k), scalar2=None, op0=ALU.mult), [ones, src_tb], [tmp])
                S.op("pe", lambda e: e.transpose(out=pbank.t[:, (k % 4) * 128:(k % 4 + 1) * 128], in_=tmp.t[:], identity=identf.t[:]), [tmp, identf], [pbank])
                if k % 4 == 3:
                    S.op("act", lambda e: e.activation(out=dst.t[:, (k - 3) * 128:(k + 1) * 128], in_=pbank.t[:], func=AF.Copy), [pbank], [dst])

        rb_tmp = S.sb("rb_tmp", [128, 128], F32)

        if stop_after == "A":
            S.finish()
            return nc, dbg_outs
        iota_f = S.sb("iota_f", [128, NE], F32); ebase = None
        with S.scope() as s1:
            iota_i = S.sb("iota_i", [128, NE], I32, s1)
            S.op("pool", lambda e: e.iota(iota_i.t[:], pattern=[[1, NE]], base=0, channel_multiplier=0), [], [iota_i])
            S.op("dve", lambda e: e.tensor_copy(out=iota_f.t[:], in_=iota_i.t[:]), [iota_i], [iota_f])
        Lst = S.sb("Lst", [128, 128], F32)
        S.op("pool", lambda e: e.memset(Lst.t[:], 1.0), [], [Lst])
        S.op("pool", lambda e: e.affine_select(out=Lst.t[:], in_=Lst.t[:], compare_op=ALU.is_ge, fill=0.0, base=-1,
                                               pattern=[[1, 128]], channel_multiplier=-1), [Lst], [Lst])
        carry = S.sb("carry", [128, NE], F32)
        S.op("pool", lambda e: e.memset(carry.t[:], 0.0), [], [carry])
        eidx_all = S.sb("eidx_all", [128, 256], F32); pos_all = S.sb("pos_all", [128, 256], F32)
        hx2d = S.dram("hx2d", [NB * T, D], BF16)
        gates_all = S.sb("gates_all", [128, 32, 8], F32)

        with S.scope() as sPB:
            hxT = S.sb("hxT", [128, 8, NTOK], BF16, sPB)
            yT = S.sb("yT", [128, 8, T], BF16, sPB)
            pT = [S.ps("pT%d" % i, [128, 8, 128], BF16, sPB) for i in range(2)]
            pbk = [S.ps("pbk%d" % i, [128, 512], F32, sPB) for i in range(4)]
            pbc = S.ps("pbc", [128, 512], F32, sPB)

            nb_xt = [S.sb("nb_xt%d" % i, [128, D], F32, sPB) for i in range(2)]
            nb_parts = [[TB(nb_xt[i].t, "nb_xt%d_p%d" % (i, j)) for j in range(4)] for i in range(2)]
            nb_xn = [S.sb("nb_xn%d" % i, [128, D], BF16, sPB) for i in range(2)]
            nb_st = [S.sb("nb_st%d" % i, [128, 4], F32, sPB) for i in range(2)]
            nb_junk = S.sb("nb_junk", [128, D], BF16, sPB)
            nb_tmp = S.sb("nb_tmp", [128, D], F32, sPB)

            def rms_rows(x_tb_list, x_ap, st_, width=D):
                S.op("pool", lambda e: e.memset(st_.t[:, 0:1], 0.0), [], [st_])
                S.op("act", lambda e: e.activation(out=nb_junk.t[:, 0:width], in_=x_ap, func=AF.Square, accum_out=st_.t[:, 0:1]), x_tb_list, [st_])
                S.op("dve", lambda e: e.tensor_scalar(out=st_.t[:, 1:2], in0=st_.t[:, 0:1], scalar1=1.0 / width, scalar2=EPS, op0=ALU.mult, op1=ALU.add), [st_], [st_])
                S.op("act", lambda e: e.activation(out=st_.t[:, 1:2], in_=st_.t[:, 1:2], func=AF.Sqrt), [st_], [st_])
                S.op("dve", lambda e: e.reciprocal(out=st_.t[:, 2:3], in_=st_.t[:, 1:2]), [st_], [st_])

            def norm_T(ti, srcs, gmb, shb, dst_tb, dst_ap):
                xt = nb_xt[ti % 2]; parts = nb_parts[ti % 2]; xn = nb_xn[ti % 2]; st_ = nb_st[ti % 2]; p = pT[ti % 2]
                for j, (psl, ap) in enumerate(srcs):
                    S.dma("sp", parts[j], None, out=xt.t[psl, :], in_=ap)
                rms_rows(parts, xt.t[:], st_)
                S.op("dve", lambda e: e.scalar_tensor_tensor(out=nb_tmp.t[:], in0=xt.t[:], scalar=st_.t[:, 2:3], in1=gmb.t[:], op0=ALU.mult, op1=ALU.mult), parts + [st_, gmb], [nb_tmp])
                S.op("dve", lambda e: e.tensor_tensor(out=xn.t[:], in0=nb_tmp.t[:], in1=shb.t[:], op=ALU.add), [nb_tmp, shb], [xn])
                def stage2():
                    for k in range(8):
                        S.op("pe", lambda e: e.transpose(out=p.t[:, k, :], in_=xn.t[:, k * 128:(k + 1) * 128], identity=ident.t[:]), [xn, ident], [p])
                    S.op("act", lambda e: e.activation(out=dst_ap, in_=p.t[:], func=AF.Copy), [p], [dst_tb], cols=[dst_tb])
                return stage2

            def norm_loop(items):
                pend = None
                for args in items:
                    n2 = norm_T(*args)
                    if pend is not None:
                        pend()
                    pend = n2
                if pend is not None:
                    pend()


            for b in range(NB):
                with S.scope() as sB:
                    with S.scope() as sN1:
                        gm1c_b = S.sb("gm1c_b%d" % b, [128, D], F32, sN1); sh1c_b = S.sb("sh1c_b%d" % b, [128, D], F32, sN1)
                        row_bcast(gm1c_b, lambda k: gm1.t[:, k, 2:3], gm1, pbc)
                        row_bcast(sh1c_b, lambda k: modc.t[:, k, 2:3], modc, pbc)
                        gm1x_b = S.sb("gm1x_b%d" % b, [128, D], F32, sN1); sh1x_b = S.sb("sh1x_b%d" % b, [128, D], F32, sN1)
                        row_bcast(gm1x_b, lambda k: gm1.t[:, k, b:b + 1], gm1, pbc)
                        row_bcast(sh1x_b, lambda k: modc.t[:, k, b:b + 1], modc, pbc)
                        items = [(i, [(slice(0, 128), ctx2[b, i * 128:(i + 1) * 128, :])], gm1c_b, sh1c_b, hxT, hxT.t[:, :, i * 128:(i + 1) * 128]) for i in range(2)]
                        items += [(i, [(slice(0, 128), x2[b, i * 128:(i + 1) * 128, :])], gm1x_b, sh1x_b, hxT, hxT.t[:, :, TC + i * 128:TC + (i + 1) * 128]) for i in range(16)]
                        norm_loop(items)
                    if b == 0:
                        dump("hxT", hxT, hxT.t[:], [128, 8, NTOK], BF16)

                    with S.scope() as sL:
                        wl = S.sb("wl%d" % b, [128, 8, 1024], BF16, sL)
                        S.dma("pool", wl, None, out=wl.t[:], in_=w_in[:, 0:1024].rearrange("(k p) n -> p k n", p=128))
                        rx = S.sb("l_rx%d" % b, [128, NTOK], F32, sL); u = S.sb("l_u%d" % b, [128, NTOK], F32, sL)
                        ubf = S.sb("l_ubf%d" % b, [128, NTOK], BF16, sL); gg = S.sb("l_gg%d" % b, [128, T], BF16, sL)
                        R = S.sb("l_R%d" % b, [128, NTOK], F32, sL); I_ = S.sb("l_I%d" % b, [128, NTOK], F32, sL)
                        H = S.sb("l_H%d" % b, [128, T], F32, sL); Hc = S.sb("l_Hc%d" % b, [128, TC], F32, sL)
                        gt = S.sb("l_gt%d" % b, [128, 512], F32, sL)
                        blocks = [(0, 256)] + [(TC + i * 512, 512) for i in range(4)]
                        for c in range(4):
                            for bi, (t0, n) in enumerate(blocks):
                                pp = pbk[bi % 4]
                                for k in range(8):
                                    S.op("pe", lambda e: e.matmul(pp.t[:, 0:n], lhsT=wl.t[:, k, c * 128:(c + 1) * 128], rhs=hxT.t[:, k, t0:t0 + n], start=(k == 0), stop=(k == 7)), [wl, hxT], [pp])
                                S.op("act", lambda e: e.activation(out=rx.t[:, t0:t0 + n], in_=pp.t[:, 0:n], func=AF.Copy), [pp], [rx])
                            for bi in range(4):
                                t0 = TC + bi * 512; pp = pbk[bi % 4]
                                for k in range(8):
                                    S.op("pe", lambda e: e.matmul(pp.t[:], lhsT=wl.t[:, k, 512 + c * 128:512 + (c + 1) * 128], rhs=hxT.t[:, k, t0:t0 + 512], start=(k == 0), stop=(k == 7)), [wl, hxT], [pp])
                                S.op("act", lambda e: e.activation(out=gt.t[:], in_=pp.t[:], func=AF.Square), [pp], [gt])
                                S.op("dve", lambda e: e.tensor_scalar(out=gt.t[:], in0=gt.t[:], scalar1=0.044715, scalar2=1.0, op0=ALU.mult, op1=ALU.add), [gt], [gt])
                                S.op("dve", lambda e: e.tensor_tensor(out=gt.t[:], in0=gt.t[:], in1=pp.t[:], op=ALU.mult), [gt, pp], [gt])
                                S.op("act", lambda e: e.activation(out=gt.t[:], in_=gt.t[:], func=AF.Sigmoid, scale=1.5957691216), [gt], [gt])
                                S.op("dve", lambda e: e.tensor_tensor(out=gg.t[:, bi * 512:(bi + 1) * 512], in0=gt.t[:], in1=pp.t[:], op=ALU.mult), [gt, pp], [gg])
                            cw = lambda tap: convw_s.t[:, c * 4 + tap:c * 4 + tap + 1]
                            for (s0, n) in ((0, TC), (TC, T)):
                                S.op("dve", lambda e: e.tensor_scalar(out=u.t[:, s0:s0 + n], in0=rx.t[:, s0:s0 + n], scalar1=cw(1), scalar2=convb_s.t[:, c:c + 1], op0=ALU.mult, op1=ALU.add), [rx, convw_s, convb_s], [u])
                                S.op("dve", lambda e: e.scalar_tensor_tensor(out=u.t[:, s0 + 1:s0 + n], in0=rx.t[:, s0:s0 + n - 1], scalar=cw(0), in1=u.t[:, s0 + 1:s0 + n], op0=ALU.mult, op1=ALU.add), [rx, u, convw_s], [u])
                                S.op("dve", lambda e: e.scalar_tensor_tensor(out=u.t[:, s0:s0 + n - 1], in0=rx.t[:, s0 + 1:s0 + n], scalar=cw(2), in1=u.t[:, s0:s0 + n - 1], op0=ALU.mult, op1=ALU.add), [rx, u, convw_s], [u])
                                S.op("dve", lambda e: e.scalar_tensor_tensor(out=u.t[:, s0:s0 + n - 2], in0=rx.t[:, s0 + 2:s0 + n], scalar=cw(3), in1=u.t[:, s0:s0 + n - 2], op0=ALU.mult, op1=ALU.add), [rx, u, convw_s], [u])
                            S.op("act", lambda e: e.activation(out=ubf.t[:], in_=u.t[:], func=AF.Copy), [u], [ubf])
                            if b == 0 and c == 0:
                                dump("lru_u", u, u.t[:], [128, NTOK])
                            for d in range(2):
                                dc = d * 4 + c
                                for bi, (t0, n) in enumerate(blocks):
                                    pa = pbk[(2 * bi) % 4]; px = pbk[(2 * bi + 1) % 4]
                                    S.op("pe", lambda e: e.matmul(pa.t[:, 0:n], lhsT=wabd.t[:, dc, :], rhs=ubf.t[:, t0:t0 + n], start=True, stop=True), [wabd, ubf], [pa])
                                    S.op("pe", lambda e: e.matmul(px.t[:, 0:n], lhsT=wxbd.t[:, dc, :], rhs=ubf.t[:, t0:t0 + n], start=True, stop=True), [wxbd, ubf], [px])
                                    S.op("act", lambda e: e.activation(out=R.t[:, t0:t0 + n], in_=pa.t[:, 0:n], func=AF.Sigmoid, bias=ba_s.t[:, dc:dc + 1]), [pa, ba_s], [R])
                                    S.op("act", lambda e: e.activation(out=I_.t[:, t0:t0 + n], in_=px.t[:, 0:n], func=AF.Sigmoid, bias=bx_s.t[:, dc:dc + 1]), [px, bx_s], [I_])
                                S.op("act", lambda e: e.activation(out=R.t[:], in_=R.t[:], func=AF.Exp, scale=c8.t[:, dc:dc + 1]), [R, c8], [R])
                                S.op("dve", lambda e: e.tensor_tensor(out=rx.t[:], in0=R.t[:], in1=R.t[:], op=ALU.mult), [R], [rx])
                                S.op("act", lambda e: e.activation(out=rx.t[:], in_=rx.t[:], func=AF.Sqrt, scale=-1.0, bias=1.0), [rx], [rx])
                                S.op("dve", lambda e: e.tensor_tensor(out=I_.t[:], in0=I_.t[:], in1=rx.t[:], op=ALU.mult), [I_, rx], [I_])
                                S.op("pool", lambda e: e.tensor_tensor(out=I_.t[:], in0=I_.t[:], in1=u.t[:], op=ALU.mult), [I_, u], [I_])
                                A_c = R.t[:, 0:TC]; B_c = I_.t[:, 0:TC]; A_l = R.t[:, TC:NTOK]; B_l = I_.t[:, TC:NTOK]
                                if d == 0:
                                    S.op("dve", lambda e: e.tensor_tensor_scan(out=Hc.t[:], data0=A_c, data1=B_c, initial=0.0, op0=ALU.mult, op1=ALU.add), [R, I_], [Hc])
                                    S.op("dve", lambda e: e.tensor_tensor_scan(out=H.t[:], data0=A_l, data1=B_l, initial=Hc.t[:, TC - 1:TC], op0=ALU.mult, op1=ALU.add), [R, I_, Hc], [H])
                                else:
                                    S.op("dve", lambda e: e.tensor_tensor_scan(out=Hc.t[:][:, ::-1], data0=A_c[:, ::-1], data1=B_c[:, ::-1], initial=0.0, op0=ALU.mult, op1=ALU.add), [R, I_], [Hc])
                                    hb = rx.t[:, 0:T]
                                    S.op("dve", lambda e: e.tensor_tensor_scan(out=hb[:, ::-1], data0=A_l[:, ::-1], data1=B_l[:, ::-1], initial=Hc.t[:, 0:1], op0=ALU.mult, op1=ALU.add), [R, I_, Hc], [rx])
                                    S.op("pool", lambda e: e.tensor_tensor(out=H.t[:], in0=H.t[:], in1=hb, op=ALU.add), [H, rx], [H])
                            if b == 0 and c == 0:
                                dump("lru_H", H, H.t[:], [128, T])
                            S.op("dve", lambda e: e.tensor_tensor(out=yT.t[:, c, :], in0=H.t[:], in1=gg.t[:], op=ALU.mult), [H, gg], [yT])
                    if b == 0:
                        dump("yT_lru", yT, yT.t[:, 0:4, :], [128, 4, T], BF16)
                    if stop_after == "L":
                        S.finish()
                        return nc, dbg_outs
                    with S.scope() as sN2:
                        gm1x_b = S.sb("gm1xc_b%d" % b, [128, D], F32, sN2); sh1x_b = S.sb("sh1xc_b%d" % b, [128, D], F32, sN2)
                        row_bcast(gm1x_b, lambda k: gm1.t[:, k, b:b + 1], gm1, pbc)
                        row_bcast(sh1x_b, lambda k: modc.t[:, k, b:b + 1], modc, pbc)
                        xcm = x2[b].rearrange("(r c) d -> c r d", c=64)
                        items = []
                        for i in range(16):
                            srcs = [(slice(cl * 32, (cl + 1) * 32), xcm[4 * i + cl]) for cl in range(4)]
                            items.append((i, srcs, gm1x_b, sh1x_b, hxT, hxT.t[:, :, TC + i * 128:TC + (i + 1) * 128]))
                        norm_loop(items)
                    if stop_after == "CM":
                        dump("hxT_cm", hxT, hxT.t[:], [128, 8, NTOK], BF16)
                        S.finish()
                        return nc, dbg_outs
                    with S.scope() as sH:
                        pb8 = S.ps("pb8_%d" % b, [128, 512], F32, sH)
                        whg = S.sb("whg%d" % b, [128, 8, 2048], BF16, sH)
                        whs = [TB(whg.t, "whg%d_s%d" % (b, i)) for i in range(4)]
                        WCOL = {"q": 1024, "v": 1536, "ff": 2048, "fb": 2560, "g": 3072}

                        def wload(slot, name):
                            c0 = WCOL[name]
                            S.dma("pool", whs[slot], None, out=whg.t[:, :, slot * 512:(slot + 1) * 512], in_=w_in[:, c0:c0 + 512].rearrange("(k p) n -> p k n", p=128))

                        wload(0, "q"); wload(1, "v"); wload(2, "ff"); wload(3, "fb")
                        of = S.sb("h_of%d" % b, [128, 16, 512], BF16, sH)
                        Sst = [S.sb("h_S%d_%d" % (b, d), [128, 512], F32, sH) for d in range(2)]
                        Sbf = S.sb("h_Sbf%d" % b, [128, 512], BF16, sH)
                        t_s = S.sb("h_ts%d" % b, [128, 512], F32, sH); t_lf = S.sb("h_tlf%d" % b, [128, 512], F32, sH)
                        t_kf = S.sb("h_tkf%d" % b, [128, 512], F32, sH); t_e = S.sb("h_te%d" % b, [128, 512], F32, sH)
                        dec = S.sb("h_dec%d" % b, [128, 16], F32, sH)
                        kkc4 = [S.sb("h_kkc%d_%d" % (b, cc), [128, 512], BF16, sH) for cc in range(4)]
                        kkc4_src = [None, None]
                        hsl = lambda h: slice(h * 128, (h + 1) * 128)

                        def proj_tok(pp, tok0, slot):
                            for k in range(8):
                                S.op("pe", lambda e: e.matmul(pp.t[:], lhsT=hxT.t[:, k, tok0:tok0 + 128], rhs=whg.t[:, k, slot * 512:(slot + 1) * 512], start=(k == 0), stop=(k == 7)), [hxT, whs[slot]], [pp])

                        def gates(tok0, d, slot, kk_out, dec_out, latent):
                            pz = pbk[0]
                            proj_tok(pz, tok0, slot)
                            S.op("act", lambda e: e.activation(out=t_s.t[:], in_=pz.t[:], func=AF.Sigmoid), [pz], [t_s])
                            S.op("dve", lambda e: e.tensor_tensor(out=t_s.t[:], in0=t_s.t[:], in1=omlB.t[:, d, :], op=ALU.mult), [t_s, omlB], [t_s])
                            S.op("dve", lambda e: e.tensor_tensor(out=t_s.t[:], in0=t_s.t[:], in1=lbB.t[:, d, :], op=ALU.add), [t_s, lbB], [t_s])
                            S.op("act", lambda e: e.activation(out=t_lf.t[:], in_=t_s.t[:], func=AF.Ln), [t_s], [t_lf])
                            S.op("dve", lambda e: e.tensor_scalar(out=t_kf.t[:], in0=t_s.t[:], scalar1=-1.0, scalar2=1.0, op0=ALU.mult, op1=ALU.add), [t_s], [t_kf])
                            prg = pbk[1]
                            Mr = M_gt if d == 0 else M_lt
                            S.op("pe", lambda e: e.matmul(prg.t[:], lhsT=Mr.t[:], rhs=t_lf.t[:], start=True, stop=True), [Mr, t_lf], [prg])
                            S.op("act", lambda e: e.activation(out=t_e.t[:], in_=prg.t[:], func=AF.Exp), [prg], [t_e])
                            S.op("dve", lambda e: e.tensor_tensor(out=kk_out.t[:], in0=t_kf.t[:], in1=t_e.t[:], op=ALU.mult), [t_kf, t_e], [kk_out])
                            for h in range(4):
                                S.op("pe", lambda e: e.matmul(pbc.t[:, h * 4:(h + 1) * 4], lhsT=t_lf.t[:, hsl(h)], rhs=blk1.t[:], start=True, stop=True), [t_lf, blk1], [pbc])
                            S.op("act", lambda e: e.activation(out=dec_out.t[:], in_=pbc.t[:, 0:16], func=AF.Exp), [pbc], [dec_out])
                            if latent:
                                pg = pbk[1]
                                Mg = M_le if d == 0 else M_ge
                                S.op("pe", lambda e: e.matmul(pg.t[:], lhsT=Mg.t[:], rhs=t_lf.t[:], start=True, stop=True), [Mg, t_lf], [pg])
                                S.op("act", lambda e: e.activation(out=t_e2.t[:], in_=pg.t[:], func=AF.Exp), [pg], [t_e2])
                                S.op("dve", lambda e: e.tensor_tensor(out=qd.t[:], in0=qf.t[:], in1=t_e2.t[:], op=ALU.mult), [qf, t_e2], [qd])
                                S.op("act", lambda e: e.activation(out=t_e.t[:], in_=pg.t[:], func=AF.Exp, scale=-1.0), [pg], [t_e])
                                S.op("dve", lambda e: e.tensor_tensor(out=kd.t[:], in0=t_kf.t[:], in1=t_e.t[:], op=ALU.mult), [t_kf, t_e], [kd])

                        def state_step(d, c, kk_tb, v_tb, dec_tb):
                            pkv = pb8
                            kc_ = kk_tb
                            for h in range(4):
                                S.op("pe", lambda e: e.matmul(pkv.t[:, hsl(h)], lhsT=kc_.t[:, hsl(h)], rhs=v_tb.t[:, hsl(h)], start=True, stop=True), [kc_, v_tb], [pkv])
                            for h in range(4):
                                S.op("dve", lambda e: e.scalar_tensor_tensor(out=Sst[d].t[:, hsl(h)], in0=Sst[d].t[:, hsl(h)], scalar=dec_tb.t[:, h * 4 + c:h * 4 + c + 1], in1=pkv.t[:, hsl(h)], op0=ALU.mult, op1=ALU.add), [Sst[d], dec_tb, pkv], [Sst[d]], cols=[Sst[d]])
                            S.op("act", lambda e: e.activation(out=Sbf.t[:], in_=Sst[d].t[:], func=AF.Copy), [Sst[d]], [Sbf])

                        sC_cm = S.scope(); sC = sC_cm.__enter__()
                        c_v = [S.sb("h_cv%d_%d" % (b, t), [128, 512], BF16, sC) for t in range(2)]
                        c_kk = [[S.sb("h_ckk%d_%d_%d" % (b, d, t), [128, 512], BF16, sC) for t in range(2)] for d in range(2)]
                        c_dec = [[S.sb("h_cdec%d_%d_%d" % (b, d, t), [128, 16], F32, sC) for t in range(2)] for d in range(2)]
                        for d in range(2):
                            S.op("pool", lambda e: e.memset(Sst[d].t[:], 0.0), [], [Sst[d]])
                        for t in range(2):
                            pv = pbk[3]
                            proj_tok(pv, t * 128, 1)
                            S.op("act", lambda e: e.activation(out=c_v[t].t[:], in_=pv.t[:], func=AF.Copy), [pv], [c_v[t]])
                            for d in range(2):
                                gates(t * 128, d, 2 + d, c_kk[d][t], c_dec[d][t], False)
                        for d in range(2):
                            order = [(t, c) for t in range(2) for c in range(4)]
                            if d == 1:
                                order = order[::-1]
                            for (t, c) in order:
                                S.op("dve", lambda e: e.tensor_scalar(out=kkc4[c].t[:], in0=c_kk[d][t].t[:], scalar1=blk1.t[:, c:c + 1], scalar2=None, op0=ALU.mult), [c_kk[d][t], blk1], [kkc4[c]])
                                state_step(d, c, kkc4[c], c_v[t], c_dec[d][t])
                        if b == 0:
                            dump("hg_sc0", Sst[0], Sst[0].t[:], [128, 512]); dump("hg_sc1", Sst[1], Sst[1].t[:], [128, 512])

                        if stop_after == "CTX":
                            S.finish()
                            return nc, dbg_outs
                        sC_cm.__exit__(None, None, None)
                        vt = S.sb("h_vt%d" % b, [128, 512], BF16, sH); qf = S.sb("h_qf%d" % b, [128, 512], F32, sH)
                        t_e2 = S.sb("h_te2%d" % b, [128, 512], F32, sH)
                        qd = S.sb("h_qd%d" % b, [128, 512], BF16, sH); kd = S.sb("h_kd%d" % b, [128, 512], BF16, sH)
                        qdT = S.sb("h_qdT%d" % b, [128, 4, 128], BF16, sH); kdT = S.sb("h_kdT%d" % b, [128, 4, 128], BF16, sH)
                        sTm = S.sb("h_sTm%d" % b, [128, 512], BF16, sH)
                        qdTc = S.sb("h_qdTc%d" % b, [128, 4, 4, 128], BF16, sH)
                        t_hg = TB(nb_tmp.t[:, 0:512], "h_hg%d" % b); sg = TB(nb_tmp.t[:, 512:1024], "h_sg%d" % b)
                        ybf = S.sb("h_ybf%d" % b, [128, 512], BF16, sH); st4 = S.sb("h_st4%d" % b, [128, 12], F32, sH)
                        kk = S.sb("h_kk%d" % b, [128, 512], BF16, sH)
                        dec_b = S.sb("h_dec2_%d" % b, [128, 16], F32, sH)
                        a0 = nb_xt[0].t[:].bitcast(BF16); a1 = nb_xt[1].t[:].bitcast(BF16); a2 = nb_xn[0].t[:]
                        vt_s = [vt, TB(a2[:, 0:512], "h_vt2_%d" % b)]
                        sTm_s = [sTm, TB(a2[:, 512:1024], "h_sTm2_%d" % b)]
                        dec_s = [dec, dec_b]
                        qdTc_s = [qdTc, TB(a0.rearrange("p (c h i) -> p c h i", c=4, h=4), "h_qdTc2_%d" % b)]
                        kkc4_s = [kkc4, [TB(a1[:, cc * 512:(cc + 1) * 512], "h_kkc2_%d_%d" % (b, cc)) for cc in range(4)]]

                        def prep_gen(d, t, B):
                            tok0 = TC + t * 128
                            vt_ = vt_s[B]; sTm_ = sTm_s[B]; dec_ = dec_s[B]; qdTc_ = qdTc_s[B]; kkc_ = kkc4_s[B]
                            pv = pbk[3]
                            if d == 0:
                                proj_tok(pv, tok0, 1)
                                S.op("act", lambda e: e.activation(out=vt_.t[:], in_=pv.t[:], func=AF.Copy), [pv], [vt_])
                                proj_tok(pv, tok0, 0)
                                S.op("act", lambda e: e.activation(out=qf.t[:], in_=pv.t[:], func=AF.Copy), [pv], [qf])
                                S.dma("sp", vsave_tb[t], vt_, out=vsave.t.ap()[t * 128:(t + 1) * 128, :], in_=vt_.t[:])
                                S.dma("sp", qsave_tb[t], qf, out=qsave.t.ap()[t * 128:(t + 1) * 128, :], in_=qf.t[:])
                            else:
                                S.dma("sp", vt_, vsave_tb[t], out=vt_.t[:], in_=vsave.t.ap()[t * 128:(t + 1) * 128, :])
                                S.dma("sp", qf, qsave_tb[t], out=qf.t[:], in_=qsave.t.ap()[t * 128:(t + 1) * 128, :])
                            yield
                            gates(tok0, d, 2, kk, dec_, True)
                            for cc in range(4):
                                S.op("dve", lambda e: e.tensor_scalar(out=kkc_[cc].t[:], in0=kk.t[:], scalar1=blk1.t[:, cc:cc + 1], scalar2=None, op0=ALU.mult), [kk, blk1], [kkc_[cc]])
                            yield
                            pq = pT[0]
                            for h in range(4):
                                S.op("pe", lambda e: e.transpose(out=pq.t[:, h, :], in_=qd.t[:, hsl(h)], identity=ident.t[:]), [qd, ident], [pq])
                                S.op("pe", lambda e: e.transpose(out=pq.t[:, 4 + h, :], in_=kd.t[:, hsl(h)], identity=ident.t[:]), [kd, ident], [pq])
                            S.op("act", lambda e: e.activation(out=qdT.t[:], in_=pq.t[:, 0:4, :], func=AF.Copy), [pq], [qdT])
                            S.op("act", lambda e: e.activation(out=kdT.t[:], in_=pq.t[:, 4:8, :], func=AF.Copy), [pq], [kdT])
                            yield
                            ps_ = pbk[0]
                            for h in range(4):
                                S.op("pe", lambda e: e.matmul(ps_.t[:, hsl(h)], lhsT=kdT.t[:, h, :], rhs=qdT.t[:, h, :], start=True, stop=True), [kdT, qdT], [ps_])
                            msk = mask_f if d == 0 else mask_b
                            S.op("dve", lambda e: e.tensor_tensor(out=sTm_.t[:], in0=ps_.t[:], in1=msk.t[:].rearrange("p h i -> p (h i)"), op=ALU.mult), [ps_, msk], [sTm_])
                            for c in range(4):
                                S.op("dve", lambda e: e.tensor_tensor(out=qdTc_.t[:, c, :, :], in0=qdT.t[:], in1=cmask.t[:, c, :, :], op=ALU.mult), [qdT, cmask], [qdTc_])
                            yield

                        def recur_gen(d, t, B):
                            tok0 = TC + t * 128
                            vt_ = vt_s[B]; sTm_ = sTm_s[B]; dec_ = dec_s[B]; qdTc_ = qdTc_s[B]; kkc_ = kkc4_s[B]
                            po = pbk[2]
                            for h in range(4):
                                S.op("pe", lambda e: e.matmul(po.t[:, hsl(h)], lhsT=sTm_.t[:, hsl(h)], rhs=vt_.t[:, hsl(h)], start=(h == 0), stop=False), [sTm_, vt_], [po])
                            cs = range(4) if d == 0 else range(3, -1, -1)
                            for c in cs:
                                for h in range(4):
                                    S.op("pe", lambda e: e.matmul(po.t[:, hsl(h)], lhsT=qdTc_.t[:, c, h, :], rhs=Sbf.t[:, hsl(h)], start=False, stop=True), [qdTc_, Sbf], [po])
                                state_step(d, c, kkc_[c], vt_, dec_)
                                yield
                            if d == 0:
                                S.op("act", lambda e: e.activation(out=of.t[:, t, :], in_=po.t[:], func=AF.Copy), [po], [of])
                                return
                            S.op("dve", lambda e: e.tensor_tensor(out=t_hg.t[:], in0=po.t[:], in1=of.t[:, t, :], op=ALU.add), [po, of], [t_hg])
                            if b == 0 and t == 3:
                                dump("hg_t3", t_hg, t_hg.t[:], [128, 512])
                            S.op("pool", lambda e: e.memset(st4.t[:], 0.0), [], [st4])
                            for h in range(4):
                                S.op("act", lambda e: e.activation(out=nb_junk.t[:, 0:128], in_=t_hg.t[:, hsl(h)], func=AF.Square, accum_out=st4.t[:, h:h + 1]), [t_hg], [st4], cols=[st4])
                            S.op("dve", lambda e: e.tensor_scalar(out=st4.t[:, 4:8], in0=st4.t[:, 0:4], scalar1=1.0 / 128, scalar2=EPS, op0=ALU.mult, op1=ALU.add), [st4], [st4])
                            S.op("act", lambda e: e.activation(out=st4.t[:, 4:8], in_=st4.t[:, 4:8], func=AF.Sqrt), [st4], [st4])
                            S.op("dve", lambda e: e.reciprocal(out=st4.t[:, 8:12], in_=st4.t[:, 4:8]), [st4], [st4])
                            yield
                            pgx = pbk[2]
                            proj_tok(pgx, tok0, 3)
                            S.op("act", lambda e: e.activation(out=sg.t[:], in_=pgx.t[:], func=AF.Silu), [pgx], [sg])
                            for h in range(4):
                                S.op("dve", lambda e: e.scalar_tensor_tensor(out=t_hg.t[:, hsl(h)], in0=t_hg.t[:, hsl(h)], scalar=st4.t[:, 8 + h:9 + h], in1=hn_b.t[:, hsl(h)], op0=ALU.mult, op1=ALU.mult), [t_hg, st4, hn_b], [t_hg], cols=[t_hg])
                            S.op("dve", lambda e: e.tensor_tensor(out=ybf.t[:], in0=t_hg.t[:], in1=sg.t[:], op=ALU.mult), [t_hg, sg], [ybf])
                            yield
                            py_ = pT[1]
                            for h in range(4):
                                S.op("pe", lambda e: e.transpose(out=py_.t[:, h, :], in_=ybf.t[:, hsl(h)], identity=ident.t[:]), [ybf, ident], [py_])
                            for h in range(4):
                                dst = yT.t[:, 4 + h, :].rearrange("p (r c) -> p c r", c=64)[:, 4 * t:4 * t + 4, :]
                                src = py_.t[:, h, :].rearrange("p (c r) -> p c r", r=32)
                                S.op("act", lambda e: e.activation(out=dst, in_=src, func=AF.Copy), [py_], [yT], cols=[yT])

                        for d in range(2):
                            if d == 1:
                                wload(2, "fb"); wload(3, "g")
                            S.op("act", lambda e: e.activation(out=Sbf.t[:], in_=Sst[d].t[:], func=AF.Copy), [Sst[d]], [Sbf])
                            tiles = range(16) if d == 0 else range(15, -1, -1)
                            tiles = list(tiles)[:int(os.environ.get('HG_NT', '16'))]
                            for _ in prep_gen(d, tiles[0], 0):
                                pass
                            for n_, t in enumerate(tiles):
                                rec = recur_gen(d, t, n_ % 2)
                                nxt = prep_gen(d, tiles[n_ + 1], (n_ + 1) % 2) if n_ + 1 < len(tiles) else iter(())
                                ra = True; na = True
                                while ra or na:
                                    if ra:
                                        try:
                                            next(rec)
                                        except StopIteration:
                                            ra = False
                                    if na:
                                        try:
                                            next(nxt)
                                        except StopIteration:
                                            na = False
                            if b == 0 and d == 0:
                                dump("hg_of", of, of.t[:], [128, 16, 512], BF16)
                    if b == 0:
                        dump("yT", yT, yT.t[:], [128, 8, T], BF16)
                    if stop_after == "HG":
                        S.finish()
                        return nc, dbg_outs
                    with S.scope() as sW:
                        wo = S.sb("wo%d" % b, [128, 8, D], BF16, sW)
                        S.dma("pool", wo, None, out=wo.t[:], in_=w_out.rearrange("(k p) n -> p k n", p=128))
                        g1_b = S.sb("g1_b%d" % b, [128, D], F32, sW); gm2_b = S.sb("gm2_b%d" % b, [128, D], F32, sW); sh2_b = S.sb("sh2_b%d" % b, [128, D], F32, sW)
                        row_bcast(g1_b, lambda k: modc.t[:, 16 + k, b:b + 1], modc, pbc)
                        row_bcast(gm2_b, lambda k: gm2.t[:, k, b:b + 1], gm2, pbc)
                        row_bcast(sh2_b, lambda k: modc.t[:, 24 + k, b:b + 1], modc, pbc)
                        rw_s = S.sb("rw_s%d" % b, [128, 8, NE], BF16, sW)
                        S.dma("pool", rw_s, None, out=rw_s.t[:], in_=router_w.rearrange("(k p) n -> p k n", p=128))
                        sw13_s = S.sb("sw13_s%d" % b, [128, 8, 512], BF16, sW)
                        S.dma("pool", sw13_s, None, out=sw13_s.t[:], in_=sw13.rearrange("(k p) n -> p k n", p=128))
                        sw2_s = S.sb("sw2_s%d" % b, [128, 2, D], BF16, sW)
                        S.dma("pool", sw2_s, None, out=sw2_s.t[:], in_=sw2.rearrange("(k p) n -> p k n", p=128))
                        x1t = S.sb("w_x1t%d" % b, [128, D], F32, sW); hx2T = S.sb("w_hx2T%d" % b, [128, 8, 128], BF16, sW)
                        r_sc = S.sb("r_sc%d" % b, [128, NE], F32, sW); r_bi = S.sb("r_bi%d" % b, [128, NE], F32, sW)
                        r_mk = S.sb("r_mk%d" % b, [128, NE], F32, sW); r_sel = S.sb("r_sel%d" % b, [128, NE], F32, sW)
                        r_pos = S.sb("r_pos%d" % b, [128, NE], F32, sW); r_t1 = S.sb("r_t1%d" % b, [128, NE], F32, sW)
                        r_m8 = S.sb("r_m8%d" % b, [128, 8, 8], F32, sW); r_sm = S.sb("r_sm%d" % b, [128, 64], F32, sW)
                        r_ei = S.sb("r_ei%d" % b, [128, 8], U32, sW)
                        hTs = S.sb("w_hTs%d" % b, [128, 2, 128], BF16, sW); hsil = S.sb("w_hsil%d" % b, [128, 256], F32, sW)
                        ysh_t = S.sb("w_ysh%d" % b, [128, D], F32, sW)
                        hx2T_s = [hx2T, S.sb("w_hx2T1_%d" % b, [128, 8, 128], BF16, sW)]
                        x1t_s = [x1t, S.sb("w_x1t1_%d" % b, [128, D], F32, sW)]
                        pw8 = S.ps("pw8_%d" % b, [128, 512], F32, sW)

                        def wo_s1(i):
                            gi = b * 16 + i
                            par = i % 2
                            xt = nb_xt[par]; parts = nb_parts[par]; hx2b = nb_xn[par]; st_ = nb_st[par]; p = pT[par]
                            hx2T = hx2T_s[par]; x1t = x1t_s[par]
                            S.dma("sp", parts[0], None, out=xt.t[:], in_=x2[b, i * 128:(i + 1) * 128, :])
                            for half in range(2):
                                pp = pbk[half]
                                for k in range(8):
                                    S.op("pe", lambda e: e.matmul(pp.t[:], lhsT=yT.t[:, k, i * 128:(i + 1) * 128], rhs=wo.t[:, k, half * 512:(half + 1) * 512], start=(k == 0), stop=(k == 7)), [yT, wo], [pp])
                                S.op("dve", lambda e: e.tensor_tensor(out=nb_tmp.t[:, half * 512:(half + 1) * 512], in0=pp.t[:], in1=g1_b.t[:, half * 512:(half + 1) * 512], op=ALU.mult), [pp, g1_b], [nb_tmp])
                            S.op("dve", lambda e: e.tensor_tensor(out=x1t.t[:], in0=nb_tmp.t[:], in1=xt.t[:], op=ALU.add), [nb_tmp] + parts, [x1t])
                            S.dma("sp", x1d, x1t, out=x1d.t.ap()[gi * 128:(gi + 1) * 128, :], in_=x1t.t[:])
                            rms_rows([x1t], x1t.t[:], st_)
                            S.op("dve", lambda e: e.scalar_tensor_tensor(out=nb_tmp.t[:], in0=x1t.t[:], scalar=st_.t[:, 2:3], in1=gm2_b.t[:], op0=ALU.mult, op1=ALU.mult), [x1t, st_, gm2_b], [nb_tmp])
                            S.op("dve", lambda e: e.tensor_tensor(out=hx2b.t[:], in0=nb_tmp.t[:], in1=sh2_b.t[:], op=ALU.add), [nb_tmp, sh2_b], [hx2b])
                            for k in range(8):
                                S.op("pe", lambda e: e.transpose(out=p.t[:, k, :], in_=hx2b.t[:, k * 128:(k + 1) * 128], identity=ident.t[:]), [hx2b, ident], [p])
                            S.op("act", lambda e: e.activation(out=hx2T.t[:], in_=p.t[:], func=AF.Copy), [p], [hx2T])
                            if gi == 0:
                                dump("x1t0", x1t, x1t.t[:], [128, D]); dump("hx2b0", hx2b, hx2b.t[:], [128, D], BF16)

                        def wo_s2(i):
                            gi = b * 16 + i
                            par = i % 2
                            xt = nb_xt[par]; parts = nb_parts[par]; hx2b = nb_xn[par]; st_ = nb_st[par]; p = pT[par]
                            hx2T = hx2T_s[par]; x1t = x1t_s[par]
                            pl = pbk[2]
                            for k in range(8):
                                S.op("pe", lambda e: e.matmul(pl.t[:, 0:NE], lhsT=hx2T.t[:, k, :], rhs=rw_s.t[:, k, :], start=(k == 0), stop=(k == 7)), [hx2T, rw_s], [pl])
                            S.op("act", lambda e: e.activation(out=r_sc.t[:], in_=pl.t[:, 0:NE], func=AF.Sigmoid), [pl], [r_sc])
                            S.op("dve", lambda e: e.tensor_tensor(out=r_bi.t[:], in0=r_sc.t[:], in1=rb_b.t[:], op=ALU.add), [r_sc, rb_b], [r_bi])
                            for g in range(8):
                                S.op("dve", lambda e: e.max(out=r_m8.t[:, g, :], in_=r_bi.t[:, g * 32:(g + 1) * 32]), [r_bi], [r_m8], cols=[r_m8])
                            S.op("dve", lambda e: e.tensor_tensor(out=r_sm.t[:, 0:8], in0=r_m8.t[:, :, 0], in1=r_m8.t[:, :, 1], op=ALU.add), [r_m8], [r_sm])
                            S.op("dve", lambda e: e.max(out=r_sm.t[:, 8:16], in_=r_sm.t[:, 0:8]), [r_sm], [r_sm])
                            S.op("dve", lambda e: e.tensor_scalar(out=r_sm.t[:, 16:24], in0=r_sm.t[:, 0:8], scalar1=r_sm.t[:, 11:12], scalar2=None, op0=ALU.is_ge), [r_sm], [r_sm])
                            for g in range(8):
                                S.op("dve", lambda e: e.tensor_scalar(out=r_mk.t[:, g * 32:(g + 1) * 32], in0=r_bi.t[:, g * 32:(g + 1) * 32], scalar1=8.0, scalar2=r_sm.t[:, 16 + g:17 + g], op0=ALU.add, op1=ALU.mult), [r_bi, r_sm], [r_mk], cols=[r_mk])
                            S.op("dve", lambda e: e.max(out=r_sm.t[:, 24:32], in_=r_mk.t[:]), [r_mk], [r_sm])
                            S.op("dve", lambda e: e.max_index(out=r_ei.t[:], in_max=r_sm.t[:, 24:32], in_values=r_mk.t[:]), [r_sm, r_mk], [r_ei])
                            S.op("dve", lambda e: e.tensor_copy(out=r_sm.t[:, 32:40], in_=r_ei.t[:]), [r_ei], [r_sm])
                            S.op("dve", lambda e: e.tensor_scalar(out=r_sel.t[:], in0=r_mk.t[:], scalar1=r_sm.t[:, 31:32], scalar2=None, op0=ALU.is_ge), [r_mk, r_sm], [r_sel])
                            pq_ = pbk[3]
                            S.op("pe", lambda e: e.matmul(pq_.t[:, 0:NE], lhsT=Lst.t[:], rhs=r_sel.t[:], start=True, stop=True), [Lst, r_sel], [pq_])
                            S.op("pe", lambda e: e.matmul(pq_.t[:, NE:2 * NE], lhsT=ones.t[:], rhs=r_sel.t[:], start=True, stop=True), [ones, r_sel], [pq_])
                            S.op("dve", lambda e: e.tensor_tensor(out=r_pos.t[:], in0=pq_.t[:, 0:NE], in1=carry.t[:], op=ALU.add), [pq_, carry], [r_pos])
                            S.op("dve", lambda e: e.tensor_tensor(out=carry.t[:], in0=pq_.t[:, NE:2 * NE], in1=carry.t[:], op=ALU.add), [pq_, carry], [carry])
                            S.op("pool", lambda e: e.memset(r_sm.t[:, 40:64], 0.0), [r_sm], [r_sm])
                            for k in range(8):
                                S.op("dve", lambda e: e.scalar_tensor_tensor(out=r_t1.t[:], in0=iota_f.t[:], scalar=r_sm.t[:, 32 + k:33 + k], in1=r_pos.t[:], op0=ALU.is_equal, op1=ALU.mult, accum_out=r_sm.t[:, 40 + k:41 + k]), [iota_f, r_sm, r_pos], [r_sm], cols=[r_sm])
                                S.op("dve", lambda e: e.scalar_tensor_tensor(out=r_t1.t[:], in0=iota_f.t[:], scalar=r_sm.t[:, 32 + k:33 + k], in1=r_sc.t[:], op0=ALU.is_equal, op1=ALU.mult, accum_out=r_sm.t[:, 48 + k:49 + k]), [iota_f, r_sm, r_sc], [r_sm], cols=[r_sm])
                            S.op("dve", lambda e: e.reduce_sum(out=r_sm.t[:, 56:57], in_=r_sm.t[:, 48:56], axis=AX.X), [r_sm], [r_sm])
                            S.op("dve", lambda e: e.reciprocal(out=r_sm.t[:, 57:58], in_=r_sm.t[:, 56:57]), [r_sm], [r_sm])
                            S.op("dve", lambda e: e.tensor_scalar(out=gates_all.t[:, gi, :], in0=r_sm.t[:, 48:56], scalar1=r_sm.t[:, 57:58], scalar2=2.5, op0=ALU.mult, op1=ALU.mult), [r_sm], [gates_all])
                            S.op("pool", lambda e: e.tensor_copy(out=eidx_all.t[:, gi * 8:gi * 8 + 8], in_=r_sm.t[:, 32:40]), [r_sm], [eidx_all])
                            S.op("pool", lambda e: e.tensor_copy(out=pos_all.t[:, gi * 8:gi * 8 + 8], in_=r_sm.t[:, 40:48]), [r_sm], [pos_all])
                            S.dma("sp", hx2d, hx2b, out=hx2d.t.ap()[gi * 128:(gi + 1) * 128, :], in_=hx2b.t[:])
                            pu = pbc
                            for c in range(4):
                                for k in range(8):
                                    S.op("pe", lambda e: e.matmul(pu.t[:, c * 128:(c + 1) * 128], lhsT=sw13_s.t[:, k, c * 128:(c + 1) * 128], rhs=hx2T.t[:, k, :], start=(k == 0), stop=(k == 7)), [sw13_s, hx2T], [pu])
                            S.op("act", lambda e: e.activation(out=hsil.t[:], in_=pu.t[:, 0:256], func=AF.Silu), [pu], [hsil])
                            S.op("dve", lambda e: e.tensor_tensor(out=hTs.t[:].rearrange("p c t -> p (c t)"), in0=hsil.t[:], in1=pu.t[:, 256:512], op=ALU.mult), [hsil, pu], [hTs])
                            for half in range(2):
                                pp = pw8 if half == 0 else pbk[2]
                                for c2 in range(2):
                                    S.op("pe", lambda e: e.matmul(pp.t[:], lhsT=hTs.t[:, c2, :], rhs=sw2_s.t[:, c2, half * 512:(half + 1) * 512], start=(c2 == 0), stop=(c2 == 1)), [hTs, sw2_s], [pp])
                                S.op("act", lambda e: e.activation(out=ysh_t.t[:, half * 512:(half + 1) * 512], in_=pp.t[:], func=AF.Copy), [pp], [ysh_t])
                            S.dma("sp", yshd, ysh_t, out=yshd.t.ap()[gi * 128:(gi + 1) * 128, :], in_=ysh_t.t[:])
                            if gi == 0:
                                dump("ysh0", ysh_t, ysh_t.t[:], [128, D])
                        wo_s1(0)
                        for i in range(16):
                            if i + 1 < 16:
                                wo_s1(i + 1)
                            wo_s2(i)
        dump("eidx_all", eidx_all, eidx_all.t[:], [128, 256]); dump("pos_all", pos_all, pos_all.t[:], [128, 256]); dump("gates_all", gates_all, gates_all.t[:], [128, 32, 8])
        dump("carry", carry, carry.t[:], [128, NE])
        if stop_after == "WO":
            S.finish()
            return nc, dbg_outs
        NBLK = 512
        slots_all = S.sb("slots_all", [128, 256], I32)
        E_all = S.sb("E_all", [128, NBLK], F32)
        with S.scope() as sP:
            padded = S.sb("padded", [128, NE], F32, sP); pad_i = S.sb("pad_i", [128, NE], I32, sP)
            pends = S.sb("pends", [128, NE], F32, sP); pstart = S.sb("pstart", [128, NE], F32, sP)
            ones256 = S.sb("ones256", [128, NE], F32, sP); junk256 = S.sb("junk256", [128, NE], F32, sP)
            ps8 = S.sb("ps8", [128, 8], F32, sP)
            hxr = [S.sb("hxr%d" % i, [128, D], BF16, sP) for i in range(2)]
            S.op("pool", lambda e: e.memset(ones256.t[:], 1.0), [], [ones256])
            S.op("dve", lambda e: e.tensor_scalar(out=padded.t[:], in0=carry.t[:], scalar1=127.0, scalar2=None, op0=ALU.add), [carry], [padded])
            S.op("dve", lambda e: e.tensor_copy(out=pad_i.t[:], in_=padded.t[:]), [padded], [pad_i])
            S.op("dve", lambda e: e.tensor_scalar(out=pad_i.t[:], in0=pad_i.t[:], scalar1=7, scalar2=7, op0=ALU.arith_shift_right, op1=ALU.logical_shift_left), [pad_i], [pad_i])
            S.op("dve", lambda e: e.tensor_copy(out=padded.t[:], in_=pad_i.t[:]), [pad_i], [padded])
            S.op("dve", lambda e: e.tensor_tensor_scan(out=pends.t[:], data0=ones256.t[:], data1=padded.t[:], initial=0.0, op0=ALU.mult, op1=ALU.add), [ones256, padded], [pends])
            S.op("dve", lambda e: e.tensor_tensor(out=pstart.t[:], in0=pends.t[:], in1=padded.t[:], op=ALU.subtract), [pends, padded], [pstart])
            dump("pstart", pstart, pstart.t[:], [128, NE])
            slots_tb = [TB(slots_all.t, "slots_g%d" % g_) for g_ in range(32)]
            xs_parts = [TB(xs_pad.t, "xs_part%d" % k_) for k_ in range(8)]
            hxr.append(S.sb("hxr2", [128, D], BF16, sP))
            ps8s = [ps8, S.sb("ps8b", [128, 8], F32, sP)]
            for gi in range(32):
                hx = hxr[gi % 3]; ps8 = ps8s[gi % 2]
                S.dma("sp", hx, hx2d, out=hx.t[:], in_=hx2d.t.ap()[gi * 128:(gi + 1) * 128, :])
                S.op("pool", lambda e: e.memset(ps8.t[:], 0.0), [], [ps8])
                for k in range(8):
                    S.op("dve", lambda e: e.scalar_tensor_tensor(out=junk256.t[:], in0=iota_f.t[:], scalar=eidx_all.t[:, gi * 8 + k:gi * 8 + k + 1], in1=pstart.t[:], op0=ALU.is_equal, op1=ALU.mult, accum_out=ps8.t[:, k:k + 1]), [iota_f, eidx_all, pstart], [ps8], cols=[ps8])
                S.op("dve", lambda e: e.tensor_tensor(out=ps8.t[:], in0=ps8.t[:], in1=pos_all.t[:, gi * 8:gi * 8 + 8], op=ALU.add), [ps8, pos_all], [ps8])
                S.op("dve", lambda e: e.tensor_copy(out=slots_all.t[:, gi * 8:gi * 8 + 8], in_=ps8.t[:]), [ps8], [slots_tb[gi]])
                for k in range(8):
                    S.dma("pool", xs_parts[k], hx, out=xs_pad.t.ap(), in_=hx.t[:], extra_reads=[slots_tb[gi]],
                          indirect=dict(out_offset=bass.IndirectOffsetOnAxis(ap=slots_all.t[:, gi * 8 + k:gi * 8 + k + 1], axis=0), in_offset=None, bounds_check=NSLOT - 1, oob_is_err=False))
            S.op("pool", lambda e: e.memset(E_all.t[:], 0.0), [], [E_all])
            for j in range(NBLK):
                S.op("dve", lambda e: e.tensor_scalar(out=junk256.t[:], in0=pends.t[:], scalar1=float(128 * j), scalar2=0.0, op0=ALU.is_le, op1=ALU.add, accum_out=E_all.t[:, j:j + 1]), [pends], [E_all], cols=[E_all])
            dump("E_all", E_all, E_all.t[:], [128, NBLK])
        S.barrier()
        dump("slots_all", slots_all, slots_all.t[:], [128, 256], I32)
        if stop_after == "SC":
            S.finish()
            return nc, dbg_outs

        with S.scope() as sE:
            pidx_i = S.sb("pidx_i", [128, 1], I32, sE); pidx = S.sb("pidx", [128, 1], F32, sE)
            S.op("pool", lambda e: e.iota(pidx_i.t[:], pattern=[[0, 1]], base=0, channel_multiplier=1), [], [pidx_i])
            S.op("dve", lambda e: e.tensor_copy(out=pidx.t[:], in_=pidx_i.t[:]), [pidx_i], [pidx])
            NBUF = 4
            w13f = [S.sb("w13f%d" % i, [128, 8, 512], F32, sE) for i in range(NBUF)]
            w2f = [S.sb("w2f%d" % i, [128, 2, D], F32, sE) for i in range(NBUF)]
            w13b = [S.sb("w13b%d" % i, [128, 8, 512], BF16, sE) for i in range(2)]
            w2b = [S.sb("w2b%d" % i, [128, 2, D], BF16, sE) for i in range(2)]
            wix_f = [S.sb("wixf%d" % i, [128, 1], F32, sE) for i in range(NBUF)]
            wix = [S.sb("wix%d" % i, [128, 1], I32, sE) for i in range(NBUF)]
            xr = [S.sb("e_xr%d" % i, [128, D], BF16, sE) for i in range(2)]
            xTe = [S.sb("e_xT%d" % i, [128, 8, 128], BF16, sE) for i in range(2)]
            usil = S.sb("e_usil", [128, 256], F32, sE); hb = S.sb("e_hb", [128, 256], BF16, sE)
            hTe = S.sb("e_hT", [128, 2, 128], BF16, sE)
            yo = [S.sb("e_yo%d" % i, [128, D], F32, sE) for i in range(2)]
            ptx = [S.ps("e_ptx%d" % i, [128, 8, 128], BF16, sE) for i in range(2)]
            pu_ = [S.ps("e_pu%d" % i, [128, 512], F32, sE) for i in range(2)]
            py2 = [[S.ps("e_py%d_%d" % (i, h), [128, 512], F32, sE) for h in range(2)] for i in range(2)]
            nblk_run = int(os.environ.get("MOE_NBLK", NBLK))
            WB = ne_decl * 128 - 1

            def load_w(j):
                i = j % NBUF
                if os.environ.get("MOE_NOLOAD"):
                    return
                S.op("dve", lambda e: e.tensor_scalar(out=wix_f[i].t[:], in0=E_all.t[:, j:j + 1], scalar1=128.0, scalar2=pidx.t[:, 0:1], op0=ALU.mult, op1=ALU.add), [E_all, pidx], [wix_f[i]])
                S.op("dve", lambda e: e.tensor_copy(out=wix[i].t[:], in_=wix_f[i].t[:]), [wix_f[i]], [wix[i]])
                S.dma("pool", w13f[i], None, out=w13f[i].t[:].rearrange("p k n -> p (k n)"), in_=w13, extra_reads=[wix[i]],
                      indirect=dict(out_offset=None, in_offset=bass.IndirectOffsetOnAxis(ap=wix[i].t[:, 0:1], axis=0), bounds_check=WB, oob_is_err=False))
                S.dma("pool", w2f[i], None, out=w2f[i].t[:].rearrange("p k n -> p (k n)"), in_=w2, extra_reads=[wix[i]],
                      indirect=dict(out_offset=None, in_offset=bass.IndirectOffsetOnAxis(ap=wix[i].t[:, 0:1], axis=0), bounds_check=WB, oob_is_err=False))

            def cast_w(j):
                i = j % NBUF; o = j % 2
                if os.environ.get("MOE_NOCAST"):
                    return
                S.op("act", lambda e: e.activation(out=w13b[o].t[:, 0:3, :], in_=w13f[i].t[:, 0:3, :], func=AF.Copy), [w13f[i]], [w13b[o]])
                S.op("dve", lambda e: e.tensor_scalar(out=w13b[o].t[:, 3:8, :], in0=w13f[i].t[:, 3:8, :], scalar1=1.0, scalar2=None, op0=ALU.mult), [w13f[i]], [w13b[o]])
                S.op("act", lambda e: e.activation(out=w2b[o].t[:, 0, :], in_=w2f[i].t[:, 0, :], func=AF.Copy), [w2f[i]], [w2b[o]])
                S.op("dve", lambda e: e.tensor_scalar(out=w2b[o].t[:, 1, :], in0=w2f[i].t[:, 1, :], scalar1=1.0, scalar2=None, op0=ALU.mult), [w2f[i]], [w2b[o]])

            xr3 = xr + [S.sb("e_xr2", [128, D], BF16, sE)]
            usil2 = [usil, S.sb("e_usil1", [128, 256], F32, sE)]
            hb2 = [hb, S.sb("e_hb1", [128, 256], BF16, sE)]
            hTe2 = [hTe, S.sb("e_hT1", [128, 2, 128], BF16, sE)]

            def s_load_x(j):
                S.dma("sp", xr3[j % 3], xs_pad, out=xr3[j % 3].t[:], in_=xs_pad.t.ap()[j * 128:(j + 1) * 128, :])

            def s_Tx(j):
                x_ = xr3[j % 3]; p_ = ptx[j % 2]
                for k in range(8):
                    S.op("pe", lambda e: e.transpose(out=p_.t[:, k, :], in_=x_.t[:, k * 128:(k + 1) * 128], identity=ident.t[:]), [x_, ident], [p_])
                S.op("act", lambda e: e.activation(out=xTe[j % 2].t[:], in_=p_.t[:], func=AF.Copy), [p_], [xTe[j % 2]])

            def s_up(j):
                o = j % 2
                for k in range(8):
                    S.op("pe", lambda e: e.matmul(pu_[o].t[:], lhsT=xTe[o].t[:, k, :], rhs=w13b[o].t[:, k, :], start=(k == 0), stop=(k == 7)), [xTe[o], w13b[o]], [pu_[o]])
                S.op("act", lambda e: e.activation(out=usil2[o].t[:], in_=pu_[o].t[:, 0:256], func=AF.Silu), [pu_[o]], [usil2[o]])
                S.op("dve", lambda e: e.tensor_tensor(out=hb2[o].t[:], in0=usil2[o].t[:], in1=pu_[o].t[:, 256:512], op=ALU.mult), [usil2[o], pu_[o]], [hb2[o]])

            def s_Th(j):
                o = j % 2; p_ = ptx[o]
                for c2 in range(2):
                    S.op("pe", lambda e: e.transpose(out=p_.t[:, c2, :], in_=hb2[o].t[:, c2 * 128:(c2 + 1) * 128], identity=ident.t[:]), [hb2[o], ident], [p_])
                S.op("act", lambda e: e.activation(out=hTe2[o].t[:], in_=p_.t[:, 0:2, :], func=AF.Copy), [p_], [hTe2[o]])

            def s_down(j):
                o = j % 2
                for half in range(2):
                    pp = py2[o][half]
                    for c2 in range(2):
                        S.op("pe", lambda e: e.matmul(pp.t[:], lhsT=hTe2[o].t[:, c2, :], rhs=w2b[o].t[:, c2, half * 512:(half + 1) * 512], start=(c2 == 0), stop=(c2 == 1)), [hTe2[o], w2b[o]], [pp])
                    if half == 0:
                        S.op("act", lambda e: e.activation(out=yo[o].t[:, 0:512], in_=pp.t[:], func=AF.Copy), [pp], [yo[o]])
                    else:
                        S.op("dve", lambda e: e.tensor_copy(out=yo[o].t[:, 512:1024], in_=pp.t[:]), [pp], [yo[o]])
                S.dma("sp", ys_pad, yo[o], out=ys_pad.t.ap()[j * 128:(j + 1) * 128, :], in_=yo[o].t[:])

            n_ = nblk_run
            load_w(0)
            if n_ > 1:
                load_w(1)
            if n_ > 2:
                load_w(2)
            s_load_x(0)
            if n_ > 1:
                s_load_x(1)
            cast_w(0); s_Tx(0); s_up(0)
            for j in range(n_):
                if j + 3 < n_:
                    load_w(j + 3)
                if j + 2 < n_:
                    s_load_x(j + 2)
                if j + 1 < n_:
                    cast_w(j + 1); s_Tx(j + 1)
                s_Th(j)
                if j + 1 < n_:
                    s_up(j + 1)
                s_down(j)
        if stop_after == "EXP":
            S.finish()
            return nc, dbg_outs

        with S.scope() as sF:
            g2_b = [S.sb("g2_b%d" % b, [128, D], F32, sF) for b in range(NB)]
            pbf = S.ps("pbf", [128, 512], F32, sF)
            for b in range(NB):
                row_bcast(g2_b[b], lambda k: modc.t[:, 40 + k, b:b + 1], modc, pbf)
            accs = [S.sb("f_acc%d" % i, [128, D], F32, sF) for i in range(2)]
            gat = [S.sb("f_gat%d" % i, [128, D], F32, sF) for i in range(12)]
            x1rs = [S.sb("f_x1r%d" % i, [128, D], F32, sF) for i in range(2)]; fo = [S.sb("f_o%d" % i, [128, D], F32, sF) for i in range(2)]
            fst = S.sb("f_st", [128, 4], F32, sF); fjunk = S.sb("f_junk", [128, D], BF16, sF)
            for gi in range(32):
                b = gi // 16
                acc = accs[gi % 2]; x1r = x1rs[gi % 2]
                S.dma("sp", acc, yshd, out=acc.t[:], in_=yshd.t.ap()[gi * 128:(gi + 1) * 128, :])
                S.dma("sp", x1r, x1d, out=x1r.t[:], in_=x1d.t.ap()[gi * 128:(gi + 1) * 128, :])
                for k in range(8):
                    g = gat[(gi * 8 + k) % 12]
                    S.dma("pool", g, ys_pad, out=g.t[:], in_=ys_pad.t.ap(), extra_reads=[slots_all],
                          indirect=dict(out_offset=None, in_offset=bass.IndirectOffsetOnAxis(ap=slots_all.t[:, gi * 8 + k:gi * 8 + k + 1], axis=0)))
                    S.op("dve", lambda e: e.scalar_tensor_tensor(out=acc.t[:], in0=g.t[:], scalar=gates_all.t[:, gi, k:k + 1], in1=acc.t[:], op0=ALU.mult, op1=ALU.add), [g, gates_all, acc], [acc])
                S.op("pool", lambda e: e.tensor_tensor(out=acc.t[:], in0=acc.t[:], in1=g2_b[b].t[:], op=ALU.mult), [acc, g2_b[b]], [acc])
                S.op("pool", lambda e: e.tensor_tensor(out=acc.t[:], in0=acc.t[:], in1=x1r.t[:], op=ALU.add), [acc, x1r], [acc])
                S.op("pool", lambda e: e.memset(fst.t[:, 0:1], 0.0), [], [fst])
                S.op("act", lambda e: e.activation(out=fjunk.t[:], in_=acc.t[:], func=AF.Square, accum_out=fst.t[:, 0:1]), [acc], [fst])
                S.op("dve", lambda e: e.tensor_scalar(out=fst.t[:, 1:2], in0=fst.t[:, 0:1], scalar1=1.0 / D, scalar2=EPS, op0=ALU.mult, op1=ALU.add), [fst], [fst])
                S.op("act", lambda e: e.activation(out=fst.t[:, 1:2], in_=fst.t[:, 1:2], func=AF.Sqrt), [fst], [fst])
                S.op("dve", lambda e: e.reciprocal(out=fst.t[:, 2:3], in_=fst.t[:, 1:2]), [fst], [fst])
                o_ = fo[gi % 2]
                S.op("dve", lambda e: e.scalar_tensor_tensor(out=o_.t[:], in0=acc.t[:], scalar=fst.t[:, 2:3], in1=nfin_b.t[:], op0=ALU.mult, op1=ALU.mult), [acc, fst, nfin_b], [o_])
                S.dma("sp", None, o_, out=out[b, (gi % 16) * 128:(gi % 16 + 1) * 128, :], in_=o_.t[:], final=True)
        S.finish()
    return nc, dbg_outs


def prep_inputs(inp, cores=range(8), ne_decl=256):
    f = lambda a: np.ascontiguousarray(np.asarray(a, dtype=np.float32))
    col8 = lambda v: f(np.asarray(v).reshape(8, 128).T)
    ada_bc = f(np.asarray(inp["ada_b"])[0].reshape(48, 128).T)
    conv_w = np.asarray(inp["lru_conv_w"])[0]
    convw = f(conv_w.T.reshape(4, 128, 4).transpose(1, 0, 2).reshape(128, 16))
    convb = f(np.asarray(inp["lru_conv_b"])[0].reshape(4, 128).T)

    def blockdiag(w):
        w = np.asarray(w)[0]
        o = np.zeros((128, 8, 128), np.float32)
        for d in range(2):
            for c in range(4):
                for hh in range(2):
                    o[hh * 64:(hh + 1) * 64, d * 4 + c, hh * 64:(hh + 1) * 64] = w[d, 2 * c + hh]
        return o

    def col_dc(v):
        return f(np.asarray(v)[0].reshape(2, 4, 128).transpose(2, 0, 1).reshape(128, 8))

    shared = {
        "ada_w": f(np.asarray(inp["ada_w"])[0]), "ada_bc": ada_bc,
        "nmix": col8(np.asarray(inp["norm_mix"])[0]), "nffn": col8(np.asarray(inp["norm_ffn"])[0]),
        "nfin": f(np.asarray(inp["norm_final"]).reshape(1, 1024)),
        "w_in": f(np.asarray(inp["w_in"])[0]), "w_out": f(np.asarray(inp["w_out"])[0]),
        "convw": convw, "convb": convb,
        "wa_bd": blockdiag(inp["lru_wa"]), "wx_bd": blockdiag(inp["lru_wx"]),
        "ba_c": col_dc(inp["lru_ba"]), "bx_c": col_dc(inp["lru_bx"]), "lam_c": col_dc(inp["lru_lambda"]),
        "lbl": f(np.asarray(inp["hgrn_lb_logits"]).reshape(1, 2048)),
        "hnorm": f(np.asarray(inp["hgrn_norm"]).reshape(1, 512)),
        "router_w": f(np.asarray(inp["router_w"])[0]), "router_b": f(np.asarray(inp["router_b"]).reshape(1, 256)),
        "w13": f(np.asarray(inp["exp_w13"])[0][:ne_decl].reshape(ne_decl, 8, 128, 512).transpose(0, 2, 1, 3).reshape(ne_decl * 128, 4096)),
        "w2": f(np.asarray(inp["exp_w2"])[0][:ne_decl].reshape(ne_decl, 2, 128, 1024).transpose(0, 2, 1, 3).reshape(ne_decl * 128, 2048)),
        "sw13": f(np.asarray(inp["shared_w13"])[0]), "sw2": f(np.asarray(inp["shared_w2"])[0]),
    }
    x = np.asarray(inp["x"]); c = np.asarray(inp["c"]); ctx = np.asarray(inp["ctx"]); c_ctx = np.asarray(inp["c_ctx"])
    maps = []
    for i in cores:
        m = dict(shared)
        m["x2"] = f(x[2 * i:2 * i + 2]); m["ctx2"] = f(ctx[2 * i:2 * i + 2])
        m["cT"] = f(np.stack([c[2 * i], c[2 * i + 1], c_ctx], axis=1))
        maps.append(m)
    return maps


_NC_CACHE = {}


def kernel(**inputs):
    if "nc" not in _NC_CACHE:
        _NC_CACHE["nc"] = build_nc()[0]
    nc = _NC_CACHE["nc"]
    maps = prep_inputs(inputs)
    res = run_bass_kernel_spmd(nc, maps, core_ids=list(range(8)))
    return np.concatenate([r["out"] for r in res.results], axis=0).astype(np.float32)
```

```python
import os
import numpy as np
from contextlib import ExitStack
import concourse.bass as bass
import concourse.mybir as mybir
from concourse.bass_utils import run_bass_kernel_spmd

F32 = mybir.dt.float32
BF16 = mybir.dt.bfloat16
I32 = mybir.dt.int32
U32 = mybir.dt.uint32
AF = mybir.ActivationFunctionType
ALU = mybir.AluOpType
AX = mybir.AxisListType

N_DMA_SEMS = 16


class TB:
    def __init__(self, t, name):
        self.t = t
        self.name = name
        self.w = None
        self.r = {}

    def sub(self, name=None):
        return TB(self.t, name or self.name + "_s")


class _Scope:
    def __init__(self, S):
        self.S = S
        self.st = ExitStack()

    def __enter__(self):
        self.st.__enter__()
        return self.st

    def __exit__(self, *a):
        r = self.st.__exit__(*a)
        if a[0] is None:
            self.S.barrier()
        return r


class Sched:
    def __init__(self, nc, st):
        self.nc = nc
        self.st = st
        self.eng = {"pe": nc.tensor, "act": nc.scalar, "dve": nc.vector, "pool": nc.gpsimd, "sp": nc.sync}
        self.sem = {}
        for k in self.eng:
            self.sem[k] = st.enter_context(nc.semaphore("s_" + k))
        self.dq = {"sp": 0, "pool": 1, "act": 2}
        for i in range(3 * N_DMA_SEMS):
            self.sem[("d", i)] = st.enter_context(nc.semaphore("d%d" % i))
        self.tick = {k: 0 for k in self.eng}
        self.seen = {k: {} for k in self.eng}
        self.dval = [0] * (3 * N_DMA_SEMS)
        self.dnext = [0, 0, 0]
        self.finals = []
        self.bregs = {}
        self.ninstr = 0

    def sb(self, name, shape, dtype, st=None):
        t = (st or self.st).enter_context(self.nc.sbuf_tensor(name, list(shape), dtype))
        return TB(t, name)

    def ps(self, name, shape, dtype, st=None):
        t = (st or self.st).enter_context(self.nc.psum_tensor(name, list(shape), dtype))
        return TB(t, name)

    def dram(self, name, shape, dtype):
        t = self.nc.dram_tensor(name, list(shape), dtype, kind="Internal")
        return TB(t, name)

    def _wait(self, E, tok):
        if tok is None:
            return
        key, val = tok
        if key == E and E == "pe":
            return
        if self.seen[E].get(key, 0) >= val:
            return
        self.eng[E].wait_ge(self.sem[key], val)
        self.seen[E][key] = val

    def _deps(self, E, reads, writes, cols=()):
        for b in reads:
            self._wait(E, b.w)
        for b in writes:
            same_ok = any(b is c for c in cols)
            if not (same_ok and b.w is not None and b.w[0] == E):
                self._wait(E, b.w)
            for k, v in list(b.r.items()):
                if same_ok and k == E:
                    continue
                self._wait(E, (k, v))

    def _commit(self, tok, reads, writes):
        key, val = tok
        for b in reads:
            if b.r.get(key, 0) < val:
                b.r[key] = val
        for b in writes:
            b.w = tok
            b.r = {}

    def op(self, E, fn, reads=(), writes=(), cols=()):
        reads = [b for b in reads if isinstance(b, TB)]
        writes = [b for b in writes if isinstance(b, TB)]
        self._deps(E, reads, writes, cols)
        ins = fn(self.eng[E])
        self.tick[E] += 1
        ins.then_inc(self.sem[E], 1)
        self._commit((E, self.tick[E]), reads, writes)
        self.ninstr += 1
        return ins

    def dma(self, Q, dst, src, out, in_, final=False, indirect=None, extra_reads=(), **kw):
        reads = [b for b in [src] + list(extra_reads) if isinstance(b, TB)]
        writes = [b for b in [dst] if isinstance(b, TB)]
        self._deps(Q, reads, writes)
        qi = self.dq[Q]
        s = qi * N_DMA_SEMS + self.dnext[qi]
        self.dnext[qi] = (self.dnext[qi] + 1) % N_DMA_SEMS
        key = ("d", s)
        if self.dval[s] > 0:
            self._wait(Q, (key, self.dval[s]))
        if indirect is not None:
            indirect = dict(indirect)
            bc = indirect.get("bounds_check")
            if isinstance(bc, int):
                if bc not in self.bregs:
                    r = self.eng[Q].alloc_register("bc_%d" % bc)
                    self.eng[Q].reg_mov(r, bc)
                    self.bregs[bc] = r
                indirect["bounds_check"] = self.bregs[bc]
            ins = self.eng[Q].indirect_dma_start(out=out, in_=in_, **indirect, **kw)
        else:
            ins = self.eng[Q].dma_start(out=out, in_=in_, **kw)
        self.dval[s] += 16
        ins.then_inc(self.sem[key], 16)
        tok = (key, self.dval[s])
        self._commit(tok, reads, writes)
        if final:
            self.finals.append(tok)
        self.ninstr += 1
        return ins

    def barrier(self):
        for E in self.eng:
            for F in ("pe", "act", "dve", "pool"):
                if F != E and self.tick[F] > 0:
                    self._wait(E, (F, self.tick[F]))
            for i in range(3 * N_DMA_SEMS):
                if self.dval[i] > 0:
                    self._wait(E, (("d", i), self.dval[i]))

    def scope(self):
        return _Scope(self)

    def finish(self):
        for tok in self.finals:
            self._wait("sp", tok)
        for k in ("pe", "act", "dve", "pool"):
            if self.tick[k] > 0:
                self._wait("sp", (k, self.tick[k]))

    def make_ident(self, ident):
        self.op("pool", lambda e: e.memset(ident.t[:], 0.0), [], [ident])
        n = ident.t.shape[0]
        self.op("pool", lambda e: e.affine_select(out=ident.t[:], in_=ident.t[:], compare_op=ALU.not_equal,
                                                  fill=1.0, base=0, pattern=[[-1, n]], channel_multiplier=1),
                [ident], [ident])


D = 1024
T = 2048
TC = 256
NTOK = TC + T
NB = 2
NE = 256
CAP = 256
NSLOT = 65536
EPS = 1e-6


def build_nc(dbg=(), stop_after=None, ne_decl=NE):
    nc = bass.Bass("TRN2", target_bir_lowering=False)

    def din(name, shape, dt=F32):
        return nc.dram_tensor(name, list(shape), dt, kind="ExternalInput").ap()

    x2 = din("x2", [NB, T, D]); ctx2 = din("ctx2", [NB, TC, D]); cT = din("cT", [D, 3])
    ada_w = din("ada_w", [D, 6 * D]); ada_bc = din("ada_bc", [128, 48])
    nmix = din("nmix", [128, 8]); nffn = din("nffn", [128, 8]); nfin = din("nfin", [1, D])
    w_in = din("w_in", [D, 3584]); w_out = din("w_out", [D, D])
    convw = din("convw", [128, 16]); convb = din("convb", [128, 4])
    wa_bd = din("wa_bd", [128, 8, 128]); wx_bd = din("wx_bd", [128, 8, 128])
    ba_c = din("ba_c", [128, 8]); bx_c = din("bx_c", [128, 8]); lam_c = din("lam_c", [128, 8])
    lbl = din("lbl", [1, 2048]); hnorm = din("hnorm", [1, 512])
    router_w = din("router_w", [D, NE]); router_b = din("router_b", [1, NE])
    w13 = din("w13", [ne_decl * 128, 4096]); w2 = din("w2", [ne_decl * 128, 2048])
    sw13 = din("sw13", [D, 512]); sw2 = din("sw2", [256, D])
    out = nc.dram_tensor("out", [NB, T, D], F32, kind="ExternalOutput").ap()

    dbg_outs = {}

    with ExitStack() as st:
        S = Sched(nc, st)

        def dump(name, tb, ap, shape, dt=F32):
            if name not in dbg:
                return
            d = nc.dram_tensor("dbg_" + name, list(shape), dt, kind="ExternalOutput").ap()
            S.dma("sp", None, tb, out=d, in_=ap, final=True)
            dbg_outs[name] = (shape, dt)

        x1d = S.dram("x1d", [NB * T, D], F32)
        vsave = S.dram("vsave", [T, 512], BF16); qsave = S.dram("qsave", [T, 512], F32)
        vsave_tb = [TB(vsave.t, "vsave%d" % i) for i in range(16)]; qsave_tb = [TB(qsave.t, "qsave%d" % i) for i in range(16)]
        yshd = S.dram("yshd", [NB * T, D], F32)
        xs_pad = S.dram("xs_pad", [NSLOT, D], BF16)
        ys_pad = S.dram("ys_pad", [NSLOT, D], F32)

        ident = S.sb("ident", [128, 128], BF16); S.make_ident(ident)
        identf = S.sb("identf", [128, 128], F32); S.make_ident(identf)
        ones = S.sb("ones", [128, 128], F32)
        S.op("pool", lambda e: e.memset(ones.t[:], 1.0), [], [ones])

        def tri(name, base, cm, step, dt):
            m = S.sb(name, [128, 128], dt)
            S.op("pool", lambda e: e.memset(m.t[:], 1.0), [], [m])
            S.op("pool", lambda e: e.affine_select(out=m.t[:], in_=m.t[:], compare_op=ALU.is_ge, fill=0.0, base=base,
                                                   pattern=[[step, 128]], channel_multiplier=cm), [m], [m])
            v3 = m.t[:].rearrange("p (b i) -> p b i", i=32)
            S.op("pool", lambda e: e.affine_select(out=v3, in_=v3, compare_op=ALU.is_ge, fill=0.0, base=0,
                                                   pattern=[[-32, 4], [0, 32]], channel_multiplier=1), [m], [m])
            S.op("pool", lambda e: e.affine_select(out=v3, in_=v3, compare_op=ALU.is_ge, fill=0.0, base=31,
                                                   pattern=[[32, 4], [0, 32]], channel_multiplier=-1), [m], [m])
            return m

        M_le = tri("M_le", 0, -1, 1, F32)
        M_ge = tri("M_ge", 0, 1, -1, F32)
        M_gt = tri("M_gt", -1, 1, -1, F32)
        M_lt = tri("M_lt", -1, -1, 1, F32)
        blk1 = S.sb("blk1", [128, 4], F32)
        S.op("pool", lambda e: e.memset(blk1.t[:], 1.0), [], [blk1])
        S.op("pool", lambda e: e.affine_select(out=blk1.t[:], in_=blk1.t[:], compare_op=ALU.is_ge, fill=0.0, base=0,
                                               pattern=[[-32, 4]], channel_multiplier=1), [blk1], [blk1])
        S.op("pool", lambda e: e.affine_select(out=blk1.t[:], in_=blk1.t[:], compare_op=ALU.is_ge, fill=0.0, base=31,
                                               pattern=[[32, 4]], channel_multiplier=-1), [blk1], [blk1])
        mask_f = S.sb("mask_f", [128, 4, 128], F32); mask_b = S.sb("mask_b", [128, 4, 128], F32)
        for h in range(4):
            S.op("pool", lambda e: e.tensor_copy(out=mask_f.t[:, h, :], in_=M_le.t[:]), [M_le], [mask_f])
            S.op("pool", lambda e: e.tensor_copy(out=mask_b.t[:, h, :], in_=M_ge.t[:]), [M_ge], [mask_b])

        cmask = S.sb("cmask", [128, 4, 4, 128], BF16)
        S.op("pool", lambda e: e.memset(cmask.t[:], 0.0), [], [cmask])
        for c in range(4):
            S.op("pool", lambda e: e.memset(cmask.t[:, c, :, 32 * c:32 * c + 32], 1.0), [cmask], [cmask])
        def load(name, shape, src, q="sp", dt=F32):
            t = S.sb(name, shape, dt)
            S.dma(q, t, None, out=t.t[:], in_=src)
            return t

        cTs = load("cTs", [128, 8, 3], cT.rearrange("(k p) j -> p k j", p=128))
        adab = load("adab", [128, 48], ada_bc)
        nmix_s = load("nmix_s", [128, 8], nmix); nffn_s = load("nffn_s", [128, 8], nffn)
        nfin_b = load("nfin_b", [128, D], nfin.partition_broadcast(128))
        convw_s = load("convw_s", [128, 16], convw); convb_s = load("convb_s", [128, 4], convb)
        wabd = load("wabd", [128, 8, 128], wa_bd, q="pool", dt=BF16)
        wxbd = load("wxbd", [128, 8, 128], wx_bd, q="pool", dt=BF16)
        ba_s = load("ba_s", [128, 8], ba_c); bx_s = load("bx_s", [128, 8], bx_c); lam_s = load("lam_s", [128, 8], lam_c)
        hn_b = load("hn_b", [128, 512], hnorm.partition_broadcast(128))
        rb_b = load("rb_b", [128, NE], router_b.partition_broadcast(128))

        lbB = S.sb("lbB", [128, 2, 512], F32); omlB = S.sb("omlB", [128, 2, 512], F32)
        with S.scope() as s0:
            lbl_b = S.sb("lbl_b", [128, 2048], F32, s0)
            S.dma("sp", lbl_b, None, out=lbl_b.t[:], in_=lbl.partition_broadcast(128))
            lv = lbl_b.t[:].rearrange("p (d s c) -> p d s c", d=2, s=2)
            for d in range(2):
                S.op("dve", lambda e: e.tensor_tensor(out=lbB.t[:, d, :], in0=lv[:, d, 0, :], in1=lv[:, d, 1, :], op=ALU.subtract), [lbl_b], [lbB])
        S.op("act", lambda e: e.activation(out=lbB.t[:], in_=lbB.t[:], func=AF.Sigmoid), [lbB], [lbB])
        S.op("dve", lambda e: e.tensor_scalar(out=omlB.t[:], in0=lbB.t[:], scalar1=-1.0, scalar2=1.0, op0=ALU.mult, op1=ALU.add), [lbB], [omlB])

        c8 = S.sb("c8", [128, 8], F32); spt = S.sb("spt", [128, 8], F32)
        S.op("dve", lambda e: e.tensor_scalar(out=spt.t[:], in0=lam_s.t[:], scalar1=-1.0, scalar2=None, op0=ALU.mult), [lam_s], [spt])
        S.op("dve", lambda e: e.tensor_tensor(out=spt.t[:], in0=spt.t[:], in1=lam_s.t[:], op=ALU.max), [lam_s, spt], [spt])
        S.op("act", lambda e: e.activation(out=spt.t[:], in_=spt.t[:], func=AF.Exp, scale=-1.0), [spt], [spt])
        S.op("act", lambda e: e.activation(out=spt.t[:], in_=spt.t[:], func=AF.Ln, bias=1.0), [spt], [spt])
        S.op("dve", lambda e: e.tensor_scalar(out=c8.t[:], in0=lam_s.t[:], scalar1=-1.0, scalar2=0.0, op0=ALU.mult, op1=ALU.max), [lam_s], [c8])
        S.op("dve", lambda e: e.tensor_tensor(out=c8.t[:], in0=c8.t[:], in1=spt.t[:], op=ALU.add), [c8, spt], [c8])
        S.op("dve", lambda e: e.tensor_scalar(out=c8.t[:], in0=c8.t[:], scalar1=-8.0, scalar2=None, op0=ALU.mult), [c8], [c8])
        dump("c8", c8, c8.t[:], [128, 8])

        modc = S.sb("modc", [128, 48, 3], F32)
        scT = S.sb("scT", [128, 8, 3], F32)
        S.op("act", lambda e: e.activation(out=scT.t[:], in_=cTs.t[:], func=AF.Silu), [cTs], [scT])
        with S.scope() as sA:
            aw = [S.sb("aw%d" % i, [128, 8, 1024], F32, sA) for i in range(2)]
            pm = S.ps("pm", [128, 512], F32, sA)
            for g in range(6):
                t = aw[g % 2]
                S.dma("sp", t, None, out=t.t[:], in_=ada_w[:, g * 1024:(g + 1) * 1024].rearrange("(k p) n -> p k n", p=128))
                for fc in range(8):
                    f = g * 8 + fc
                    for k in range(8):
                        S.op("pe", lambda e: e.matmul(pm.t[:, f * 3:(f + 1) * 3], lhsT=t.t[:, k, fc * 128:(fc + 1) * 128],
                                                      rhs=scT.t[:, k, :], start=(k == 0), stop=(k == 7)), [t, scT], [pm])
            pmv = pm.t[:, 0:144].rearrange("p (f j) -> p f j", j=3)
            for j in range(3):
                S.op("dve", lambda e: e.tensor_tensor(out=modc.t[:, :, j], in0=pmv[:, :, j], in1=adab.t[:], op=ALU.add), [pm, adab], [modc])
        dump("modc", modc, modc.t[:], [128, 48, 3])
        gm1 = S.sb("gm1", [128, 8, 3], F32); gm2 = S.sb("gm2", [128, 8, 3], F32)
        for j in range(3):
            S.op("dve", lambda e: e.scalar_tensor_tensor(out=gm1.t[:, :, j], in0=modc.t[:, 8:16, j], scalar=1.0, in1=nmix_s.t[:], op0=ALU.add, op1=ALU.mult), [modc, nmix_s], [gm1])
            S.op("dve", lambda e: e.scalar_tensor_tensor(out=gm2.t[:, :, j], in0=modc.t[:, 32:40, j], scalar=1.0, in1=nffn_s.t[:], op0=ALU.add, op1=ALU.mult), [modc, nffn_s], [gm2])

        def row_bcast(dst, col_ap_fn, src_tb, pbank):
            tmp = rb_tmp
            for k in range(8):
                S.op("dve", lambda e: e.tensor_scalar(out=tmp.t[:], in0=ones.t[:], scalar1=col_ap_fn(k), scalar2=None, op0=ALU.mult), [ones, src_tb], [tmp])
                S.op("pe", lambda e: e.transpose(out=pbank.t[:, (k % 4) * 128:(k % 4 + 1) * 128], in_=tmp.t[:], identity=identf.t[:]), [tmp, identf], [pbank])
                if k % 4 == 3:
                    S.op("act", lambda e: e.activation(out=dst.t[:, (k - 3) * 128:(k + 1) * 128], in_=pbank.t[:], func=AF.Copy), [pbank], [dst])

        rb_tmp = S.sb("rb_tmp", [128, 128], F32)

        if stop_after == "A":
            S.finish()
            return nc, dbg_outs
        iota_f = S.sb("iota_f", [128, NE], F32); ebase = None
        with S.scope() as s1:
            iota_i = S.sb("iota_i", [128, NE], I32, s1)
            S.op("pool", lambda e: e.iota(iota_i.t[:], pattern=[[1, NE]], base=0, channel_multiplier=0), [], [iota_i])
            S.op("dve", lambda e: e.tensor_copy(out=iota_f.t[:], in_=iota_i.t[:]), [iota_i], [iota_f])
        Lst = S.sb("Lst", [128, 128], F32)
        S.op("pool", lambda e: e.memset(Lst.t[:], 1.0), [], [Lst])
        S.op("pool", lambda e: e.affine_select(out=Lst.t[:], in_=Lst.t[:], compare_op=ALU.is_ge, fill=0.0, base=-1,
                                               pattern=[[1, 128]], channel_multiplier=-1), [Lst], [Lst])
        carry = S.sb("carry", [128, NE], F32)
        S.op("pool", lambda e: e.memset(carry.t[:], 0.0), [], [carry])
        eidx_all = S.sb("eidx_all", [128, 256], F32); pos_all = S.sb("pos_all", [128, 256], F32)
        hx2d = S.dram("hx2d", [NB * T, D], BF16)
        gates_all = S.sb("gates_all", [128, 32, 8], F32)

        with S.scope() as sPB:
            hxT = S.sb("hxT", [128, 8, NTOK], BF16, sPB)
            yT = S.sb("yT", [128, 8, T], BF16, sPB)
            pT = [S.ps("pT%d" % i, [128, 8, 128], BF16, sPB) for i in range(2)]
            pbk = [S.ps("pbk%d" % i, [128, 512], F32, sPB) for i in range(4)]
            pbc = S.ps("pbc", [128, 512], F32, sPB)

            nb_xt = [S.sb("nb_xt%d" % i, [128, D], F32, sPB) for i in range(2)]
            nb_parts = [[TB(nb_xt[i].t, "nb_xt%d_p%d" % (i, j)) for j in range(4)] for i in range(2)]
            nb_xn = [S.sb("nb_xn%d" % i, [128, D], BF16, sPB) for i in range(2)]
            nb_st = [S.sb("nb_st%d" % i, [128, 4], F32, sPB) for i in range(2)]
            nb_junk = S.sb("nb_junk", [128, D], BF16, sPB)
            nb_tmp = S.sb("nb_tmp", [128, D], F32, sPB)

            def rms_rows(x_tb_list, x_ap, st_, width=D):
                S.op("pool", lambda e: e.memset(st_.t[:, 0:1], 0.0), [], [st_])
                S.op("act", lambda e: e.activation(out=nb_junk.t[:, 0:width], in_=x_ap, func=AF.Square, accum_out=st_.t[:, 0:1]), x_tb_list, [st_])
                S.op("dve", lambda e: e.tensor_scalar(out=st_.t[:, 1:2], in0=st_.t[:, 0:1], scalar1=1.0 / width, scalar2=EPS, op0=ALU.mult, op1=ALU.add), [st_], [st_])
                S.op("act", lambda e: e.activation(out=st_.t[:, 1:2], in_=st_.t[:, 1:2], func=AF.Sqrt), [st_], [st_])
                S.op("dve", lambda e: e.reciprocal(out=st_.t[:, 2:3], in_=st_.t[:, 1:2]), [st_], [st_])

            def norm_T(ti, srcs, gmb, shb, dst_tb, dst_ap):
                xt = nb_xt[ti % 2]; parts = nb_parts[ti % 2]; xn = nb_xn[ti % 2]; st_ = nb_st[ti % 2]; p = pT[ti % 2]
                for j, (psl, ap) in enumerate(srcs):
                    S.dma("sp", parts[j], None, out=xt.t[psl, :], in_=ap)
                rms_rows(parts, xt.t[:], st_)
                S.op("dve", lambda e: e.scalar_tensor_tensor(out=nb_tmp.t[:], in0=xt.t[:], scalar=st_.t[:, 2:3], in1=gmb.t[:], op0=ALU.mult, op1=ALU.mult), parts + [st_, gmb], [nb_tmp])
                S.op("dve", lambda e: e.tensor_tensor(out=xn.t[:], in0=nb_tmp.t[:], in1=shb.t[:], op=ALU.add), [nb_tmp, shb], [xn])
                def stage2():
                    for k in range(8):
                        S.op("pe", lambda e: e.transpose(out=p.t[:, k, :], in_=xn.t[:, k * 128:(k + 1) * 128], identity=ident.t[:]), [xn, ident], [p])
                    S.op("act", lambda e: e.activation(out=dst_ap, in_=p.t[:], func=AF.Copy), [p], [dst_tb], cols=[dst_tb])
                return stage2

            def norm_loop(items):
                pend = None
                for args in items:
                    n2 = norm_T(*args)
                    if pend is not None:
                        pend()
                    pend = n2
                if pend is not None:
                    pend()


            for b in range(NB):
                with S.scope() as sB:
                    with S.scope() as sN1:
                        gm1c_b = S.sb("gm1c_b%d" % b, [128, D], F32, sN1); sh1c_b = S.sb("sh1c_b%d" % b, [128, D], F32, sN1)
                        row_bcast(gm1c_b, lambda k: gm1.t[:, k, 2:3], gm1, pbc)
                        row_bcast(sh1c_b, lambda k: modc.t[:, k, 2:3], modc, pbc)
                        gm1x_b = S.sb("gm1x_b%d" % b, [128, D], F32, sN1); sh1x_b = S.sb("sh1x_b%d" % b, [128, D], F32, sN1)
                        row_bcast(gm1x_b, lambda k: gm1.t[:, k, b:b + 1], gm1, pbc)
                        row_bcast(sh1x_b, lambda k: modc.t[:, k, b:b + 1], modc, pbc)
                        items = [(i, [(slice(0, 128), ctx2[b, i * 128:(i + 1) * 128, :])], gm1c_b, sh1c_b, hxT, hxT.t[:, :, i * 128:(i + 1) * 128]) for i in range(2)]
                        items += [(i, [(slice(0, 128), x2[b, i * 128:(i + 1) * 128, :])], gm1x_b, sh1x_b, hxT, hxT.t[:, :, TC + i * 128:TC + (i + 1) * 128]) for i in range(16)]
                        norm_loop(items)
                    if b == 0:
                        dump("hxT", hxT, hxT.t[:], [128, 8, NTOK], BF16)

                    with S.scope() as sL:
                        wl = S.sb("wl%d" % b, [128, 8, 1024], BF16, sL)
                        S.dma("pool", wl, None, out=wl.t[:], in_=w_in[:, 0:1024].rearrange("(k p) n -> p k n", p=128))
                        rx = S.sb("l_rx%d" % b, [128, NTOK], F32, sL); u = S.sb("l_u%d" % b, [128, NTOK], F32, sL)
                        ubf = S.sb("l_ubf%d" % b, [128, NTOK], BF16, sL); gg = S.sb("l_gg%d" % b, [128, T], BF16, sL)
                        R = S.sb("l_R%d" % b, [128, NTOK], F32, sL); I_ = S.sb("l_I%d" % b, [128, NTOK], F32, sL)
                        H = S.sb("l_H%d" % b, [128, T], F32, sL); Hc = S.sb("l_Hc%d" % b, [128, TC], F32, sL)
                        gt = S.sb("l_gt%d" % b, [128, 512], F32, sL)
                        blocks = [(0, 256)] + [(TC + i * 512, 512) for i in range(4)]
                        for c in range(4):
                            for bi, (t0, n) in enumerate(blocks):
                                pp = pbk[bi % 4]
                                for k in range(8):
                                    S.op("pe", lambda e: e.matmul(pp.t[:, 0:n], lhsT=wl.t[:, k, c * 128:(c + 1) * 128], rhs=hxT.t[:, k, t0:t0 + n], start=(k == 0), stop=(k == 7)), [wl, hxT], [pp])
                                S.op("act", lambda e: e.activation(out=rx.t[:, t0:t0 + n], in_=pp.t[:, 0:n], func=AF.Copy), [pp], [rx])
                            for bi in range(4):
                                t0 = TC + bi * 512; pp = pbk[bi % 4]
                                for k in range(8):
                                    S.op("pe", lambda e: e.matmul(pp.t[:], lhsT=wl.t[:, k, 512 + c * 128:512 + (c + 1) * 128], rhs=hxT.t[:, k, t0:t0 + 512], start=(k == 0), stop=(k == 7)), [wl, hxT], [pp])
                                S.op("act", lambda e: e.activation(out=gt.t[:], in_=pp.t[:], func=AF.Square), [pp], [gt])
                                S.op("dve", lambda e: e.tensor_scalar(out=gt.t[:], in0=gt.t[:], scalar1=0.044715, scalar2=1.0, op0=ALU.mult, op1=ALU.add), [gt], [gt])
                                S.op("dve", lambda e: e.tensor_tensor(out=gt.t[:], in0=gt.t[:], in1=pp.t[:], op=ALU.mult), [gt, pp], [gt])
                                S.op("act", lambda e: e.activation(out=gt.t[:], in_=gt.t[:], func=AF.Sigmoid, scale=1.5957691216), [gt], [gt])
                                S.op("dve", lambda e: e.tensor_tensor(out=gg.t[:, bi * 512:(bi + 1) * 512], in0=gt.t[:], in1=pp.t[:], op=ALU.mult), [gt, pp], [gg])
                            cw = lambda tap: convw_s.t[:, c * 4 + tap:c * 4 + tap + 1]
                            for (s0, n) in ((0, TC), (TC, T)):
                                S.op("dve", lambda e: e.tensor_scalar(out=u.t[:, s0:s0 + n], in0=rx.t[:, s0:s0 + n], scalar1=cw(1), scalar2=convb_s.t[:, c:c + 1], op0=ALU.mult, op1=ALU.add), [rx, convw_s, convb_s], [u])
                                S.op("dve", lambda e: e.scalar_tensor_tensor(out=u.t[:, s0 + 1:s0 + n], in0=rx.t[:, s0:s0 + n - 1], scalar=cw(0), in1=u.t[:, s0 + 1:s0 + n], op0=ALU.mult, op1=ALU.add), [rx, u, convw_s], [u])
                                S.op("dve", lambda e: e.scalar_tensor_tensor(out=u.t[:, s0:s0 + n - 1], in0=rx.t[:, s0 + 1:s0 + n], scalar=cw(2), in1=u.t[:, s0:s0 + n - 1], op0=ALU.mult, op1=ALU.add), [rx, u, convw_s], [u])
                                S.op("dve", lambda e: e.scalar_tensor_tensor(out=u.t[:, s0:s0 + n - 2], in0=rx.t[:, s0 + 2:s0 + n], scalar=cw(3), in1=u.t[:, s0:s0 + n - 2], op0=ALU.mult, op1=ALU.add), [rx, u, convw_s], [u])
                            S.op("act", lambda e: e.activation(out=ubf.t[:], in_=u.t[:], func=AF.Copy), [u], [ubf])
                            if b == 0 and c == 0:
                                dump("lru_u", u, u.t[:], [128, NTOK])
                            for d in range(2):
                                dc = d * 4 + c
                                for bi, (t0, n) in enumerate(blocks):
                                    pa = pbk[(2 * bi) % 4]; px = pbk[(2 * bi + 1) % 4]
                                    S.op("pe", lambda e: e.matmul(pa.t[:, 0:n], lhsT=wabd.t[:, dc, :], rhs=ubf.t[:, t0:t0 + n], start=True, stop=True), [wabd, ubf], [pa])
                                    S.op("pe", lambda e: e.matmul(px.t[:, 0:n], lhsT=wxbd.t[:, dc, :], rhs=ubf.t[:, t0:t0 + n], start=True, stop=True), [wxbd, ubf], [px])
                                    S.op("act", lambda e: e.activation(out=R.t[:, t0:t0 + n], in_=pa.t[:, 0:n], func=AF.Sigmoid, bias=ba_s.t[:, dc:dc + 1]), [pa, ba_s], [R])
                                    S.op("act", lambda e: e.activation(out=I_.t[:, t0:t0 + n], in_=px.t[:, 0:n], func=AF.Sigmoid, bias=bx_s.t[:, dc:dc + 1]), [px, bx_s], [I_])
                                S.op("act", lambda e: e.activation(out=R.t[:], in_=R.t[:], func=AF.Exp, scale=c8.t[:, dc:dc + 1]), [R, c8], [R])
                                S.op("dve", lambda e: e.tensor_tensor(out=rx.t[:], in0=R.t[:], in1=R.t[:], op=ALU.mult), [R], [rx])
                                S.op("act", lambda e: e.activation(out=rx.t[:], in_=rx.t[:], func=AF.Sqrt, scale=-1.0, bias=1.0), [rx], [rx])
                                S.op("dve", lambda e: e.tensor_tensor(out=I_.t[:], in0=I_.t[:], in1=rx.t[:], op=ALU.mult), [I_, rx], [I_])
                                S.op("dve", lambda e: e.tensor_tensor(out=I_.t[:], in0=I_.t[:], in1=u.t[:], op=ALU.mult), [I_, u], [I_])
                                A_c = R.t[:, 0:TC]; B_c = I_.t[:, 0:TC]; A_l = R.t[:, TC:NTOK]; B_l = I_.t[:, TC:NTOK]
                                if d == 0:
                                    S.op("dve", lambda e: e.tensor_tensor_scan(out=Hc.t[:], data0=A_c, data1=B_c, initial=0.0, op0=ALU.mult, op1=ALU.add), [R, I_], [Hc])
                                    S.op("dve", lambda e: e.tensor_tensor_scan(out=H.t[:], data0=A_l, data1=B_l, initial=Hc.t[:, TC - 1:TC], op0=ALU.mult, op1=ALU.add), [R, I_, Hc], [H])
                                else:
                                    S.op("dve", lambda e: e.tensor_tensor_scan(out=Hc.t[:][:, ::-1], data0=A_c[:, ::-1], data1=B_c[:, ::-1], initial=0.0, op0=ALU.mult, op1=ALU.add), [R, I_], [Hc])
                                    hb = rx.t[:, 0:T]
                                    S.op("dve", lambda e: e.tensor_tensor_scan(out=hb[:, ::-1], data0=A_l[:, ::-1], data1=B_l[:, ::-1], initial=Hc.t[:, 0:1], op0=ALU.mult, op1=ALU.add), [R, I_, Hc], [rx])
                                    S.op("dve", lambda e: e.tensor_tensor(out=H.t[:], in0=H.t[:], in1=hb, op=ALU.add), [H, rx], [H])
                            if b == 0 and c == 0:
                                dump("lru_H", H, H.t[:], [128, T])
                            S.op("dve", lambda e: e.tensor_tensor(out=yT.t[:, c, :], in0=H.t[:], in1=gg.t[:], op=ALU.mult), [H, gg], [yT])
                    if b == 0:
                        dump("yT_lru", yT, yT.t[:, 0:4, :], [128, 4, T], BF16)
                    if stop_after == "L":
                        S.finish()
                        return nc, dbg_outs
                    with S.scope() as sN2:
                        gm1x_b = S.sb("gm1xc_b%d" % b, [128, D], F32, sN2); sh1x_b = S.sb("sh1xc_b%d" % b, [128, D], F32, sN2)
                        row_bcast(gm1x_b, lambda k: gm1.t[:, k, b:b + 1], gm1, pbc)
                        row_bcast(sh1x_b, lambda k: modc.t[:, k, b:b + 1], modc, pbc)
                        xcm = x2[b].rearrange("(r c) d -> c r d", c=64)
                        items = []
                        for i in range(16):
                            srcs = [(slice(cl * 32, (cl + 1) * 32), xcm[4 * i + cl]) for cl in range(4)]
                            items.append((i, srcs, gm1x_b, sh1x_b, hxT, hxT.t[:, :, TC + i * 128:TC + (i + 1) * 128]))
                        norm_loop(items)
                    if stop_after == "CM":
                        dump("hxT_cm", hxT, hxT.t[:], [128, 8, NTOK], BF16)
                        S.finish()
                        return nc, dbg_outs
                    with S.scope() as sH:
                        pb8 = S.ps("pb8_%d" % b, [128, 512], F32, sH)
                        whg = S.sb("whg%d" % b, [128, 8, 2048], BF16, sH)
                        whs = [TB(whg.t, "whg%d_s%d" % (b, i)) for i in range(4)]
                        WCOL = {"q": 1024, "v": 1536, "ff": 2048, "fb": 2560, "g": 3072}

                        def wload(slot, name):
                            c0 = WCOL[name]
                            S.dma("pool", whs[slot], None, out=whg.t[:, :, slot * 512:(slot + 1) * 512], in_=w_in[:, c0:c0 + 512].rearrange("(k p) n -> p k n", p=128))

                        wload(0, "q"); wload(1, "v"); wload(2, "ff"); wload(3, "fb")
                        of = S.sb("h_of%d" % b, [128, 16, 512], BF16, sH)
                        Sst = [S.sb("h_S%d_%d" % (b, d), [128, 512], F32, sH) for d in range(2)]
                        Sbf = S.sb("h_Sbf%d" % b, [128, 512], BF16, sH)
                        t_s = S.sb("h_ts%d" % b, [128, 512], F32, sH); t_lf = S.sb("h_tlf%d" % b, [128, 512], F32, sH)
                        t_kf = S.sb("h_tkf%d" % b, [128, 512], F32, sH); t_e = S.sb("h_te%d" % b, [128, 512], F32, sH)
                        dec = S.sb("h_dec%d" % b, [128, 16], F32, sH)
                        kkc4 = [S.sb("h_kkc%d_%d" % (b, cc), [128, 512], BF16, sH) for cc in range(4)]
                        kkc4_src = [None, None]
                        hsl = lambda h: slice(h * 128, (h + 1) * 128)

                        def proj_tok(pp, tok0, slot):
                            for k in range(8):
                                S.op("pe", lambda e: e.matmul(pp.t[:], lhsT=hxT.t[:, k, tok0:tok0 + 128], rhs=whg.t[:, k, slot * 512:(slot + 1) * 512], start=(k == 0), stop=(k == 7)), [hxT, whs[slot]], [pp])

                        def gates(tok0, d, slot, kk_out, dec_out, latent):
                            pz = pbk[0]
                            proj_tok(pz, tok0, slot)
                            S.op("act", lambda e: e.activation(out=t_s.t[:], in_=pz.t[:], func=AF.Sigmoid), [pz], [t_s])
                            S.op("dve", lambda e: e.tensor_tensor(out=t_s.t[:], in0=t_s.t[:], in1=omlB.t[:, d, :], op=ALU.mult), [t_s, omlB], [t_s])
                            S.op("dve", lambda e: e.tensor_tensor(out=t_s.t[:], in0=t_s.t[:], in1=lbB.t[:, d, :], op=ALU.add), [t_s, lbB], [t_s])
                            S.op("act", lambda e: e.activation(out=t_lf.t[:], in_=t_s.t[:], func=AF.Ln), [t_s], [t_lf])
                            S.op("dve", lambda e: e.tensor_scalar(out=t_kf.t[:], in0=t_s.t[:], scalar1=-1.0, scalar2=1.0, op0=ALU.mult, op1=ALU.add), [t_s], [t_kf])
                            prg = pbk[1]
                            Mr = M_gt if d == 0 else M_lt
                            S.op("pe", lambda e: e.matmul(prg.t[:], lhsT=Mr.t[:], rhs=t_lf.t[:], start=True, stop=True), [Mr, t_lf], [prg])
                            S.op("act", lambda e: e.activation(out=t_e.t[:], in_=prg.t[:], func=AF.Exp), [prg], [t_e])
                            S.op("dve", lambda e: e.tensor_tensor(out=kk_out.t[:], in0=t_kf.t[:], in1=t_e.t[:], op=ALU.mult), [t_kf, t_e], [kk_out])
                            for h in range(4):
                                S.op("pe", lambda e: e.matmul(pbc.t[:, h * 4:(h + 1) * 4], lhsT=t_lf.t[:, hsl(h)], rhs=blk1.t[:], start=True, stop=True), [t_lf, blk1], [pbc])
                            S.op("act", lambda e: e.activation(out=dec_out.t[:], in_=pbc.t[:, 0:16], func=AF.Exp), [pbc], [dec_out])
                            if latent:
                                pg = pbk[1]
                                Mg = M_le if d == 0 else M_ge
                                S.op("pe", lambda e: e.matmul(pg.t[:], lhsT=Mg.t[:], rhs=t_lf.t[:], start=True, stop=True), [Mg, t_lf], [pg])
                                S.op("act", lambda e: e.activation(out=t_e2.t[:], in_=pg.t[:], func=AF.Exp), [pg], [t_e2])
                                S.op("dve", lambda e: e.tensor_tensor(out=qd.t[:], in0=qf.t[:], in1=t_e2.t[:], op=ALU.mult), [qf, t_e2], [qd])
                                S.op("act", lambda e: e.activation(out=t_e.t[:], in_=pg.t[:], func=AF.Exp, scale=-1.0), [pg], [t_e])
                                S.op("dve", lambda e: e.tensor_tensor(out=kd.t[:], in0=t_kf.t[:], in1=t_e.t[:], op=ALU.mult), [t_kf, t_e], [kd])

                        def state_step(d, c, kk_tb, v_tb, dec_tb):
                            pkv = pb8
                            kc_ = kk_tb
                            for h in range(4):
                                S.op("pe", lambda e: e.matmul(pkv.t[:, hsl(h)], lhsT=kc_.t[:, hsl(h)], rhs=v_tb.t[:, hsl(h)], start=True, stop=True), [kc_, v_tb], [pkv])
                            for h in range(4):
                                S.op("dve", lambda e: e.scalar_tensor_tensor(out=Sst[d].t[:, hsl(h)], in0=Sst[d].t[:, hsl(h)], scalar=dec_tb.t[:, h * 4 + c:h * 4 + c + 1], in1=pkv.t[:, hsl(h)], op0=ALU.mult, op1=ALU.add), [Sst[d], dec_tb, pkv], [Sst[d]], cols=[Sst[d]])
                            S.op("act", lambda e: e.activation(out=Sbf.t[:], in_=Sst[d].t[:], func=AF.Copy), [Sst[d]], [Sbf])

                        sC_cm = S.scope(); sC = sC_cm.__enter__()
                        c_v = [S.sb("h_cv%d_%d" % (b, t), [128, 512], BF16, sC) for t in range(2)]
                        c_kk = [[S.sb("h_ckk%d_%d_%d" % (b, d, t), [128, 512], BF16, sC) for t in range(2)] for d in range(2)]
                        c_dec = [[S.sb("h_cdec%d_%d_%d" % (b, d, t), [128, 16], F32, sC) for t in range(2)] for d in range(2)]
                        for d in range(2):
                            S.op("pool", lambda e: e.memset(Sst[d].t[:], 0.0), [], [Sst[d]])
                        for t in range(2):
                            pv = pbk[3]
                            proj_tok(pv, t * 128, 1)
                            S.op("act", lambda e: e.activation(out=c_v[t].t[:], in_=pv.t[:], func=AF.Copy), [pv], [c_v[t]])
                            for d in range(2):
                                gates(t * 128, d, 2 + d, c_kk[d][t], c_dec[d][t], False)
                        for d in range(2):
                            order = [(t, c) for t in range(2) for c in range(4)]
                            if d == 1:
                                order = order[::-1]
                            for (t, c) in order:
                                S.op("dve", lambda e: e.tensor_scalar(out=kkc4[c].t[:], in0=c_kk[d][t].t[:], scalar1=blk1.t[:, c:c + 1], scalar2=None, op0=ALU.mult), [c_kk[d][t], blk1], [kkc4[c]])
                                state_step(d, c, kkc4[c], c_v[t], c_dec[d][t])
                        if b == 0:
                            dump("hg_sc0", Sst[0], Sst[0].t[:], [128, 512]); dump("hg_sc1", Sst[1], Sst[1].t[:], [128, 512])

                        if stop_after == "CTX":
                            S.finish()
                            return nc, dbg_outs
                        sC_cm.__exit__(None, None, None)
                        vt = S.sb("h_vt%d" % b, [128, 512], BF16, sH); qf = S.sb("h_qf%d" % b, [128, 512], F32, sH)
                        t_e2 = S.sb("h_te2%d" % b, [128, 512], F32, sH)
                        qd = S.sb("h_qd%d" % b, [128, 512], BF16, sH); kd = S.sb("h_kd%d" % b, [128, 512], BF16, sH)
                        qdT = S.sb("h_qdT%d" % b, [128, 4, 128], BF16, sH); kdT = S.sb("h_kdT%d" % b, [128, 4, 128], BF16, sH)
                        sTm = S.sb("h_sTm%d" % b, [128, 512], BF16, sH)
                        qdTc = S.sb("h_qdTc%d" % b, [128, 4, 4, 128], BF16, sH)
                        t_hg = TB(nb_tmp.t[:, 0:512], "h_hg%d" % b); sg = TB(nb_tmp.t[:, 512:1024], "h_sg%d" % b)
                        ybf = S.sb("h_ybf%d" % b, [128, 512], BF16, sH); st4 = S.sb("h_st4%d" % b, [128, 12], F32, sH)
                        kk = S.sb("h_kk%d" % b, [128, 512], BF16, sH)
                        dec_b = S.sb("h_dec2_%d" % b, [128, 16], F32, sH)
                        a0 = nb_xt[0].t[:].bitcast(BF16); a1 = nb_xt[1].t[:].bitcast(BF16); a2 = nb_xn[0].t[:]
                        vt_s = [vt, TB(a2[:, 0:512], "h_vt2_%d" % b)]
                        sTm_s = [sTm, TB(a2[:, 512:1024], "h_sTm2_%d" % b)]
                        dec_s = [dec, dec_b]
                        qdTc_s = [qdTc, TB(a0.rearrange("p (c h i) -> p c h i", c=4, h=4), "h_qdTc2_%d" % b)]
                        kkc4_s = [kkc4, [TB(a1[:, cc * 512:(cc + 1) * 512], "h_kkc2_%d_%d" % (b, cc)) for cc in range(4)]]

                        def prep_gen(d, t, B):
                            tok0 = TC + t * 128
                            vt_ = vt_s[B]; sTm_ = sTm_s[B]; dec_ = dec_s[B]; qdTc_ = qdTc_s[B]; kkc_ = kkc4_s[B]
                            pv = pbk[3]
                            if d == 0:
                                proj_tok(pv, tok0, 1)
                                S.op("act", lambda e: e.activation(out=vt_.t[:], in_=pv.t[:], func=AF.Copy), [pv], [vt_])
                                proj_tok(pv, tok0, 0)
                                S.op("act", lambda e: e.activation(out=qf.t[:], in_=pv.t[:], func=AF.Copy), [pv], [qf])
                                S.dma("sp", vsave_tb[t], vt_, out=vsave.t.ap()[t * 128:(t + 1) * 128, :], in_=vt_.t[:])
                                S.dma("sp", qsave_tb[t], qf, out=qsave.t.ap()[t * 128:(t + 1) * 128, :], in_=qf.t[:])
                            else:
                                S.dma("sp", vt_, vsave_tb[t], out=vt_.t[:], in_=vsave.t.ap()[t * 128:(t + 1) * 128, :])
                                S.dma("sp", qf, qsave_tb[t], out=qf.t[:], in_=qsave.t.ap()[t * 128:(t + 1) * 128, :])
                            yield
                            gates(tok0, d, 2, kk, dec_, True)
                            for cc in range(4):
                                S.op("dve", lambda e: e.tensor_scalar(out=kkc_[cc].t[:], in0=kk.t[:], scalar1=blk1.t[:, cc:cc + 1], scalar2=None, op0=ALU.mult), [kk, blk1], [kkc_[cc]])
                            yield
                            pq = pT[0]
                            for h in range(4):
                                S.op("pe", lambda e: e.transpose(out=pq.t[:, h, :], in_=qd.t[:, hsl(h)], identity=ident.t[:]), [qd, ident], [pq])
                                S.op("pe", lambda e: e.transpose(out=pq.t[:, 4 + h, :], in_=kd.t[:, hsl(h)], identity=ident.t[:]), [kd, ident], [pq])
                            S.op("act", lambda e: e.activation(out=qdT.t[:], in_=pq.t[:, 0:4, :], func=AF.Copy), [pq], [qdT])
                            S.op("act", lambda e: e.activation(out=kdT.t[:], in_=pq.t[:, 4:8, :], func=AF.Copy), [pq], [kdT])
                            yield
                            ps_ = pbk[0]
                            for h in range(4):
                                S.op("pe", lambda e: e.matmul(ps_.t[:, hsl(h)], lhsT=kdT.t[:, h, :], rhs=qdT.t[:, h, :], start=True, stop=True), [kdT, qdT], [ps_])
                            msk = mask_f if d == 0 else mask_b
                            S.op("dve", lambda e: e.tensor_tensor(out=sTm_.t[:], in0=ps_.t[:], in1=msk.t[:].rearrange("p h i -> p (h i)"), op=ALU.mult), [ps_, msk], [sTm_])
                            for c in range(4):
                                S.op("dve", lambda e: e.tensor_tensor(out=qdTc_.t[:, c, :, :], in0=qdT.t[:], in1=cmask.t[:, c, :, :], op=ALU.mult), [qdT, cmask], [qdTc_])
                            yield

                        def recur_gen(d, t, B):
                            tok0 = TC + t * 128
                            vt_ = vt_s[B]; sTm_ = sTm_s[B]; dec_ = dec_s[B]; qdTc_ = qdTc_s[B]; kkc_ = kkc4_s[B]
                            po = pbk[2]
                            for h in range(4):
                                S.op("pe", lambda e: e.matmul(po.t[:, hsl(h)], lhsT=sTm_.t[:, hsl(h)], rhs=vt_.t[:, hsl(h)], start=(h == 0), stop=False), [sTm_, vt_], [po])
                            cs = range(4) if d == 0 else range(3, -1, -1)
                            for c in cs:
                                for h in range(4):
                                    S.op("pe", lambda e: e.matmul(po.t[:, hsl(h)], lhsT=qdTc_.t[:, c, h, :], rhs=Sbf.t[:, hsl(h)], start=False, stop=True), [qdTc_, Sbf], [po])
                                state_step(d, c, kkc_[c], vt_, dec_)
                                yield
                            if d == 0:
                                S.op("act", lambda e: e.activation(out=of.t[:, t, :], in_=po.t[:], func=AF.Copy), [po], [of])
                                return
                            S.op("dve", lambda e: e.tensor_tensor(out=t_hg.t[:], in0=po.t[:], in1=of.t[:, t, :], op=ALU.add), [po, of], [t_hg])
                            if b == 0 and t == 3:
                                dump("hg_t3", t_hg, t_hg.t[:], [128, 512])
                            S.op("pool", lambda e: e.memset(st4.t[:], 0.0), [], [st4])
                            for h in range(4):
                                S.op("act", lambda e: e.activation(out=nb_junk.t[:, 0:128], in_=t_hg.t[:, hsl(h)], func=AF.Square, accum_out=st4.t[:, h:h + 1]), [t_hg], [st4], cols=[st4])
                            S.op("dve", lambda e: e.tensor_scalar(out=st4.t[:, 4:8], in0=st4.t[:, 0:4], scalar1=1.0 / 128, scalar2=EPS, op0=ALU.mult, op1=ALU.add), [st4], [st4])
                            S.op("act", lambda e: e.activation(out=st4.t[:, 4:8], in_=st4.t[:, 4:8], func=AF.Sqrt), [st4], [st4])
                            S.op("dve", lambda e: e.reciprocal(out=st4.t[:, 8:12], in_=st4.t[:, 4:8]), [st4], [st4])
                            yield
                            pgx = pbk[2]
                            proj_tok(pgx, tok0, 3)
                            S.op("act", lambda e: e.activation(out=sg.t[:], in_=pgx.t[:], func=AF.Silu), [pgx], [sg])
                            for h in range(4):
                                S.op("dve", lambda e: e.scalar_tensor_tensor(out=t_hg.t[:, hsl(h)], in0=t_hg.t[:, hsl(h)], scalar=st4.t[:, 8 + h:9 + h], in1=hn_b.t[:, hsl(h)], op0=ALU.mult, op1=ALU.mult), [t_hg, st4, hn_b], [t_hg], cols=[t_hg])
                            S.op("dve", lambda e: e.tensor_tensor(out=ybf.t[:], in0=t_hg.t[:], in1=sg.t[:], op=ALU.mult), [t_hg, sg], [ybf])
                            yield
                            py_ = pT[1]
                            for h in range(4):
                                S.op("pe", lambda e: e.transpose(out=py_.t[:, h, :], in_=ybf.t[:, hsl(h)], identity=ident.t[:]), [ybf, ident], [py_])
                            for h in range(4):
                                dst = yT.t[:, 4 + h, :].rearrange("p (r c) -> p c r", c=64)[:, 4 * t:4 * t + 4, :]
                                src = py_.t[:, h, :].rearrange("p (c r) -> p c r", r=32)
                                S.op("act", lambda e: e.activation(out=dst, in_=src, func=AF.Copy), [py_], [yT], cols=[yT])

                        for d in range(2):
                            if d == 1:
                                wload(2, "fb"); wload(3, "g")
                            S.op("act", lambda e: e.activation(out=Sbf.t[:], in_=Sst[d].t[:], func=AF.Copy), [Sst[d]], [Sbf])
                            tiles = range(16) if d == 0 else range(15, -1, -1)
                            tiles = list(tiles)[:int(os.environ.get('HG_NT', '16'))]
                            for _ in prep_gen(d, tiles[0], 0):
                                pass
                            for n_, t in enumerate(tiles):
                                rec = recur_gen(d, t, n_ % 2)
                                nxt = prep_gen(d, tiles[n_ + 1], (n_ + 1) % 2) if n_ + 1 < len(tiles) else iter(())
                                ra = True; na = True
                                while ra or na:
                                    if ra:
                                        try:
                                            next(rec)
                                        except StopIteration:
                                            ra = False
                                    if na:
                                        try:
                                            next(nxt)
                                        except StopIteration:
                                            na = False
                            if b == 0 and d == 0:
                                dump("hg_of", of, of.t[:], [128, 16, 512], BF16)
                    if b == 0:
                        dump("yT", yT, yT.t[:], [128, 8, T], BF16)
                    if stop_after == "HG":
                        S.finish()
                        return nc, dbg_outs
                    with S.scope() as sW:
                        wo = S.sb("wo%d" % b, [128, 8, D], BF16, sW)
                        S.dma("pool", wo, None, out=wo.t[:], in_=w_out.rearrange("(k p) n -> p k n", p=128))
                        g1_b = S.sb("g1_b%d" % b, [128, D], F32, sW); gm2_b = S.sb("gm2_b%d" % b, [128, D], F32, sW); sh2_b = S.sb("sh2_b%d" % b, [128, D], F32, sW)
                        row_bcast(g1_b, lambda k: modc.t[:, 16 + k, b:b + 1], modc, pbc)
                        row_bcast(gm2_b, lambda k: gm2.t[:, k, b:b + 1], gm2, pbc)
                        row_bcast(sh2_b, lambda k: modc.t[:, 24 + k, b:b + 1], modc, pbc)
                        rw_s = S.sb("rw_s%d" % b, [128, 8, NE], BF16, sW)
                        S.dma("pool", rw_s, None, out=rw_s.t[:], in_=router_w.rearrange("(k p) n -> p k n", p=128))
                        sw13_s = S.sb("sw13_s%d" % b, [128, 8, 512], BF16, sW)
                        S.dma("pool", sw13_s, None, out=sw13_s.t[:], in_=sw13.rearrange("(k p) n -> p k n", p=128))
                        sw2_s = S.sb("sw2_s%d" % b, [128, 2, D], BF16, sW)
                        S.dma("pool", sw2_s, None, out=sw2_s.t[:], in_=sw2.rearrange("(k p) n -> p k n", p=128))
                        x1t = S.sb("w_x1t%d" % b, [128, D], F32, sW); hx2T = S.sb("w_hx2T%d" % b, [128, 8, 128], BF16, sW)
                        r_sc = S.sb("r_sc%d" % b, [128, NE], F32, sW); r_bi = S.sb("r_bi%d" % b, [128, NE], F32, sW)
                        r_mk = S.sb("r_mk%d" % b, [128, NE], F32, sW); r_sel = S.sb("r_sel%d" % b, [128, NE], F32, sW)
                        r_pos = S.sb("r_pos%d" % b, [128, NE], F32, sW); r_t1 = S.sb("r_t1%d" % b, [128, NE], F32, sW)
                        r_m8 = S.sb("r_m8%d" % b, [128, 8, 8], F32, sW); r_sm = S.sb("r_sm%d" % b, [128, 64], F32, sW)
                        r_ei = S.sb("r_ei%d" % b, [128, 8], U32, sW)
                        hTs = S.sb("w_hTs%d" % b, [128, 2, 128], BF16, sW); hsil = S.sb("w_hsil%d" % b, [128, 256], F32, sW)
                        ysh_t = S.sb("w_ysh%d" % b, [128, D], F32, sW)
                        hx2T_s = [hx2T, S.sb("w_hx2T1_%d" % b, [128, 8, 128], BF16, sW)]
                        x1t_s = [x1t, S.sb("w_x1t1_%d" % b, [128, D], F32, sW)]
                        pw8 = S.ps("pw8_%d" % b, [128, 512], F32, sW)

                        def wo_s1(i):
                            gi = b * 16 + i
                            par = i % 2
                            xt = nb_xt[par]; parts = nb_parts[par]; hx2b = nb_xn[par]; st_ = nb_st[par]; p = pT[par]
                            hx2T = hx2T_s[par]; x1t = x1t_s[par]
                            S.dma("sp", parts[0], None, out=xt.t[:], in_=x2[b, i * 128:(i + 1) * 128, :])
                            for half in range(2):
                                pp = pbk[half]
                                for k in range(8):
                                    S.op("pe", lambda e: e.matmul(pp.t[:], lhsT=yT.t[:, k, i * 128:(i + 1) * 128], rhs=wo.t[:, k, half * 512:(half + 1) * 512], start=(k == 0), stop=(k == 7)), [yT, wo], [pp])
                                S.op("dve", lambda e: e.tensor_tensor(out=nb_tmp.t[:, half * 512:(half + 1) * 512], in0=pp.t[:], in1=g1_b.t[:, half * 512:(half + 1) * 512], op=ALU.mult), [pp, g1_b], [nb_tmp])
                            S.op("dve", lambda e: e.tensor_tensor(out=x1t.t[:], in0=nb_tmp.t[:], in1=xt.t[:], op=ALU.add), [nb_tmp] + parts, [x1t])
                            S.dma("sp", x1d, x1t, out=x1d.t.ap()[gi * 128:(gi + 1) * 128, :], in_=x1t.t[:])
                            rms_rows([x1t], x1t.t[:], st_)
                            S.op("dve", lambda e: e.scalar_tensor_tensor(out=nb_tmp.t[:], in0=x1t.t[:], scalar=st_.t[:, 2:3], in1=gm2_b.t[:], op0=ALU.mult, op1=ALU.mult), [x1t, st_, gm2_b], [nb_tmp])
                            S.op("dve", lambda e: e.tensor_tensor(out=hx2b.t[:], in0=nb_tmp.t[:], in1=sh2_b.t[:], op=ALU.add), [nb_tmp, sh2_b], [hx2b])
                            for k in range(8):
                                S.op("pe", lambda e: e.transpose(out=p.t[:, k, :], in_=hx2b.t[:, k * 128:(k + 1) * 128], identity=ident.t[:]), [hx2b, ident], [p])
                            S.op("act", lambda e: e.activation(out=hx2T.t[:], in_=p.t[:], func=AF.Copy), [p], [hx2T])
                            if gi == 0:
                                dump("x1t0", x1t, x1t.t[:], [128, D]); dump("hx2b0", hx2b, hx2b.t[:], [128, D], BF16)

                        def wo_s2(i):
                            gi = b * 16 + i
                            par = i % 2
                            xt = nb_xt[par]; parts = nb_parts[par]; hx2b = nb_xn[par]; st_ = nb_st[par]; p = pT[par]
                            hx2T = hx2T_s[par]; x1t = x1t_s[par]
                            pl = pbk[2]
                            for k in range(8):
                                S.op("pe", lambda e: e.matmul(pl.t[:, 0:NE], lhsT=hx2T.t[:, k, :], rhs=rw_s.t[:, k, :], start=(k == 0), stop=(k == 7)), [hx2T, rw_s], [pl])
                            S.op("act", lambda e: e.activation(out=r_sc.t[:], in_=pl.t[:, 0:NE], func=AF.Sigmoid), [pl], [r_sc])
                            S.op("dve", lambda e: e.tensor_tensor(out=r_bi.t[:], in0=r_sc.t[:], in1=rb_b.t[:], op=ALU.add), [r_sc, rb_b], [r_bi])
                            for g in range(8):
                                S.op("dve", lambda e: e.max(out=r_m8.t[:, g, :], in_=r_bi.t[:, g * 32:(g + 1) * 32]), [r_bi], [r_m8], cols=[r_m8])
                            S.op("dve", lambda e: e.tensor_tensor(out=r_sm.t[:, 0:8], in0=r_m8.t[:, :, 0], in1=r_m8.t[:, :, 1], op=ALU.add), [r_m8], [r_sm])
                            S.op("dve", lambda e: e.max(out=r_sm.t[:, 8:16], in_=r_sm.t[:, 0:8]), [r_sm], [r_sm])
                            S.op("dve", lambda e: e.tensor_scalar(out=r_sm.t[:, 16:24], in0=r_sm.t[:, 0:8], scalar1=r_sm.t[:, 11:12], scalar2=None, op0=ALU.is_ge), [r_sm], [r_sm])
                            for g in range(8):
                                S.op("dve", lambda e: e.tensor_scalar(out=r_mk.t[:, g * 32:(g + 1) * 32], in0=r_bi.t[:, g * 32:(g + 1) * 32], scalar1=8.0, scalar2=r_sm.t[:, 16 + g:17 + g], op0=ALU.add, op1=ALU.mult), [r_bi, r_sm], [r_mk], cols=[r_mk])
                            S.op("dve", lambda e: e.max(out=r_sm.t[:, 24:32], in_=r_mk.t[:]), [r_mk], [r_sm])
                            S.op("dve", lambda e: e.max_index(out=r_ei.t[:], in_max=r_sm.t[:, 24:32], in_values=r_mk.t[:]), [r_sm, r_mk], [r_ei])
                            S.op("dve", lambda e: e.tensor_copy(out=r_sm.t[:, 32:40], in_=r_ei.t[:]), [r_ei], [r_sm])
                            S.op("dve", lambda e: e.tensor_scalar(out=r_sel.t[:], in0=r_mk.t[:], scalar1=r_sm.t[:, 31:32], scalar2=None, op0=ALU.is_ge), [r_mk, r_sm], [r_sel])
                            pq_ = pbk[3]
                            S.op("pe", lambda e: e.matmul(pq_.t[:, 0:NE], lhsT=Lst.t[:], rhs=r_sel.t[:], start=True, stop=True), [Lst, r_sel], [pq_])
                            S.op("pe", lambda e: e.matmul(pq_.t[:, NE:2 * NE], lhsT=ones.t[:], rhs=r_sel.t[:], start=True, stop=True), [ones, r_sel], [pq_])
                            S.op("dve", lambda e: e.tensor_tensor(out=r_pos.t[:], in0=pq_.t[:, 0:NE], in1=carry.t[:], op=ALU.add), [pq_, carry], [r_pos])
                            S.op("dve", lambda e: e.tensor_tensor(out=carry.t[:], in0=pq_.t[:, NE:2 * NE], in1=carry.t[:], op=ALU.add), [pq_, carry], [carry])
                            S.op("pool", lambda e: e.memset(r_sm.t[:, 40:64], 0.0), [r_sm], [r_sm])
                            for k in range(8):
                                S.op("dve", lambda e: e.scalar_tensor_tensor(out=r_t1.t[:], in0=iota_f.t[:], scalar=r_sm.t[:, 32 + k:33 + k], in1=r_pos.t[:], op0=ALU.is_equal, op1=ALU.mult, accum_out=r_sm.t[:, 40 + k:41 + k]), [iota_f, r_sm, r_pos], [r_sm], cols=[r_sm])
                                S.op("dve", lambda e: e.scalar_tensor_tensor(out=r_t1.t[:], in0=iota_f.t[:], scalar=r_sm.t[:, 32 + k:33 + k], in1=r_sc.t[:], op0=ALU.is_equal, op1=ALU.mult, accum_out=r_sm.t[:, 48 + k:49 + k]), [iota_f, r_sm, r_sc], [r_sm], cols=[r_sm])
                            S.op("dve", lambda e: e.reduce_sum(out=r_sm.t[:, 56:57], in_=r_sm.t[:, 48:56], axis=AX.X), [r_sm], [r_sm])
                            S.op("dve", lambda e: e.reciprocal(out=r_sm.t[:, 57:58], in_=r_sm.t[:, 56:57]), [r_sm], [r_sm])
                            S.op("dve", lambda e: e.tensor_scalar(out=gates_all.t[:, gi, :], in0=r_sm.t[:, 48:56], scalar1=r_sm.t[:, 57:58], scalar2=2.5, op0=ALU.mult, op1=ALU.mult), [r_sm], [gates_all])
                            S.op("pool", lambda e: e.tensor_copy(out=eidx_all.t[:, gi * 8:gi * 8 + 8], in_=r_sm.t[:, 32:40]), [r_sm], [eidx_all])
                            S.op("pool", lambda e: e.tensor_copy(out=pos_all.t[:, gi * 8:gi * 8 + 8], in_=r_sm.t[:, 40:48]), [r_sm], [pos_all])
                            S.dma("sp", hx2d, hx2b, out=hx2d.t.ap()[gi * 128:(gi + 1) * 128, :], in_=hx2b.t[:])
                            pu = pbc
                            for c in range(4):
                                for k in range(8):
                                    S.op("pe", lambda e: e.matmul(pu.t[:, c * 128:(c + 1) * 128], lhsT=sw13_s.t[:, k, c * 128:(c + 1) * 128], rhs=hx2T.t[:, k, :], start=(k == 0), stop=(k == 7)), [sw13_s, hx2T], [pu])
                            S.op("act", lambda e: e.activation(out=hsil.t[:], in_=pu.t[:, 0:256], func=AF.Silu), [pu], [hsil])
                            S.op("dve", lambda e: e.tensor_tensor(out=hTs.t[:].rearrange("p c t -> p (c t)"), in0=hsil.t[:], in1=pu.t[:, 256:512], op=ALU.mult), [hsil, pu], [hTs])
                            for half in range(2):
                                pp = pw8 if half == 0 else pbk[2]
                                for c2 in range(2):
                                    S.op("pe", lambda e: e.matmul(pp.t[:], lhsT=hTs.t[:, c2, :], rhs=sw2_s.t[:, c2, half * 512:(half + 1) * 512], start=(c2 == 0), stop=(c2 == 1)), [hTs, sw2_s], [pp])
                                S.op("act", lambda e: e.activation(out=ysh_t.t[:, half * 512:(half + 1) * 512], in_=pp.t[:], func=AF.Copy), [pp], [ysh_t])
                            S.dma("sp", yshd, ysh_t, out=yshd.t.ap()[gi * 128:(gi + 1) * 128, :], in_=ysh_t.t[:])
                            if gi == 0:
                                dump("ysh0", ysh_t, ysh_t.t[:], [128, D])
                        wo_s1(0)
                        for i in range(16):
                            if i + 1 < 16:
                                wo_s1(i + 1)
                            wo_s2(i)
        dump("eidx_all", eidx_all, eidx_all.t[:], [128, 256]); dump("pos_all", pos_all, pos_all.t[:], [128, 256]); dump("gates_all", gates_all, gates_all.t[:], [128, 32, 8])
        dump("carry", carry, carry.t[:], [128, NE])
        if stop_after == "WO":
            S.finish()
            return nc, dbg_outs
        NBLK = 512
        slots_all = S.sb("slots_all", [128, 256], I32)
        E_all = S.sb("E_all", [128, NBLK], F32)
        with S.scope() as sP:
            padded = S.sb("padded", [128, NE], F32, sP); pad_i = S.sb("pad_i", [128, NE], I32, sP)
            pends = S.sb("pends", [128, NE], F32, sP); pstart = S.sb("pstart", [128, NE], F32, sP)
            ones256 = S.sb("ones256", [128, NE], F32, sP); junk256 = S.sb("junk256", [128, NE], F32, sP)
            ps8 = S.sb("ps8", [128, 8], F32, sP)
            hxr = [S.sb("hxr%d" % i, [128, D], BF16, sP) for i in range(2)]
            S.op("pool", lambda e: e.memset(ones256.t[:], 1.0), [], [ones256])
            S.op("dve", lambda e: e.tensor_scalar(out=padded.t[:], in0=carry.t[:], scalar1=127.0, scalar2=None, op0=ALU.add), [carry], [padded])
            S.op("dve", lambda e: e.tensor_copy(out=pad_i.t[:], in_=padded.t[:]), [padded], [pad_i])
            S.op("dve", lambda e: e.tensor_scalar(out=pad_i.t[:], in0=pad_i.t[:], scalar1=7, scalar2=7, op0=ALU.arith_shift_right, op1=ALU.logical_shift_left), [pad_i], [pad_i])
            S.op("dve", lambda e: e.tensor_copy(out=padded.t[:], in_=pad_i.t[:]), [pad_i], [padded])
            S.op("dve", lambda e: e.tensor_tensor_scan(out=pends.t[:], data0=ones256.t[:], data1=padded.t[:], initial=0.0, op0=ALU.mult, op1=ALU.add), [ones256, padded], [pends])
            S.op("dve", lambda e: e.tensor_tensor(out=pstart.t[:], in0=pends.t[:], in1=padded.t[:], op=ALU.subtract), [pends, padded], [pstart])
            dump("pstart", pstart, pstart.t[:], [128, NE])
            slots_tb = [TB(slots_all.t, "slots_g%d" % g_) for g_ in range(32)]
            xs_parts = [TB(xs_pad.t, "xs_part%d" % k_) for k_ in range(8)]
            hxr.append(S.sb("hxr2", [128, D], BF16, sP))
            ps8s = [ps8, S.sb("ps8b", [128, 8], F32, sP)]
            for gi in range(32):
                hx = hxr[gi % 3]; ps8 = ps8s[gi % 2]
                S.dma("sp", hx, hx2d, out=hx.t[:], in_=hx2d.t.ap()[gi * 128:(gi + 1) * 128, :])
                S.op("dve", lambda e: e.memset(ps8.t[:], 0.0), [], [ps8])
                for k in range(8):
                    S.op("dve", lambda e: e.scalar_tensor_tensor(out=junk256.t[:], in0=iota_f.t[:], scalar=eidx_all.t[:, gi * 8 + k:gi * 8 + k + 1], in1=pstart.t[:], op0=ALU.is_equal, op1=ALU.mult, accum_out=ps8.t[:, k:k + 1]), [iota_f, eidx_all, pstart], [ps8], cols=[ps8])
                S.op("dve", lambda e: e.tensor_tensor(out=ps8.t[:], in0=ps8.t[:], in1=pos_all.t[:, gi * 8:gi * 8 + 8], op=ALU.add), [ps8, pos_all], [ps8])
                S.op("dve", lambda e: e.tensor_copy(out=slots_all.t[:, gi * 8:gi * 8 + 8], in_=ps8.t[:]), [ps8], [slots_tb[gi]])
                for k in range(8):
                    S.dma("pool", xs_parts[k], hx, out=xs_pad.t.ap(), in_=hx.t[:], extra_reads=[slots_tb[gi]],
                          indirect=dict(out_offset=bass.IndirectOffsetOnAxis(ap=slots_all.t[:, gi * 8 + k:gi * 8 + k + 1], axis=0), in_offset=None, bounds_check=NSLOT - 1, oob_is_err=False))
            S.op("pool", lambda e: e.memset(E_all.t[:], 0.0), [], [E_all])
            for j in range(NBLK):
                S.op("dve", lambda e: e.tensor_scalar(out=junk256.t[:], in0=pends.t[:], scalar1=float(128 * j), scalar2=0.0, op0=ALU.is_le, op1=ALU.add, accum_out=E_all.t[:, j:j + 1]), [pends], [E_all], cols=[E_all])
            dump("E_all", E_all, E_all.t[:], [128, NBLK])
        S.barrier()
        dump("slots_all", slots_all, slots_all.t[:], [128, 256], I32)
        if stop_after == "SC":
            S.finish()
            return nc, dbg_outs

        with S.scope() as sE:
            pidx_i = S.sb("pidx_i", [128, 1], I32, sE); pidx = S.sb("pidx", [128, 1], F32, sE)
            S.op("pool", lambda e: e.iota(pidx_i.t[:], pattern=[[0, 1]], base=0, channel_multiplier=1), [], [pidx_i])
            S.op("dve", lambda e: e.tensor_copy(out=pidx.t[:], in_=pidx_i.t[:]), [pidx_i], [pidx])
            NBUF = 4
            w13f = [S.sb("w13f%d" % i, [128, 8, 512], F32, sE) for i in range(NBUF)]
            w2f = [S.sb("w2f%d" % i, [128, 2, D], F32, sE) for i in range(NBUF)]
            w13b = [S.sb("w13b%d" % i, [128, 8, 512], BF16, sE) for i in range(2)]
            w2b = [S.sb("w2b%d" % i, [128, 2, D], BF16, sE) for i in range(2)]
            wix_f = [S.sb("wixf%d" % i, [128, 1], F32, sE) for i in range(NBUF)]
            wix = [S.sb("wix%d" % i, [128, 1], I32, sE) for i in range(NBUF)]
            xr = [S.sb("e_xr%d" % i, [128, D], BF16, sE) for i in range(2)]
            xTe = [S.sb("e_xT%d" % i, [128, 8, 128], BF16, sE) for i in range(2)]
            usil = S.sb("e_usil", [128, 256], F32, sE); hb = S.sb("e_hb", [128, 256], BF16, sE)
            hTe = S.sb("e_hT", [128, 2, 128], BF16, sE)
            yo = [S.sb("e_yo%d" % i, [128, D], F32, sE) for i in range(2)]
            ptx = [S.ps("e_ptx%d" % i, [128, 8, 128], BF16, sE) for i in range(2)]
            pu_ = [S.ps("e_pu%d" % i, [128, 512], F32, sE) for i in range(2)]
            py2 = [[S.ps("e_py%d_%d" % (i, h), [128, 512], F32, sE) for h in range(2)] for i in range(2)]
            nblk_run = int(os.environ.get("MOE_NBLK", NBLK))
            WB = ne_decl * 128 - 1

            def load_w(j):
                i = j % NBUF
                if os.environ.get("MOE_NOLOAD"):
                    return
                S.op("dve", lambda e: e.tensor_scalar(out=wix_f[i].t[:], in0=E_all.t[:, j:j + 1], scalar1=128.0, scalar2=pidx.t[:, 0:1], op0=ALU.mult, op1=ALU.add), [E_all, pidx], [wix_f[i]])
                S.op("dve", lambda e: e.tensor_copy(out=wix[i].t[:], in_=wix_f[i].t[:]), [wix_f[i]], [wix[i]])
                S.dma("pool", w13f[i], None, out=w13f[i].t[:].rearrange("p k n -> p (k n)"), in_=w13, extra_reads=[wix[i]],
                      indirect=dict(out_offset=None, in_offset=bass.IndirectOffsetOnAxis(ap=wix[i].t[:, 0:1], axis=0), bounds_check=WB, oob_is_err=False))
                S.dma("pool", w2f[i], None, out=w2f[i].t[:].rearrange("p k n -> p (k n)"), in_=w2, extra_reads=[wix[i]],
                      indirect=dict(out_offset=None, in_offset=bass.IndirectOffsetOnAxis(ap=wix[i].t[:, 0:1], axis=0), bounds_check=WB, oob_is_err=False))

            def cast_w(j):
                i = j % NBUF; o = j % 2
                if os.environ.get("MOE_NOCAST"):
                    return
                S.op("act", lambda e: e.activation(out=w13b[o].t[:, 0:3, :], in_=w13f[i].t[:, 0:3, :], func=AF.Copy), [w13f[i]], [w13b[o]])
                S.op("dve", lambda e: e.tensor_scalar(out=w13b[o].t[:, 3:8, :], in0=w13f[i].t[:, 3:8, :], scalar1=1.0, scalar2=None, op0=ALU.mult), [w13f[i]], [w13b[o]])
                S.op("act", lambda e: e.activation(out=w2b[o].t[:, 0, :], in_=w2f[i].t[:, 0, :], func=AF.Copy), [w2f[i]], [w2b[o]])
                S.op("dve", lambda e: e.tensor_scalar(out=w2b[o].t[:, 1, :], in0=w2f[i].t[:, 1, :], scalar1=1.0, scalar2=None, op0=ALU.mult), [w2f[i]], [w2b[o]])

            xr3 = xr + [S.sb("e_xr2", [128, D], BF16, sE)]
            usil2 = [usil, S.sb("e_usil1", [128, 256], F32, sE)]
            hb2 = [hb, S.sb("e_hb1", [128, 256], BF16, sE)]
            hTe2 = [hTe, S.sb("e_hT1", [128, 2, 128], BF16, sE)]

            def s_load_x(j):
                S.dma("sp", xr3[j % 3], xs_pad, out=xr3[j % 3].t[:], in_=xs_pad.t.ap()[j * 128:(j + 1) * 128, :])

            def s_Tx(j):
                x_ = xr3[j % 3]; p_ = ptx[j % 2]
                for k in range(8):
                    S.op("pe", lambda e: e.transpose(out=p_.t[:, k, :], in_=x_.t[:, k * 128:(k + 1) * 128], identity=ident.t[:]), [x_, ident], [p_])
                S.op("act", lambda e: e.activation(out=xTe[j % 2].t[:], in_=p_.t[:], func=AF.Copy), [p_], [xTe[j % 2]])

            def s_up(j):
                o = j % 2
                for k in range(8):
                    S.op("pe", lambda e: e.matmul(pu_[o].t[:], lhsT=xTe[o].t[:, k, :], rhs=w13b[o].t[:, k, :], start=(k == 0), stop=(k == 7)), [xTe[o], w13b[o]], [pu_[o]])
                S.op("act", lambda e: e.activation(out=usil2[o].t[:], in_=pu_[o].t[:, 0:256], func=AF.Silu), [pu_[o]], [usil2[o]])
                S.op("dve", lambda e: e.tensor_tensor(out=hb2[o].t[:], in0=usil2[o].t[:], in1=pu_[o].t[:, 256:512], op=ALU.mult), [usil2[o], pu_[o]], [hb2[o]])

            def s_Th(j):
                o = j % 2; p_ = ptx[o]
                for c2 in range(2):
                    S.op("pe", lambda e: e.transpose(out=p_.t[:, c2, :], in_=hb2[o].t[:, c2 * 128:(c2 + 1) * 128], identity=ident.t[:]), [hb2[o], ident], [p_])
                S.op("act", lambda e: e.activation(out=hTe2[o].t[:], in_=p_.t[:, 0:2, :], func=AF.Copy), [p_], [hTe2[o]])

            def s_down(j):
                o = j % 2
                for half in range(2):
                    pp = py2[o][half]
                    for c2 in range(2):
                        S.op("pe", lambda e: e.matmul(pp.t[:], lhsT=hTe2[o].t[:, c2, :], rhs=w2b[o].t[:, c2, half * 512:(half + 1) * 512], start=(c2 == 0), stop=(c2 == 1)), [hTe2[o], w2b[o]], [pp])
                    if half == 0:
                        S.op("act", lambda e: e.activation(out=yo[o].t[:, 0:512], in_=pp.t[:], func=AF.Copy), [pp], [yo[o]])
                    else:
                        S.op("dve", lambda e: e.tensor_copy(out=yo[o].t[:, 512:1024], in_=pp.t[:]), [pp], [yo[o]])
                S.dma("sp", ys_pad, yo[o], out=ys_pad.t.ap()[j * 128:(j + 1) * 128, :], in_=yo[o].t[:])

            n_ = nblk_run
            load_w(0)
            if n_ > 1:
                load_w(1)
            if n_ > 2:
                load_w(2)
            s_load_x(0)
            if n_ > 1:
                s_load_x(1)
            cast_w(0); s_Tx(0); s_up(0)
            for j in range(n_):
                if j + 3 < n_:
                    load_w(j + 3)
                if j + 2 < n_:
                    s_load_x(j + 2)
                if j + 1 < n_:
                    cast_w(j + 1); s_Tx(j + 1)
                s_Th(j)
                if j + 1 < n_:
                    s_up(j + 1)
                s_down(j)
        if stop_after == "EXP":
            S.finish()
            return nc, dbg_outs

        with S.scope() as sF:
            g2_b = [S.sb("g2_b%d" % b, [128, D], F32, sF) for b in range(NB)]
            pbf = S.ps("pbf", [128, 512], F32, sF)
            for b in range(NB):
                row_bcast(g2_b[b], lambda k: modc.t[:, 40 + k, b:b + 1], modc, pbf)
            accs = [S.sb("f_acc%d" % i, [128, D], F32, sF) for i in range(2)]
            gat = [S.sb("f_gat%d" % i, [128, D], F32, sF) for i in range(12)]
            x1rs = [S.sb("f_x1r%d" % i, [128, D], F32, sF) for i in range(2)]; fo = [S.sb("f_o%d" % i, [128, D], F32, sF) for i in range(2)]
            fst = S.sb("f_st", [128, 4], F32, sF); fjunk = S.sb("f_junk", [128, D], BF16, sF)
            for gi in range(32):
                b = gi // 16
                acc = accs[gi % 2]; x1r = x1rs[gi % 2]
                S.dma("sp", acc, yshd, out=acc.t[:], in_=yshd.t.ap()[gi * 128:(gi + 1) * 128, :])
                S.dma("sp", x1r, x1d, out=x1r.t[:], in_=x1d.t.ap()[gi * 128:(gi + 1) * 128, :])
                for k in range(8):
                    g = gat[(gi * 8 + k) % 12]
                    S.dma("pool", g, ys_pad, out=g.t[:], in_=ys_pad.t.ap(), extra_reads=[slots_all],
                          indirect=dict(out_offset=None, in_offset=bass.IndirectOffsetOnAxis(ap=slots_all.t[:, gi * 8 + k:gi * 8 + k + 1], axis=0)))
                    S.op("dve", lambda e: e.scalar_tensor_tensor(out=acc.t[:], in0=g.t[:], scalar=gates_all.t[:, gi, k:k + 1], in1=acc.t[:], op0=ALU.mult, op1=ALU.add), [g, gates_all, acc], [acc])
                S.op("dve", lambda e: e.tensor_tensor(out=acc.t[:], in0=acc.t[:], in1=g2_b[b].t[:], op=ALU.mult), [acc, g2_b[b]], [acc])
                S.op("dve", lambda e: e.tensor_tensor(out=acc.t[:], in0=acc.t[:], in1=x1r.t[:], op=ALU.add), [acc, x1r], [acc])
                S.op("dve", lambda e: e.memset(fst.t[:, 0:1], 0.0), [], [fst])
                S.op("act", lambda e: e.activation(out=fjunk.t[:], in_=acc.t[:], func=AF.Square, accum_out=fst.t[:, 0:1]), [acc], [fst])
                S.op("dve", lambda e: e.tensor_scalar(out=fst.t[:, 1:2], in0=fst.t[:, 0:1], scalar1=1.0 / D, scalar2=EPS, op0=ALU.mult, op1=ALU.add), [fst], [fst])
                S.op("act", lambda e: e.activation(out=fst.t[:, 1:2], in_=fst.t[:, 1:2], func=AF.Sqrt), [fst], [fst])
                S.op("dve", lambda e: e.reciprocal(out=fst.t[:, 2:3], in_=fst.t[:, 1:2]), [fst], [fst])
                o_ = fo[gi % 2]
                S.op("dve", lambda e: e.scalar_tensor_tensor(out=o_.t[:], in0=acc.t[:], scalar=fst.t[:, 2:3], in1=nfin_b.t[:], op0=ALU.mult, op1=ALU.mult), [acc, fst, nfin_b], [o_])
                S.dma("sp", None, o_, out=out[b, (gi % 16) * 128:(gi % 16 + 1) * 128, :], in_=o_.t[:], final=True)
        S.finish()
    return nc, dbg_outs


def prep_inputs(inp, cores=range(8), ne_decl=256):
    f = lambda a: np.ascontiguousarray(np.asarray(a, dtype=np.float32))
    col8 = lambda v: f(np.asarray(v).reshape(8, 128).T)
    ada_bc = f(np.asarray(inp["ada_b"])[0].reshape(48, 128).T)
    conv_w = np.asarray(inp["lru_conv_w"])[0]
    convw = f(conv_w.T.reshape(4, 128, 4).transpose(1, 0, 2).reshape(128, 16))
    convb = f(np.asarray(inp["lru_conv_b"])[0].reshape(4, 128).T)

    def blockdiag(w):
        w = np.asarray(w)[0]
        o = np.zeros((128, 8, 128), np.float32)
        for d in range(2):
            for c in range(4):
                for hh in range(2):
                    o[hh * 64:(hh + 1) * 64, d * 4 + c, hh * 64:(hh + 1) * 64] = w[d, 2 * c + hh]
        return o

    def col_dc(v):
        return f(np.asarray(v)[0].reshape(2, 4, 128).transpose(2, 0, 1).reshape(128, 8))

    shared = {
        "ada_w": f(np.asarray(inp["ada_w"])[0]), "ada_bc": ada_bc,
        "nmix": col8(np.asarray(inp["norm_mix"])[0]), "nffn": col8(np.asarray(inp["norm_ffn"])[0]),
        "nfin": f(np.asarray(inp["norm_final"]).reshape(1, 1024)),
        "w_in": f(np.asarray(inp["w_in"])[0]), "w_out": f(np.asarray(inp["w_out"])[0]),
        "convw": convw, "convb": convb,
        "wa_bd": blockdiag(inp["lru_wa"]), "wx_bd": blockdiag(inp["lru_wx"]),
        "ba_c": col_dc(inp["lru_ba"]), "bx_c": col_dc(inp["lru_bx"]), "lam_c": col_dc(inp["lru_lambda"]),
        "lbl": f(np.asarray(inp["hgrn_lb_logits"]).reshape(1, 2048)),
        "hnorm": f(np.asarray(inp["hgrn_norm"]).reshape(1, 512)),
        "router_w": f(np.asarray(inp["router_w"])[0]), "router_b": f(np.asarray(inp["router_b"]).reshape(1, 256)),
        "w13": f(np.asarray(inp["exp_w13"])[0][:ne_decl].reshape(ne_decl, 8, 128, 512).transpose(0, 2, 1, 3).reshape(ne_decl * 128, 4096)),
        "w2": f(np.asarray(inp["exp_w2"])[0][:ne_decl].reshape(ne_decl, 2, 128, 1024).transpose(0, 2, 1, 3).reshape(ne_decl * 128, 2048)),
        "sw13": f(np.asarray(inp["shared_w13"])[0]), "sw2": f(np.asarray(inp["shared_w2"])[0]),
    }
    x = np.asarray(inp["x"]); c = np.asarray(inp["c"]); ctx = np.asarray(inp["ctx"]); c_ctx = np.asarray(inp["c_ctx"])
    maps = []
    for i in cores:
        m = dict(shared)
        m["x2"] = f(x[2 * i:2 * i + 2]); m["ctx2"] = f(ctx[2 * i:2 * i + 2])
        m["cT"] = f(np.stack([c[2 * i], c[2 * i + 1], c_ctx], axis=1))
        maps.append(m)
    return maps


_NC_CACHE = {}


def kernel(**inputs):
    if "nc" not in _NC_CACHE:
        _NC_CACHE["nc"] = build_nc()[0]
    nc = _NC_CACHE["nc"]
    maps = prep_inputs(inputs)
    res = run_bass_kernel_spmd(nc, maps, core_ids=list(range(8)))
    return np.concatenate([r["out"] for r in res.results], axis=0).astype(np.float32)
```

```python
import os
import numpy as np
from contextlib import ExitStack
import concourse.bass as bass
import concourse.mybir as mybir
from concourse.bass_utils import run_bass_kernel_spmd

F32 = mybir.dt.float32
BF16 = mybir.dt.bfloat16
I32 = mybir.dt.int32
U32 = mybir.dt.uint32
AF = mybir.ActivationFunctionType
ALU = mybir.AluOpType
AX = mybir.AxisListType

N_DMA_SEMS = 16


class TB:
    def __init__(self, t, name):
        self.t = t
        self.name = name
        self.w = None
        self.r = {}

    def sub(self, name=None):
        return TB(self.t, name or self.name + "_s")


class _Scope:
    def __init__(self, S):
        self.S = S
        self.st = ExitStack()

    def __enter__(self):
        self.st.__enter__()
        return self.st

    def __exit__(self, *a):
        r = self.st.__exit__(*a)
        if a[0] is None:
            self.S.barrier()
        return r


class Sched:
    def __init__(self, nc, st):
        self.nc = nc
        self.st = st
        self.eng = {"pe": nc.tensor, "act": nc.scalar, "dve": nc.vector, "pool": nc.gpsimd, "sp": nc.sync}
        self.sem = {}
        for k in self.eng:
            self.sem[k] = st.enter_context(nc.semaphore("s_" + k))
        self.dq = {"sp": 0, "pool": 1, "act": 2}
        for i in range(3 * N_DMA_SEMS):
            self.sem[("d", i)] = st.enter_context(nc.semaphore("d%d" % i))
        self.tick = {k: 0 for k in self.eng}
        self.seen = {k: {} for k in self.eng}
        self.dval = [0] * (3 * N_DMA_SEMS)
        self.dnext = [0, 0, 0]
        self.finals = []
        self.bregs = {}
        self.ninstr = 0

    def sb(self, name, shape, dtype, st=None):
        t = (st or self.st).enter_context(self.nc.sbuf_tensor(name, list(shape), dtype))
        return TB(t, name)

    def ps(self, name, shape, dtype, st=None):
        t = (st or self.st).enter_context(self.nc.psum_tensor(name, list(shape), dtype))
        return TB(t, name)

    def dram(self, name, shape, dtype):
        t = self.nc.dram_tensor(name, list(shape), dtype, kind="Internal")
        return TB(t, name)

    def _wait(self, E, tok):
        if tok is None:
            return
        key, val = tok
        if key == E and E == "pe":
            return
        if self.seen[E].get(key, 0) >= val:
            return
        self.eng[E].wait_ge(self.sem[key], val)
        self.seen[E][key] = val

    def _deps(self, E, reads, writes, cols=()):
        for b in reads:
            self._wait(E, b.w)
        for b in writes:
            same_ok = any(b is c for c in cols)
            if not (same_ok and b.w is not None and b.w[0] == E):
                self._wait(E, b.w)
            for k, v in list(b.r.items()):
                if same_ok and k == E:
                    continue
                self._wait(E, (k, v))

    def _commit(self, tok, reads, writes):
        key, val = tok
        for b in reads:
            if b.r.get(key, 0) < val:
                b.r[key] = val
        for b in writes:
            b.w = tok
            b.r = {}

    def op(self, E, fn, reads=(), writes=(), cols=()):
        reads = [b for b in reads if isinstance(b, TB)]
        writes = [b for b in writes if isinstance(b, TB)]
        self._deps(E, reads, writes, cols)
        ins = fn(self.eng[E])
        self.tick[E] += 1
        ins.then_inc(self.sem[E], 1)
        self._commit((E, self.tick[E]), reads, writes)
        self.ninstr += 1
        return ins

    def dma(self, Q, dst, src, out, in_, final=False, indirect=None, extra_reads=(), **kw):
        reads = [b for b in [src] + list(extra_reads) if isinstance(b, TB)]
        writes = [b for b in [dst] if isinstance(b, TB)]
        self._deps(Q, reads, writes)
        qi = self.dq[Q]
        s = qi * N_DMA_SEMS + self.dnext[qi]
        self.dnext[qi] = (self.dnext[qi] + 1) % N_DMA_SEMS
        key = ("d", s)
        if self.dval[s] > 0:
            self._wait(Q, (key, self.dval[s]))
        if indirect is not None:
            indirect = dict(indirect)
            bc = indirect.get("bounds_check")
            if isinstance(bc, int):
                if bc not in self.bregs:
                    r = self.eng[Q].alloc_register("bc_%d" % bc)
                    self.eng[Q].reg_mov(r, bc)
                    self.bregs[bc] = r
                indirect["bounds_check"] = self.bregs[bc]
            ins = self.eng[Q].indirect_dma_start(out=out, in_=in_, **indirect, **kw)
        else:
            ins = self.eng[Q].dma_start(out=out, in_=in_, **kw)
        self.dval[s] += 16
        ins.then_inc(self.sem[key], 16)
        tok = (key, self.dval[s])
        self._commit(tok, reads, writes)
        if final:
            self.finals.append(tok)
        self.ninstr += 1
        return ins

    def barrier(self):
        for E in self.eng:
            for F in ("pe", "act", "dve", "pool"):
                if F != E and self.tick[F] > 0:
                    self._wait(E, (F, self.tick[F]))
            for i in range(3 * N_DMA_SEMS):
                if self.dval[i] > 0:
                    self._wait(E, (("d", i), self.dval[i]))

    def scope(self):
        return _Scope(self)

    def finish(self):
        for tok in self.finals:
            self._wait("sp", tok)
        for k in ("pe", "act", "dve", "pool"):
            if self.tick[k] > 0:
                self._wait("sp", (k, self.tick[k]))

    def make_ident(self, ident):
        self.op("pool", lambda e: e.memset(ident.t[:], 0.0), [], [ident])
        n = ident.t.shape[0]
        self.op("pool", lambda e: e.affine_select(out=ident.t[:], in_=ident.t[:], compare_op=ALU.not_equal,
                                                  fill=1.0, base=0, pattern=[[-1, n]], channel_multiplier=1),
                [ident], [ident])


D = 1024
T = 2048
TC = 256
NTOK = TC + T
NB = 2
NE = 256
CAP = 256
NSLOT = 65536
EPS = 1e-6


def build_nc(dbg=(), stop_after=None, ne_decl=NE):
    nc = bass.Bass("TRN2", target_bir_lowering=False)

    def din(name, shape, dt=F32):
        return nc.dram_tensor(name, list(shape), dt, kind="ExternalInput").ap()

    x2 = din("x2", [NB, T, D]); ctx2 = din("ctx2", [NB, TC, D]); cT = din("cT", [D, 3])
    ada_w = din("ada_w", [D, 6 * D]); ada_bc = din("ada_bc", [128, 48])
    nmix = din("nmix", [128, 8]); nffn = din("nffn", [128, 8]); nfin = din("nfin", [1, D])
    w_in = din("w_in", [D, 3584]); w_out = din("w_out", [D, D])
    convw = din("convw", [128, 16]); convb = din("convb", [128, 4])
    wa_bd = din("wa_bd", [128, 8, 128]); wx_bd = din("wx_bd", [128, 8, 128])
    ba_c = din("ba_c", [128, 8]); bx_c = din("bx_c", [128, 8]); lam_c = din("lam_c", [128, 8])
    lbl = din("lbl", [1, 2048]); hnorm = din("hnorm", [1, 512])
    router_w = din("router_w", [D, NE]); router_b = din("router_b", [1, NE])
    w13 = din("w13", [ne_decl * 128, 4096]); w2 = din("w2", [ne_decl * 128, 2048])
    sw13 = din("sw13", [D, 512]); sw2 = din("sw2", [256, D])
    out = nc.dram_tensor("out", [NB, T, D], F32, kind="ExternalOutput").ap()

    dbg_outs = {}

    with ExitStack() as st:
        S = Sched(nc, st)

        def dump(name, tb, ap, shape, dt=F32):
            if name not in dbg:
                return
            d = nc.dram_tensor("dbg_" + name, list(shape), dt, kind="ExternalOutput").ap()
            S.dma("sp", None, tb, out=d, in_=ap, final=True)
            dbg_outs[name] = (shape, dt)

        x1d = S.dram("x1d", [NB * T, D], F32)
        vsave = S.dram("vsave", [T, 512], BF16); qsave = S.dram("qsave", [T, 512], F32)
        vsave_tb = [TB(vsave.t, "vsave%d" % i) for i in range(16)]; qsave_tb = [TB(qsave.t, "qsave%d" % i) for i in range(16)]
        yshd = S.dram("yshd", [NB * T, D], F32)
        xs_pad = S.dram("xs_pad", [NSLOT, D], BF16)
        ys_pad = S.dram("ys_pad", [NSLOT, D], F32)

        ident = S.sb("ident", [128, 128], BF16); S.make_ident(ident)
        identf = S.sb("identf", [128, 128], F32); S.make_ident(identf)
        ones = S.sb("ones", [128, 128], F32)
        S.op("pool", lambda e: e.memset(ones.t[:], 1.0), [], [ones])

        def tri(name, base, cm, step, dt):
            m = S.sb(name, [128, 128], dt)
            S.op("pool", lambda e: e.memset(m.t[:], 1.0), [], [m])
            S.op("pool", lambda e: e.affine_select(out=m.t[:], in_=m.t[:], compare_op=ALU.is_ge, fill=0.0, base=base,
                                                   pattern=[[step, 128]], channel_multiplier=cm), [m], [m])
            v3 = m.t[:].rearrange("p (b i) -> p b i", i=32)
            S.op("pool", lambda e: e.affine_select(out=v3, in_=v3, compare_op=ALU.is_ge, fill=0.0, base=0,
                                                   pattern=[[-32, 4], [0, 32]], channel_multiplier=1), [m], [m])
            S.op("pool", lambda e: e.affine_select(out=v3, in_=v3, compare_op=ALU.is_ge, fill=0.0, base=31,
                                                   pattern=[[32, 4], [0, 32]], channel_multiplier=-1), [m], [m])
            return m

        M_le = tri("M_le", 0, -1, 1, F32)
        M_ge = tri("M_ge", 0, 1, -1, F32)
        M_gt = tri("M_gt", -1, 1, -1, F32)
        M_lt = tri("M_lt", -1, -1, 1, F32)
        blk1 = S.sb("blk1", [128, 4], F32)
        S.op("pool", lambda e: e.memset(blk1.t[:], 1.0), [], [blk1])
        S.op("pool", lambda e: e.affine_select(out=blk1.t[:], in_=blk1.t[:], compare_op=ALU.is_ge, fill=0.0, base=0,
                                               pattern=[[-32, 4]], channel_multiplier=1), [blk1], [blk1])
        S.op("pool", lambda e: e.affine_select(out=blk1.t[:], in_=blk1.t[:], compare_op=ALU.is_ge, fill=0.0, base=31,
                                               pattern=[[32, 4]], channel_multiplier=-1), [blk1], [blk1])
        mask_f = S.sb("mask_f", [128, 4, 128], F32); mask_b = S.sb("mask_b", [128, 4, 128], F32)
        for h in range(4):
            S.op("pool", lambda e: e.tensor_copy(out=mask_f.t[:, h, :], in_=M_le.t[:]), [M_le], [mask_f])
            S.op("pool", lambda e: e.tensor_copy(out=mask_b.t[:, h, :], in_=M_ge.t[:]), [M_ge], [mask_b])

        cmask = S.sb("cmask", [128, 4, 4, 128], BF16)
        S.op("pool", lambda e: e.memset(cmask.t[:], 0.0), [], [cmask])
        for c in range(4):
            S.op("pool", lambda e: e.memset(cmask.t[:, c, :, 32 * c:32 * c + 32], 1.0), [cmask], [cmask])
        def load(name, shape, src, q="sp", dt=F32):
            t = S.sb(name, shape, dt)
            S.dma(q, t, None, out=t.t[:], in_=src)
            return t

        cTs = load("cTs", [128, 8, 3], cT.rearrange("(k p) j -> p k j", p=128))
        adab = load("adab", [128, 48], ada_bc)
        nmix_s = load("nmix_s", [128, 8], nmix); nffn_s = load("nffn_s", [128, 8], nffn)
        nfin_b = load("nfin_b", [128, D], nfin.partition_broadcast(128))
        convw_s = load("convw_s", [128, 16], convw); convb_s = load("convb_s", [128, 4], convb)
        wabd = load("wabd", [128, 8, 128], wa_bd, q="pool", dt=BF16)
        wxbd = load("wxbd", [128, 8, 128], wx_bd, q="pool", dt=BF16)
        ba_s = load("ba_s", [128, 8], ba_c); bx_s = load("bx_s", [128, 8], bx_c); lam_s = load("lam_s", [128, 8], lam_c)
        hn_b = load("hn_b", [128, 512], hnorm.partition_broadcast(128))
        rb_b = load("rb_b", [128, NE], router_b.partition_broadcast(128))

        lbB = S.sb("lbB", [128, 2, 512], F32); omlB = S.sb("omlB", [128, 2, 512], F32)
        with S.scope() as s0:
            lbl_b = S.sb("lbl_b", [128, 2048], F32, s0)
            S.dma("sp", lbl_b, None, out=lbl_b.t[:], in_=lbl.partition_broadcast(128))
            lv = lbl_b.t[:].rearrange("p (d s c) -> p d s c", d=2, s=2)
            for d in range(2):
                S.op("dve", lambda e: e.tensor_tensor(out=lbB.t[:, d, :], in0=lv[:, d, 0, :], in1=lv[:, d, 1, :], op=ALU.subtract), [lbl_b], [lbB])
        S.op("act", lambda e: e.activation(out=lbB.t[:], in_=lbB.t[:], func=AF.Sigmoid), [lbB], [lbB])
        S.op("dve", lambda e: e.tensor_scalar(out=omlB.t[:], in0=lbB.t[:], scalar1=-1.0, scalar2=1.0, op0=ALU.mult, op1=ALU.add), [lbB], [omlB])

        c8 = S.sb("c8", [128, 8], F32); spt = S.sb("spt", [128, 8], F32)
        S.op("dve", lambda e: e.tensor_scalar(out=spt.t[:], in0=lam_s.t[:], scalar1=-1.0, scalar2=None, op0=ALU.mult), [lam_s], [spt])
        S.op("dve", lambda e: e.tensor_tensor(out=spt.t[:], in0=spt.t[:], in1=lam_s.t[:], op=ALU.max), [lam_s, spt], [spt])
        S.op("act", lambda e: e.activation(out=spt.t[:], in_=spt.t[:], func=AF.Exp, scale=-1.0), [spt], [spt])
        S.op("act", lambda e: e.activation(out=spt.t[:], in_=spt.t[:], func=AF.Ln, bias=1.0), [spt], [spt])
        S.op("dve", lambda e: e.tensor_scalar(out=c8.t[:], in0=lam_s.t[:], scalar1=-1.0, scalar2=0.0, op0=ALU.mult, op1=ALU.max), [lam_s], [c8])
        S.op("dve", lambda e: e.tensor_tensor(out=c8.t[:], in0=c8.t[:], in1=spt.t[:], op=ALU.add), [c8, spt], [c8])
        S.op("dve", lambda e: e.tensor_scalar(out=c8.t[:], in0=c8.t[:], scalar1=-8.0, scalar2=None, op0=ALU.mult), [c8], [c8])
        dump("c8", c8, c8.t[:], [128, 8])

        modc = S.sb("modc", [128, 48, 3], F32)
        scT = S.sb("scT", [128, 8, 3], F32)
        S.op("act", lambda e: e.activation(out=scT.t[:], in_=cTs.t[:], func=AF.Silu), [cTs], [scT])
        with S.scope() as sA:
            aw = [S.sb("aw%d" % i, [128, 8, 1024], F32, sA) for i in range(2)]
            pm = S.ps("pm", [128, 512], F32, sA)
            for g in range(6):
                t = aw[g % 2]
                S.dma("sp", t, None, out=t.t[:], in_=ada_w[:, g * 1024:(g + 1) * 1024].rearrange("(k p) n -> p k n", p=128))
                for fc in range(8):
                    f = g * 8 + fc
                    for k in range(8):
                        S.op("pe", lambda e: e.matmul(pm.t[:, f * 3:(f + 1) * 3], lhsT=t.t[:, k, fc * 128:(fc + 1) * 128],
                                                      rhs=scT.t[:, k, :], start=(k == 0), stop=(k == 7)), [t, scT], [pm])
            pmv = pm.t[:, 0:144].rearrange("p (f j) -> p f j", j=3)
            for j in range(3):
                S.op("dve", lambda e: e.tensor_tensor(out=modc.t[:, :, j], in0=pmv[:, :, j], in1=adab.t[:], op=ALU.add), [pm, adab], [modc])
        dump("modc", modc, modc.t[:], [128, 48, 3])
        gm1 = S.sb("gm1", [128, 8, 3], F32); gm2 = S.sb("gm2", [128, 8, 3], F32)
        for j in range(3):
            S.op("dve", lambda e: e.scalar_tensor_tensor(out=gm1.t[:, :, j], in0=modc.t[:, 8:16, j], scalar=1.0, in1=nmix_s.t[:], op0=ALU.add, op1=ALU.mult), [modc, nmix_s], [gm1])
            S.op("dve", lambda e: e.scalar_tensor_tensor(out=gm2.t[:, :, j], in0=modc.t[:, 32:40, j], scalar=1.0, in1=nffn_s.t[:], op0=ALU.add, op1=ALU.mult), [modc, nffn_s], [gm2])

        def row_bcast(dst, col_ap_fn, src_tb, pbank):
            tmp = rb_tmp
            for k in range(8):
                S.op("dve", lambda e: e.tensor_scalar(out=tmp.t[:], in0=ones.t[:], scalar1=col_ap_fn(k), scalar2=None, op0=ALU.mult), [ones, src_tb], [tmp])
                S.op("pe", lambda e: e.transpose(out=pbank.t[:, (k % 4) * 128:(k % 4 + 1) * 128], in_=tmp.t[:], identity=identf.t[:]), [tmp, identf], [pbank])
                if k % 4 == 3:
                    S.op("act", lambda e: e.activation(out=dst.t[:, (k - 3) * 128:(k + 1) * 128], in_=pbank.t[:], func=AF.Copy), [pbank], [dst])

        rb_tmp = S.sb("rb_tmp", [128, 128], F32)

        if stop_after == "A":
            S.finish()
            return nc, dbg_outs
        iota_f = S.sb("iota_f", [128, NE], F32); ebase = None
        with S.scope() as s1:
            iota_i = S.sb("iota_i", [128, NE], I32, s1)
            S.op("pool", lambda e: e.iota(iota_i.t[:], pattern=[[1, NE]], base=0, channel_multiplier=0), [], [iota_i])
            S.op("dve", lambda e: e.tensor_copy(out=iota_f.t[:], in_=iota_i.t[:]), [iota_i], [iota_f])
        Lst = S.sb("Lst", [128, 128], F32)
        S.op("pool", lambda e: e.memset(Lst.t[:], 1.0), [], [Lst])
        S.op("pool", lambda e: e.affine_select(out=Lst.t[:], in_=Lst.t[:], compare_op=ALU.is_ge, fill=0.0, base=-1,
                                               pattern=[[1, 128]], channel_multiplier=-1), [Lst], [Lst])
        carry = S.sb("carry", [128, NE], F32)
        S.op("pool", lambda e: e.memset(carry.t[:], 0.0), [], [carry])
        eidx_all = S.sb("eidx_all", [128, 256], F32); pos_all = S.sb("pos_all", [128, 256], F32)
        hx2d = S.dram("hx2d", [NB * T, D], BF16)
        gates_all = S.sb("gates_all", [128, 32, 8], F32)

        with S.scope() as sPB:
            hxT = S.sb("hxT", [128, 8, NTOK], BF16, sPB)
            yT = S.sb("yT", [128, 8, T], BF16, sPB)
            pT = [S.ps("pT%d" % i, [128, 8, 128], BF16, sPB) for i in range(2)]
            pbk = [S.ps("pbk%d" % i, [128, 512], F32, sPB) for i in range(4)]
            pbc = S.ps("pbc", [128, 512], F32, sPB)

            nb_xt = [S.sb("nb_xt%d" % i, [128, D], F32, sPB) for i in range(2)]
            nb_parts = [[TB(nb_xt[i].t, "nb_xt%d_p%d" % (i, j)) for j in range(4)] for i in range(2)]
            nb_xn = [S.sb("nb_xn%d" % i, [128, D], BF16, sPB) for i in range(2)]
            nb_st = [S.sb("nb_st%d" % i, [128, 4], F32, sPB) for i in range(2)]
            nb_junk = S.sb("nb_junk", [128, D], BF16, sPB)
            nb_tmp = S.sb("nb_tmp", [128, D], F32, sPB)

            def rms_rows(x_tb_list, x_ap, st_, width=D):
                S.op("dve", lambda e: e.memset(st_.t[:, 0:1], 0.0), [], [st_])
                S.op("act", lambda e: e.activation(out=nb_junk.t[:, 0:width], in_=x_ap, func=AF.Square, accum_out=st_.t[:, 0:1]), x_tb_list, [st_])
                S.op("dve", lambda e: e.tensor_scalar(out=st_.t[:, 1:2], in0=st_.t[:, 0:1], scalar1=1.0 / width, scalar2=EPS, op0=ALU.mult, op1=ALU.add), [st_], [st_])
                S.op("act", lambda e: e.activation(out=st_.t[:, 1:2], in_=st_.t[:, 1:2], func=AF.Sqrt), [st_], [st_])
                S.op("dve", lambda e: e.reciprocal(out=st_.t[:, 2:3], in_=st_.t[:, 1:2]), [st_], [st_])

            def norm_T(ti, srcs, gmb, shb, dst_tb, dst_ap):
                xt = nb_xt[ti % 2]; parts = nb_parts[ti % 2]; xn = nb_xn[ti % 2]; st_ = nb_st[ti % 2]; p = pT[ti % 2]
                for j, (psl, ap) in enumerate(srcs):
                    S.dma("sp", parts[j], None, out=xt.t[psl, :], in_=ap)
                rms_rows(parts, xt.t[:], st_)
                S.op("dve", lambda e: e.scalar_tensor_tensor(out=nb_tmp.t[:], in0=xt.t[:], scalar=st_.t[:, 2:3], in1=gmb.t[:], op0=ALU.mult, op1=ALU.mult), parts + [st_, gmb], [nb_tmp])
                S.op("dve", lambda e: e.tensor_tensor(out=xn.t[:], in0=nb_tmp.t[:], in1=shb.t[:], op=ALU.add), [nb_tmp, shb], [xn])
                def stage2():
                    for k in range(8):
                        S.op("pe", lambda e: e.transpose(out=p.t[:, k, :], in_=xn.t[:, k * 128:(k + 1) * 128], identity=ident.t[:]), [xn, ident], [p])
                    S.op("act", lambda e: e.activation(out=dst_ap, in_=p.t[:], func=AF.Copy), [p], [dst_tb], cols=[dst_tb])
                return stage2

            def norm_loop(items):
                pend = None
                for args in items:
                    n2 = norm_T(*args)
                    if pend is not None:
                        pend()
                    pend = n2
                if pend is not None:
                    pend()


            for b in range(NB):
                with S.scope() as sB:
                    with S.scope() as sN1:
                        gm1c_b = S.sb("gm1c_b%d" % b, [128, D], F32, sN1); sh1c_b = S.sb("sh1c_b%d" % b, [128, D], F32, sN1)
                        row_bcast(gm1c_b, lambda k: gm1.t[:, k, 2:3], gm1, pbc)
                        row_bcast(sh1c_b, lambda k: modc.t[:, k, 2:3], modc, pbc)
                        gm1x_b = S.sb("gm1x_b%d" % b, [128, D], F32, sN1); sh1x_b = S.sb("sh1x_b%d" % b, [128, D], F32, sN1)
                        row_bcast(gm1x_b, lambda k: gm1.t[:, k, b:b + 1], gm1, pbc)
                        row_bcast(sh1x_b, lambda k: modc.t[:, k, b:b + 1], modc, pbc)
                        items = [(i, [(slice(0, 128), ctx2[b, i * 128:(i + 1) * 128, :])], gm1c_b, sh1c_b, hxT, hxT.t[:, :, i * 128:(i + 1) * 128]) for i in range(2)]
                        items += [(i, [(slice(0, 128), x2[b, i * 128:(i + 1) * 128, :])], gm1x_b, sh1x_b, hxT, hxT.t[:, :, TC + i * 128:TC + (i + 1) * 128]) for i in range(16)]
                        norm_loop(items)
                    if b == 0:
                        dump("hxT", hxT, hxT.t[:], [128, 8, NTOK], BF16)

                    with S.scope() as sL:
                        wl = S.sb("wl%d" % b, [128, 8, 1024], BF16, sL)
                        S.dma("pool", wl, None, out=wl.t[:], in_=w_in[:, 0:1024].rearrange("(k p) n -> p k n", p=128))
                        rx = S.sb("l_rx%d" % b, [128, NTOK], F32, sL); u = S.sb("l_u%d" % b, [128, NTOK], F32, sL)
                        ubf = S.sb("l_ubf%d" % b, [128, NTOK], BF16, sL); gg = S.sb("l_gg%d" % b, [128, T], BF16, sL)
                        R = S.sb("l_R%d" % b, [128, NTOK], F32, sL); I_ = S.sb("l_I%d" % b, [128, NTOK], F32, sL)
                        H = S.sb("l_H%d" % b, [128, T], F32, sL); Hc = S.sb("l_Hc%d" % b, [128, TC], F32, sL)
                        gt = S.sb("l_gt%d" % b, [128, 512], F32, sL)
                        blocks = [(0, 256)] + [(TC + i * 512, 512) for i in range(4)]
                        for c in range(4):
                            for bi, (t0, n) in enumerate(blocks):
                                pp = pbk[bi % 4]
                                for k in range(8):
                                    S.op("pe", lambda e: e.matmul(pp.t[:, 0:n], lhsT=wl.t[:, k, c * 128:(c + 1) * 128], rhs=hxT.t[:, k, t0:t0 + n], start=(k == 0), stop=(k == 7)), [wl, hxT], [pp])
                                S.op("act", lambda e: e.activation(out=rx.t[:, t0:t0 + n], in_=pp.t[:, 0:n], func=AF.Copy), [pp], [rx])
                            for bi in range(4):
                                t0 = TC + bi * 512; pp = pbk[bi % 4]
                                for k in range(8):
                                    S.op("pe", lambda e: e.matmul(pp.t[:], lhsT=wl.t[:, k, 512 + c * 128:512 + (c + 1) * 128], rhs=hxT.t[:, k, t0:t0 + 512], start=(k == 0), stop=(k == 7)), [wl, hxT], [pp])
                                S.op("act", lambda e: e.activation(out=gt.t[:], in_=pp.t[:], func=AF.Square), [pp], [gt])
                                S.op("dve", lambda e: e.tensor_scalar(out=gt.t[:], in0=gt.t[:], scalar1=0.044715, scalar2=1.0, op0=ALU.mult, op1=ALU.add), [gt], [gt])
                                S.op("dve", lambda e: e.tensor_tensor(out=gt.t[:], in0=gt.t[:], in1=pp.t[:], op=ALU.mult), [gt, pp], [gt])
                                S.op("act", lambda e: e.activation(out=gt.t[:], in_=gt.t[:], func=AF.Sigmoid, scale=1.5957691216), [gt], [gt])
                                S.op("dve", lambda e: e.tensor_tensor(out=gg.t[:, bi * 512:(bi + 1) * 512], in0=gt.t[:], in1=pp.t[:], op=ALU.mult), [gt, pp], [gg])
                            cw = lambda tap: convw_s.t[:, c * 4 + tap:c * 4 + tap + 1]
                            for (s0, n) in ((0, TC), (TC, T)):
                                S.op("dve", lambda e: e.tensor_scalar(out=u.t[:, s0:s0 + n], in0=rx.t[:, s0:s0 + n], scalar1=cw(1), scalar2=convb_s.t[:, c:c + 1], op0=ALU.mult, op1=ALU.add), [rx, convw_s, convb_s], [u])
                                S.op("dve", lambda e: e.scalar_tensor_tensor(out=u.t[:, s0 + 1:s0 + n], in0=rx.t[:, s0:s0 + n - 1], scalar=cw(0), in1=u.t[:, s0 + 1:s0 + n], op0=ALU.mult, op1=ALU.add), [rx, u, convw_s], [u])
                                S.op("dve", lambda e: e.scalar_tensor_tensor(out=u.t[:, s0:s0 + n - 1], in0=rx.t[:, s0 + 1:s0 + n], scalar=cw(2), in1=u.t[:, s0:s0 + n - 1], op0=ALU.mult, op1=ALU.add), [rx, u, convw_s], [u])
                                S.op("dve", lambda e: e.scalar_tensor_tensor(out=u.t[:, s0:s0 + n - 2], in0=rx.t[:, s0 + 2:s0 + n], scalar=cw(3), in1=u.t[:, s0:s0 + n - 2], op0=ALU.mult, op1=ALU.add), [rx, u, convw_s], [u])
                            S.op("act", lambda e: e.activation(out=ubf.t[:], in_=u.t[:], func=AF.Copy), [u], [ubf])
                            if b == 0 and c == 0:
                                dump("lru_u", u, u.t[:], [128, NTOK])
                            for d in range(2):
                                dc = d * 4 + c
                                for bi, (t0, n) in enumerate(blocks):
                                    pa = pbk[(2 * bi) % 4]; px = pbk[(2 * bi + 1) % 4]
                                    S.op("pe", lambda e: e.matmul(pa.t[:, 0:n], lhsT=wabd.t[:, dc, :], rhs=ubf.t[:, t0:t0 + n], start=True, stop=True), [wabd, ubf], [pa])
                                    S.op("pe", lambda e: e.matmul(px.t[:, 0:n], lhsT=wxbd.t[:, dc, :], rhs=ubf.t[:, t0:t0 + n], start=True, stop=True), [wxbd, ubf], [px])
                                    S.op("act", lambda e: e.activation(out=R.t[:, t0:t0 + n], in_=pa.t[:, 0:n], func=AF.Sigmoid, bias=ba_s.t[:, dc:dc + 1]), [pa, ba_s], [R])
                                    S.op("act", lambda e: e.activation(out=I_.t[:, t0:t0 + n], in_=px.t[:, 0:n], func=AF.Sigmoid, bias=bx_s.t[:, dc:dc + 1]), [px, bx_s], [I_])
                                S.op("act", lambda e: e.activation(out=R.t[:], in_=R.t[:], func=AF.Exp, scale=c8.t[:, dc:dc + 1]), [R, c8], [R])
                                S.op("dve", lambda e: e.tensor_tensor(out=rx.t[:], in0=R.t[:], in1=R.t[:], op=ALU.mult), [R], [rx])
                                S.op("act", lambda e: e.activation(out=rx.t[:], in_=rx.t[:], func=AF.Sqrt, scale=-1.0, bias=1.0), [rx], [rx])
                                S.op("dve", lambda e: e.tensor_tensor(out=I_.t[:], in0=I_.t[:], in1=rx.t[:], op=ALU.mult), [I_, rx], [I_])
                                S.op("dve", lambda e: e.tensor_tensor(out=I_.t[:], in0=I_.t[:], in1=u.t[:], op=ALU.mult), [I_, u], [I_])
                                A_c = R.t[:, 0:TC]; B_c = I_.t[:, 0:TC]; A_l = R.t[:, TC:NTOK]; B_l = I_.t[:, TC:NTOK]
                                if d == 0:
                                    S.op("dve", lambda e: e.tensor_tensor_scan(out=Hc.t[:], data0=A_c, data1=B_c, initial=0.0, op0=ALU.mult, op1=ALU.add), [R, I_], [Hc])
                                    S.op("dve", lambda e: e.tensor_tensor_scan(out=H.t[:], data0=A_l, data1=B_l, initial=Hc.t[:, TC - 1:TC], op0=ALU.mult, op1=ALU.add), [R, I_, Hc], [H])
                                else:
                                    S.op("dve", lambda e: e.tensor_tensor_scan(out=Hc.t[:][:, ::-1], data0=A_c[:, ::-1], data1=B_c[:, ::-1], initial=0.0, op0=ALU.mult, op1=ALU.add), [R, I_], [Hc])
                                    hb = rx.t[:, 0:T]
                                    S.op("dve", lambda e: e.tensor_tensor_scan(out=hb[:, ::-1], data0=A_l[:, ::-1], data1=B_l[:, ::-1], initial=Hc.t[:, 0:1], op0=ALU.mult, op1=ALU.add), [R, I_, Hc], [rx])
                                    S.op("dve", lambda e: e.tensor_tensor(out=H.t[:], in0=H.t[:], in1=hb, op=ALU.add), [H, rx], [H])
                            if b == 0 and c == 0:
                                dump("lru_H", H, H.t[:], [128, T])
                            S.op("dve", lambda e: e.tensor_tensor(out=yT.t[:, c, :], in0=H.t[:], in1=gg.t[:], op=ALU.mult), [H, gg], [yT])
                    if b == 0:
                        dump("yT_lru", yT, yT.t[:, 0:4, :], [128, 4, T], BF16)
                    if stop_after == "L":
                        S.finish()
                        return nc, dbg_outs
                    with S.scope() as sN2:
                        gm1x_b = S.sb("gm1xc_b%d" % b, [128, D], F32, sN2); sh1x_b = S.sb("sh1xc_b%d" % b, [128, D], F32, sN2)
                        row_bcast(gm1x_b, lambda k: gm1.t[:, k, b:b + 1], gm1, pbc)
                        row_bcast(sh1x_b, lambda k: modc.t[:, k, b:b + 1], modc, pbc)
                        xcm = x2[b].rearrange("(r c) d -> c r d", c=64)
                        items = []
                        for i in range(16):
                            srcs = [(slice(cl * 32, (cl + 1) * 32), xcm[4 * i + cl]) for cl in range(4)]
                            items.append((i, srcs, gm1x_b, sh1x_b, hxT, hxT.t[:, :, TC + i * 128:TC + (i + 1) * 128]))
                        norm_loop(items)
                    if stop_after == "CM":
                        dump("hxT_cm", hxT, hxT.t[:], [128, 8, NTOK], BF16)
                        S.finish()
                        return nc, dbg_outs
                    with S.scope() as sH:
                        pb8 = S.ps("pb8_%d" % b, [128, 512], F32, sH)
                        whg = S.sb("whg%d" % b, [128, 8, 2048], BF16, sH)
                        whs = [TB(whg.t, "whg%d_s%d" % (b, i)) for i in range(4)]
                        WCOL = {"q": 1024, "v": 1536, "ff": 2048, "fb": 2560, "g": 3072}

                        def wload(slot, name):
                            c0 = WCOL[name]
                            S.dma("pool", whs[slot], None, out=whg.t[:, :, slot * 512:(slot + 1) * 512], in_=w_in[:, c0:c0 + 512].rearrange("(k p) n -> p k n", p=128))

                        wload(0, "q"); wload(1, "v"); wload(2, "ff"); wload(3, "fb")
                        of = S.sb("h_of%d" % b, [128, 16, 512], BF16, sH)
                        Sst = [S.sb("h_S%d_%d" % (b, d), [128, 512], F32, sH) for d in range(2)]
                        Sbf = S.sb("h_Sbf%d" % b, [128, 512], BF16, sH)
                        t_s = S.sb("h_ts%d" % b, [128, 512], F32, sH); t_lf = S.sb("h_tlf%d" % b, [128, 512], F32, sH)
                        t_kf = S.sb("h_tkf%d" % b, [128, 512], F32, sH); t_e = S.sb("h_te%d" % b, [128, 512], F32, sH)
                        dec = S.sb("h_dec%d" % b, [128, 16], F32, sH)
                        kkc4 = [S.sb("h_kkc%d_%d" % (b, cc), [128, 512], BF16, sH) for cc in range(4)]
                        kkc4_src = [None, None]
                        hsl = lambda h: slice(h * 128, (h + 1) * 128)

                        def proj_tok(pp, tok0, slot):
                            for k in range(8):
                                S.op("pe", lambda e: e.matmul(pp.t[:], lhsT=hxT.t[:, k, tok0:tok0 + 128], rhs=whg.t[:, k, slot * 512:(slot + 1) * 512], start=(k == 0), stop=(k == 7)), [hxT, whs[slot]], [pp])

                        def gates(tok0, d, slot, kk_out, dec_out, latent):
                            pz = pbk[0]
                            proj_tok(pz, tok0, slot)
                            S.op("act", lambda e: e.activation(out=t_s.t[:], in_=pz.t[:], func=AF.Sigmoid), [pz], [t_s])
                            S.op("dve", lambda e: e.tensor_tensor(out=t_s.t[:], in0=t_s.t[:], in1=omlB.t[:, d, :], op=ALU.mult), [t_s, omlB], [t_s])
                            S.op("dve", lambda e: e.tensor_tensor(out=t_s.t[:], in0=t_s.t[:], in1=lbB.t[:, d, :], op=ALU.add), [t_s, lbB], [t_s])
                            S.op("act", lambda e: e.activation(out=t_lf.t[:], in_=t_s.t[:], func=AF.Ln), [t_s], [t_lf])
                            S.op("dve", lambda e: e.tensor_scalar(out=t_kf.t[:], in0=t_s.t[:], scalar1=-1.0, scalar2=1.0, op0=ALU.mult, op1=ALU.add), [t_s], [t_kf])
                            prg = pbk[1]
                            Mr = M_gt if d == 0 else M_lt
                            S.op("pe", lambda e: e.matmul(prg.t[:], lhsT=Mr.t[:], rhs=t_lf.t[:], start=True, stop=True), [Mr, t_lf], [prg])
                            S.op("act", lambda e: e.activation(out=t_e.t[:], in_=prg.t[:], func=AF.Exp), [prg], [t_e])
                            S.op("dve", lambda e: e.tensor_tensor(out=kk_out.t[:], in0=t_kf.t[:], in1=t_e.t[:], op=ALU.mult), [t_kf, t_e], [kk_out])
                            for h in range(4):
                                S.op("pe", lambda e: e.matmul(pbc.t[:, h * 4:(h + 1) * 4], lhsT=t_lf.t[:, hsl(h)], rhs=blk1.t[:], start=True, stop=True), [t_lf, blk1], [pbc])
                            S.op("act", lambda e: e.activation(out=dec_out.t[:], in_=pbc.t[:, 0:16], func=AF.Exp), [pbc], [dec_out])
                            if latent:
                                pg = pbk[1]
                                Mg = M_le if d == 0 else M_ge
                                S.op("pe", lambda e: e.matmul(pg.t[:], lhsT=Mg.t[:], rhs=t_lf.t[:], start=True, stop=True), [Mg, t_lf], [pg])
                                S.op("act", lambda e: e.activation(out=t_e2.t[:], in_=pg.t[:], func=AF.Exp), [pg], [t_e2])
                                S.op("dve", lambda e: e.tensor_tensor(out=qd.t[:], in0=qf.t[:], in1=t_e2.t[:], op=ALU.mult), [qf, t_e2], [qd])
                                S.op("act", lambda e: e.activation(out=t_e.t[:], in_=pg.t[:], func=AF.Exp, scale=-1.0), [pg], [t_e])
                                S.op("dve", lambda e: e.tensor_tensor(out=kd.t[:], in0=t_kf.t[:], in1=t_e.t[:], op=ALU.mult), [t_kf, t_e], [kd])

                        def state_step(d, c, kk_tb, v_tb, dec_tb):
                            pkv = pb8
                            kc_ = kk_tb
                            for h in range(4):
                                S.op("pe", lambda e: e.matmul(pkv.t[:, hsl(h)], lhsT=kc_.t[:, hsl(h)], rhs=v_tb.t[:, hsl(h)], start=True, stop=True), [kc_, v_tb], [pkv])
                            for h in range(4):
                                S.op("dve", lambda e: e.scalar_tensor_tensor(out=Sst[d].t[:, hsl(h)], in0=Sst[d].t[:, hsl(h)], scalar=dec_tb.t[:, h * 4 + c:h * 4 + c + 1], in1=pkv.t[:, hsl(h)], op0=ALU.mult, op1=ALU.add), [Sst[d], dec_tb, pkv], [Sst[d]], cols=[Sst[d]])
                            S.op("act", lambda e: e.activation(out=Sbf.t[:], in_=Sst[d].t[:], func=AF.Copy), [Sst[d]], [Sbf])

                        sC_cm = S.scope(); sC = sC_cm.__enter__()
                        c_v = [S.sb("h_cv%d_%d" % (b, t), [128, 512], BF16, sC) for t in range(2)]
                        c_kk = [[S.sb("h_ckk%d_%d_%d" % (b, d, t), [128, 512], BF16, sC) for t in range(2)] for d in range(2)]
                        c_dec = [[S.sb("h_cdec%d_%d_%d" % (b, d, t), [128, 16], F32, sC) for t in range(2)] for d in range(2)]
                        for d in range(2):
                            S.op("pool", lambda e: e.memset(Sst[d].t[:], 0.0), [], [Sst[d]])
                        for t in range(2):
                            pv = pbk[3]
                            proj_tok(pv, t * 128, 1)
                            S.op("act", lambda e: e.activation(out=c_v[t].t[:], in_=pv.t[:], func=AF.Copy), [pv], [c_v[t]])
                            for d in range(2):
                                gates(t * 128, d, 2 + d, c_kk[d][t], c_dec[d][t], False)
                        for d in range(2):
                            order = [(t, c) for t in range(2) for c in range(4)]
                            if d == 1:
                                order = order[::-1]
                            for (t, c) in order:
                                S.op("dve", lambda e: e.tensor_scalar(out=kkc4[c].t[:], in0=c_kk[d][t].t[:], scalar1=blk1.t[:, c:c + 1], scalar2=None, op0=ALU.mult), [c_kk[d][t], blk1], [kkc4[c]])
                                state_step(d, c, kkc4[c], c_v[t], c_dec[d][t])
                        if b == 0:
                            dump("hg_sc0", Sst[0], Sst[0].t[:], [128, 512]); dump("hg_sc1", Sst[1], Sst[1].t[:], [128, 512])

                        if stop_after == "CTX":
                            S.finish()
                            return nc, dbg_outs
                        sC_cm.__exit__(None, None, None)
                        vt = S.sb("h_vt%d" % b, [128, 512], BF16, sH); qf = S.sb("h_qf%d" % b, [128, 512], F32, sH)
                        t_e2 = S.sb("h_te2%d" % b, [128, 512], F32, sH)
                        qd = S.sb("h_qd%d" % b, [128, 512], BF16, sH); kd = S.sb("h_kd%d" % b, [128, 512], BF16, sH)
                        qdT = S.sb("h_qdT%d" % b, [128, 4, 128], BF16, sH); kdT = S.sb("h_kdT%d" % b, [128, 4, 128], BF16, sH)
                        sTm = S.sb("h_sTm%d" % b, [128, 512], BF16, sH)
                        qdTc = S.sb("h_qdTc%d" % b, [128, 4, 4, 128], BF16, sH)
                        t_hg = TB(nb_tmp.t[:, 0:512], "h_hg%d" % b); sg = TB(nb_tmp.t[:, 512:1024], "h_sg%d" % b)
                        ybf = S.sb("h_ybf%d" % b, [128, 512], BF16, sH); st4 = S.sb("h_st4%d" % b, [128, 12], F32, sH)
                        kk = S.sb("h_kk%d" % b, [128, 512], BF16, sH)
                        dec_b = S.sb("h_dec2_%d" % b, [128, 16], F32, sH)
                        a0 = nb_xt[0].t[:].bitcast(BF16); a1 = nb_xt[1].t[:].bitcast(BF16); a2 = nb_xn[0].t[:]
                        vt_s = [vt, TB(a2[:, 0:512], "h_vt2_%d" % b)]
                        sTm_s = [sTm, TB(a2[:, 512:1024], "h_sTm2_%d" % b)]
                        dec_s = [dec, dec_b]
                        qdTc_s = [qdTc, TB(a0.rearrange("p (c h i) -> p c h i", c=4, h=4), "h_qdTc2_%d" % b)]
                        kkc4_s = [kkc4, [TB(a1[:, cc * 512:(cc + 1) * 512], "h_kkc2_%d_%d" % (b, cc)) for cc in range(4)]]

                        def prep_gen(d, t, B):
                            tok0 = TC + t * 128
                            vt_ = vt_s[B]; sTm_ = sTm_s[B]; dec_ = dec_s[B]; qdTc_ = qdTc_s[B]; kkc_ = kkc4_s[B]
                            pv = pbk[3]
                            if d == 0:
                                proj_tok(pv, tok0, 1)
                                S.op("act", lambda e: e.activation(out=vt_.t[:], in_=pv.t[:], func=AF.Copy), [pv], [vt_])
                                proj_tok(pv, tok0, 0)
                                S.op("act", lambda e: e.activation(out=qf.t[:], in_=pv.t[:], func=AF.Copy), [pv], [qf])
                                S.dma("sp", vsave_tb[t], vt_, out=vsave.t.ap()[t * 128:(t + 1) * 128, :], in_=vt_.t[:])
                                S.dma("sp", qsave_tb[t], qf, out=qsave.t.ap()[t * 128:(t + 1) * 128, :], in_=qf.t[:])
                            else:
                                S.dma("sp", vt_, vsave_tb[t], out=vt_.t[:], in_=vsave.t.ap()[t * 128:(t + 1) * 128, :])
                                S.dma("sp", qf, qsave_tb[t], out=qf.t[:], in_=qsave.t.ap()[t * 128:(t + 1) * 128, :])
                            yield
                            gates(tok0, d, 2, kk, dec_, True)
                            for cc in range(4):
                                S.op("dve", lambda e: e.tensor_scalar(out=kkc_[cc].t[:], in0=kk.t[:], scalar1=blk1.t[:, cc:cc + 1], scalar2=None, op0=ALU.mult), [kk, blk1], [kkc_[cc]])
                            yield
                            pq = pT[0]
                            for h in range(4):
                                S.op("pe", lambda e: e.transpose(out=pq.t[:, h, :], in_=qd.t[:, hsl(h)], identity=ident.t[:]), [qd, ident], [pq])
                                S.op("pe", lambda e: e.transpose(out=pq.t[:, 4 + h, :], in_=kd.t[:, hsl(h)], identity=ident.t[:]), [kd, ident], [pq])
                            S.op("act", lambda e: e.activation(out=qdT.t[:], in_=pq.t[:, 0:4, :], func=AF.Copy), [pq], [qdT])
                            S.op("act", lambda e: e.activation(out=kdT.t[:], in_=pq.t[:, 4:8, :], func=AF.Copy), [pq], [kdT])
                            yield
                            ps_ = pbk[0]
                            for h in range(4):
                                S.op("pe", lambda e: e.matmul(ps_.t[:, hsl(h)], lhsT=kdT.t[:, h, :], rhs=qdT.t[:, h, :], start=True, stop=True), [kdT, qdT], [ps_])
                            msk = mask_f if d == 0 else mask_b
                            S.op("dve", lambda e: e.tensor_tensor(out=sTm_.t[:], in0=ps_.t[:], in1=msk.t[:].rearrange("p h i -> p (h i)"), op=ALU.mult), [ps_, msk], [sTm_])
                            for c in range(4):
                                S.op("dve", lambda e: e.tensor_tensor(out=qdTc_.t[:, c, :, :], in0=qdT.t[:], in1=cmask.t[:, c, :, :], op=ALU.mult), [qdT, cmask], [qdTc_])
                            yield

                        def recur_gen(d, t, B):
                            tok0 = TC + t * 128
                            vt_ = vt_s[B]; sTm_ = sTm_s[B]; dec_ = dec_s[B]; qdTc_ = qdTc_s[B]; kkc_ = kkc4_s[B]
                            po = pbk[2]
                            for h in range(4):
                                S.op("pe", lambda e: e.matmul(po.t[:, hsl(h)], lhsT=sTm_.t[:, hsl(h)], rhs=vt_.t[:, hsl(h)], start=(h == 0), stop=False), [sTm_, vt_], [po])
                            cs = range(4) if d == 0 else range(3, -1, -1)
                            for c in cs:
                                for h in range(4):
                                    S.op("pe", lambda e: e.matmul(po.t[:, hsl(h)], lhsT=qdTc_.t[:, c, h, :], rhs=Sbf.t[:, hsl(h)], start=False, stop=True), [qdTc_, Sbf], [po])
                                state_step(d, c, kkc_[c], vt_, dec_)
                                yield
                            if d == 0:
                                S.op("act", lambda e: e.activation(out=of.t[:, t, :], in_=po.t[:], func=AF.Copy), [po], [of])
                                return
                            S.op("dve", lambda e: e.tensor_tensor(out=t_hg.t[:], in0=po.t[:], in1=of.t[:, t, :], op=ALU.add), [po, of], [t_hg])
                            if b == 0 and t == 3:
                                dump("hg_t3", t_hg, t_hg.t[:], [128, 512])
                            S.op("dve", lambda e: e.memset(st4.t[:], 0.0), [], [st4])
                            for h in range(4):
                                S.op("act", lambda e: e.activation(out=nb_junk.t[:, 0:128], in_=t_hg.t[:, hsl(h)], func=AF.Square, accum_out=st4.t[:, h:h + 1]), [t_hg], [st4], cols=[st4])
                            S.op("dve", lambda e: e.tensor_scalar(out=st4.t[:, 4:8], in0=st4.t[:, 0:4], scalar1=1.0 / 128, scalar2=EPS, op0=ALU.mult, op1=ALU.add), [st4], [st4])
                            S.op("act", lambda e: e.activation(out=st4.t[:, 4:8], in_=st4.t[:, 4:8], func=AF.Sqrt), [st4], [st4])
                            S.op("dve", lambda e: e.reciprocal(out=st4.t[:, 8:12], in_=st4.t[:, 4:8]), [st4], [st4])
                            yield
                            pgx = pbk[2]
                            proj_tok(pgx, tok0, 3)
                            S.op("act", lambda e: e.activation(out=sg.t[:], in_=pgx.t[:], func=AF.Silu), [pgx], [sg])
                            for h in range(4):
                                S.op("dve", lambda e: e.scalar_tensor_tensor(out=t_hg.t[:, hsl(h)], in0=t_hg.t[:, hsl(h)], scalar=st4.t[:, 8 + h:9 + h], in1=hn_b.t[:, hsl(h)], op0=ALU.mult, op1=ALU.mult), [t_hg, st4, hn_b], [t_hg], cols=[t_hg])
                            S.op("dve", lambda e: e.tensor_tensor(out=ybf.t[:], in0=t_hg.t[:], in1=sg.t[:], op=ALU.mult), [t_hg, sg], [ybf])
                            yield
                            py_ = pT[1]
                            for h in range(4):
                                S.op("pe", lambda e: e.transpose(out=py_.t[:, h, :], in_=ybf.t[:, hsl(h)], identity=ident.t[:]), [ybf, ident], [py_])
                            for h in range(4):
                                dst = yT.t[:, 4 + h, :].rearrange("p (r c) -> p c r", c=64)[:, 4 * t:4 * t + 4, :]
                                src = py_.t[:, h, :].rearrange("p (c r) -> p c r", r=32)
                                S.op("act", lambda e: e.activation(out=dst, in_=src, func=AF.Copy), [py_], [yT], cols=[yT])

                        for d in range(2):
                            if d == 1:
                                wload(2, "fb"); wload(3, "g")
                            S.op("act", lambda e: e.activation(out=Sbf.t[:], in_=Sst[d].t[:], func=AF.Copy), [Sst[d]], [Sbf])
                            tiles = range(16) if d == 0 else range(15, -1, -1)
                            tiles = list(tiles)[:int(os.environ.get('HG_NT', '16'))]
                            for _ in prep_gen(d, tiles[0], 0):
                                pass
                            for n_, t in enumerate(tiles):
                                rec = recur_gen(d, t, n_ % 2)
                                nxt = prep_gen(d, tiles[n_ + 1], (n_ + 1) % 2) if n_ + 1 < len(tiles) else iter(())
                                ra = True; na = True
                                while ra or na:
                                    if ra:
                                        try:
                                            next(rec)
                                        except StopIteration:
                                            ra = False
                                    if na:
                                        try:
                                            next(nxt)
                                        except StopIteration:
                                            na = False
                            if b == 0 and d == 0:
                                dump("hg_of", of, of.t[:], [128, 16, 512], BF16)
                    if b == 0:
                        dump("yT", yT, yT.t[:], [128, 8, T], BF16)
                    if stop_after == "HG":
                        S.finish()
                        return nc, dbg_outs
                    with S.scope() as sW:
                        wo = S.sb("wo%d" % b, [128, 8, D], BF16, sW)
                        S.dma("pool", wo, None, out=wo.t[:], in_=w_out.rearrange("(k p) n -> p k n", p=128))
                        g1_b = S.sb("g1_b%d" % b, [128, D], F32, sW); gm2_b = S.sb("gm2_b%d" % b, [128, D], F32, sW); sh2_b = S.sb("sh2_b%d" % b, [128, D], F32, sW)
                        row_bcast(g1_b, lambda k: modc.t[:, 16 + k, b:b + 1], modc, pbc)
                        row_bcast(gm2_b, lambda k: gm2.t[:, k, b:b + 1], gm2, pbc)
                        row_bcast(sh2_b, lambda k: modc.t[:, 24 + k, b:b + 1], modc, pbc)
                        rw_s = S.sb("rw_s%d" % b, [128, 8, NE], BF16, sW)
                        S.dma("pool", rw_s, None, out=rw_s.t[:], in_=router_w.rearrange("(k p) n -> p k n", p=128))
                        sw13_s = S.sb("sw13_s%d" % b, [128, 8, 512], BF16, sW)
                        S.dma("pool", sw13_s, None, out=sw13_s.t[:], in_=sw13.rearrange("(k p) n -> p k n", p=128))
                        sw2_s = S.sb("sw2_s%d" % b, [128, 2, D], BF16, sW)
                        S.dma("pool", sw2_s, None, out=sw2_s.t[:], in_=sw2.rearrange("(k p) n -> p k n", p=128))
                        x1t = S.sb("w_x1t%d" % b, [128, D], F32, sW); hx2T = S.sb("w_hx2T%d" % b, [128, 8, 128], BF16, sW)
                        r_sc = S.sb("r_sc%d" % b, [128, NE], F32, sW); r_bi = S.sb("r_bi%d" % b, [128, NE], F32, sW)
                        r_mk = S.sb("r_mk%d" % b, [128, NE], F32, sW); r_sel = S.sb("r_sel%d" % b, [128, NE], F32, sW)
                        r_pos = S.sb("r_pos%d" % b, [128, NE], F32, sW); r_t1 = S.sb("r_t1%d" % b, [128, NE], F32, sW)
                        r_m8 = S.sb("r_m8%d" % b, [128, 8, 8], F32, sW); r_sm = S.sb("r_sm%d" % b, [128, 64], F32, sW)
                        r_ei = S.sb("r_ei%d" % b, [128, 8], U32, sW)
                        hTs = S.sb("w_hTs%d" % b, [128, 2, 128], BF16, sW); hsil = S.sb("w_hsil%d" % b, [128, 256], F32, sW)
                        ysh_t = S.sb("w_ysh%d" % b, [128, D], F32, sW)
                        hx2T_s = [hx2T, S.sb("w_hx2T1_%d" % b, [128, 8, 128], BF16, sW)]
                        x1t_s = [x1t, S.sb("w_x1t1_%d" % b, [128, D], F32, sW)]
                        pw8 = S.ps("pw8_%d" % b, [128, 512], F32, sW)

                        def wo_s1(i):
                            gi = b * 16 + i
                            par = i % 2
                            xt = nb_xt[par]; parts = nb_parts[par]; hx2b = nb_xn[par]; st_ = nb_st[par]; p = pT[par]
                            hx2T = hx2T_s[par]; x1t = x1t_s[par]
                            S.dma("sp", parts[0], None, out=xt.t[:], in_=x2[b, i * 128:(i + 1) * 128, :])
                            for half in range(2):
                                pp = pbk[half]
                                for k in range(8):
                                    S.op("pe", lambda e: e.matmul(pp.t[:], lhsT=yT.t[:, k, i * 128:(i + 1) * 128], rhs=wo.t[:, k, half * 512:(half + 1) * 512], start=(k == 0), stop=(k == 7)), [yT, wo], [pp])
                                S.op("dve", lambda e: e.tensor_tensor(out=nb_tmp.t[:, half * 512:(half + 1) * 512], in0=pp.t[:], in1=g1_b.t[:, half * 512:(half + 1) * 512], op=ALU.mult), [pp, g1_b], [nb_tmp])
                            S.op("dve", lambda e: e.tensor_tensor(out=x1t.t[:], in0=nb_tmp.t[:], in1=xt.t[:], op=ALU.add), [nb_tmp] + parts, [x1t])
                            S.dma("sp", x1d, x1t, out=x1d.t.ap()[gi * 128:(gi + 1) * 128, :], in_=x1t.t[:])
                            rms_rows([x1t], x1t.t[:], st_)
                            S.op("dve", lambda e: e.scalar_tensor_tensor(out=nb_tmp.t[:], in0=x1t.t[:], scalar=st_.t[:, 2:3], in1=gm2_b.t[:], op0=ALU.mult, op1=ALU.mult), [x1t, st_, gm2_b], [nb_tmp])
                            S.op("dve", lambda e: e.tensor_tensor(out=hx2b.t[:], in0=nb_tmp.t[:], in1=sh2_b.t[:], op=ALU.add), [nb_tmp, sh2_b], [hx2b])
                            for k in range(8):
                                S.op("pe", lambda e: e.transpose(out=p.t[:, k, :], in_=hx2b.t[:, k * 128:(k + 1) * 128], identity=ident.t[:]), [hx2b, ident], [p])
                            S.op("act", lambda e: e.activation(out=hx2T.t[:], in_=p.t[:], func=AF.Copy), [p], [hx2T])
                            if gi == 0:
                                dump("x1t0", x1t, x1t.t[:], [128, D]); dump("hx2b0", hx2b, hx2b.t[:], [128, D], BF16)

                        def wo_s2(i):
                            gi = b * 16 + i
                            par = i % 2
                            xt = nb_xt[par]; parts = nb_parts[par]; hx2b = nb_xn[par]; st_ = nb_st[par]; p = pT[par]
                            hx2T = hx2T_s[par]; x1t = x1t_s[par]
                            pl = pbk[2]
                            for k in range(8):
                                S.op("pe", lambda e: e.matmul(pl.t[:, 0:NE], lhsT=hx2T.t[:, k, :], rhs=rw_s.t[:, k, :], start=(k == 0), stop=(k == 7)), [hx2T, rw_s], [pl])
                            S.op("act", lambda e: e.activation(out=r_sc.t[:], in_=pl.t[:, 0:NE], func=AF.Sigmoid), [pl], [r_sc])
                            S.op("dve", lambda e: e.tensor_tensor(out=r_bi.t[:], in0=r_sc.t[:], in1=rb_b.t[:], op=ALU.add), [r_sc, rb_b], [r_bi])
                            for g in range(8):
                                S.op("dve", lambda e: e.max(out=r_m8.t[:, g, :], in_=r_bi.t[:, g * 32:(g + 1) * 32]), [r_bi], [r_m8], cols=[r_m8])
                            S.op("dve", lambda e: e.tensor_tensor(out=r_sm.t[:, 0:8], in0=r_m8.t[:, :, 0], in1=r_m8.t[:, :, 1], op=ALU.add), [r_m8], [r_sm])
                            S.op("dve", lambda e: e.max(out=r_sm.t[:, 8:16], in_=r_sm.t[:, 0:8]), [r_sm], [r_sm])
                            S.op("dve", lambda e: e.tensor_scalar(out=r_sm.t[:, 16:24], in0=r_sm.t[:, 0:8], scalar1=r_sm.t[:, 11:12], scalar2=None, op0=ALU.is_ge), [r_sm], [r_sm])
                            for g in range(8):
                                S.op("dve", lambda e: e.tensor_scalar(out=r_mk.t[:, g * 32:(g + 1) * 32], in0=r_bi.t[:, g * 32:(g + 1) * 32], scalar1=8.0, scalar2=r_sm.t[:, 16 + g:17 + g], op0=ALU.add, op1=ALU.mult), [r_bi, r_sm], [r_mk], cols=[r_mk])
                            S.op("dve", lambda e: e.max(out=r_sm.t[:, 24:32], in_=r_mk.t[:]), [r_mk], [r_sm])
                            S.op("dve", lambda e: e.max_index(out=r_ei.t[:], in_max=r_sm.t[:, 24:32], in_values=r_mk.t[:]), [r_sm, r_mk], [r_ei])
                            S.op("dve", lambda e: e.tensor_copy(out=r_sm.t[:, 32:40], in_=r_ei.t[:]), [r_ei], [r_sm])
                            S.op("dve", lambda e: e.tensor_scalar(out=r_sel.t[:], in0=r_mk.t[:], scalar1=r_sm.t[:, 31:32], scalar2=None, op0=ALU.is_ge), [r_mk, r_sm], [r_sel])
                            pq_ = pbk[3]
                            S.op("pe", lambda e: e.matmul(pq_.t[:, 0:NE], lhsT=Lst.t[:], rhs=r_sel.t[:], start=True, stop=True), [Lst, r_sel], [pq_])
                            S.op("pe", lambda e: e.matmul(pq_.t[:, NE:2 * NE], lhsT=ones.t[:], rhs=r_sel.t[:], start=True, stop=True), [ones, r_sel], [pq_])
                            S.op("dve", lambda e: e.tensor_tensor(out=r_pos.t[:], in0=pq_.t[:, 0:NE], in1=carry.t[:], op=ALU.add), [pq_, carry], [r_pos])
                            S.op("dve", lambda e: e.tensor_tensor(out=carry.t[:], in0=pq_.t[:, NE:2 * NE], in1=carry.t[:], op=ALU.add), [pq_, carry], [carry])
                            S.op("dve", lambda e: e.memset(r_sm.t[:, 40:64], 0.0), [r_sm], [r_sm])
                            for k in range(8):
                                S.op("dve", lambda e: e.scalar_tensor_tensor(out=r_t1.t[:], in0=iota_f.t[:], scalar=r_sm.t[:, 32 + k:33 + k], in1=r_pos.t[:], op0=ALU.is_equal, op1=ALU.mult, accum_out=r_sm.t[:, 40 + k:41 + k]), [iota_f, r_sm, r_pos], [r_sm], cols=[r_sm])
                                S.op("dve", lambda e: e.scalar_tensor_tensor(out=r_t1.t[:], in0=iota_f.t[:], scalar=r_sm.t[:, 32 + k:33 + k], in1=r_sc.t[:], op0=ALU.is_equal, op1=ALU.mult, accum_out=r_sm.t[:, 48 + k:49 + k]), [iota_f, r_sm, r_sc], [r_sm], cols=[r_sm])
                            S.op("dve", lambda e: e.reduce_sum(out=r_sm.t[:, 56:57], in_=r_sm.t[:, 48:56], axis=AX.X), [r_sm], [r_sm])
                            S.op("dve", lambda e: e.reciprocal(out=r_sm.t[:, 57:58], in_=r_sm.t[:, 56:57]), [r_sm], [r_sm])
                            S.op("dve", lambda e: e.tensor_scalar(out=gates_all.t[:, gi, :], in0=r_sm.t[:, 48:56], scalar1=r_sm.t[:, 57:58], scalar2=2.5, op0=ALU.mult, op1=ALU.mult), [r_sm], [gates_all])
                            S.op("dve", lambda e: e.tensor_scalar(out=eidx_all.t[:, gi * 8:gi * 8 + 8], in0=r_sm.t[:, 32:40], scalar1=1.0, scalar2=None, op0=ALU.mult), [r_sm], [eidx_all], cols=[eidx_all])
                            S.op("dve", lambda e: e.tensor_scalar(out=pos_all.t[:, gi * 8:gi * 8 + 8], in0=r_sm.t[:, 40:48], scalar1=1.0, scalar2=None, op0=ALU.mult), [r_sm], [pos_all], cols=[pos_all])
                            S.dma("sp", hx2d, hx2b, out=hx2d.t.ap()[gi * 128:(gi + 1) * 128, :], in_=hx2b.t[:])
                            pu = pbc
                            for c in range(4):
                                for k in range(8):
                                    S.op("pe", lambda e: e.matmul(pu.t[:, c * 128:(c + 1) * 128], lhsT=sw13_s.t[:, k, c * 128:(c + 1) * 128], rhs=hx2T.t[:, k, :], start=(k == 0), stop=(k == 7)), [sw13_s, hx2T], [pu])
                            S.op("act", lambda e: e.activation(out=hsil.t[:], in_=pu.t[:, 0:256], func=AF.Silu), [pu], [hsil])
                            S.op("dve", lambda e: e.tensor_tensor(out=hTs.t[:].rearrange("p c t -> p (c t)"), in0=hsil.t[:], in1=pu.t[:, 256:512], op=ALU.mult), [hsil, pu], [hTs])
                            for half in range(2):
                                pp = pw8 if half == 0 else pbk[2]
                                for c2 in range(2):
                                    S.op("pe", lambda e: e.matmul(pp.t[:], lhsT=hTs.t[:, c2, :], rhs=sw2_s.t[:, c2, half * 512:(half + 1) * 512], start=(c2 == 0), stop=(c2 == 1)), [hTs, sw2_s], [pp])
                                S.op("act", lambda e: e.activation(out=ysh_t.t[:, half * 512:(half + 1) * 512], in_=pp.t[:], func=AF.Copy), [pp], [ysh_t])
                            S.dma("sp", yshd, ysh_t, out=yshd.t.ap()[gi * 128:(gi + 1) * 128, :], in_=ysh_t.t[:])
                            if gi == 0:
                                dump("ysh0", ysh_t, ysh_t.t[:], [128, D])
                        wo_s1(0)
                        for i in range(16):
                            if i + 1 < 16:
                                wo_s1(i + 1)
                            wo_s2(i)
        dump("eidx_all", eidx_all, eidx_all.t[:], [128, 256]); dump("pos_all", pos_all, pos_all.t[:], [128, 256]); dump("gates_all", gates_all, gates_all.t[:], [128, 32, 8])
        dump("carry", carry, carry.t[:], [128, NE])
        if stop_after == "WO":
            S.finish()
            return nc, dbg_outs
        NBLK = 512
        slots_all = S.sb("slots_all", [128, 256], I32)
        E_all = S.sb("E_all", [128, NBLK], F32)
        with S.scope() as sP:
            padded = S.sb("padded", [128, NE], F32, sP); pad_i = S.sb("pad_i", [128, NE], I32, sP)
            pends = S.sb("pends", [128, NE], F32, sP); pstart = S.sb("pstart", [128, NE], F32, sP)
            ones256 = S.sb("ones256", [128, NE], F32, sP); junk256 = S.sb("junk256", [128, NE], F32, sP)
            ps8 = S.sb("ps8", [128, 8], F32, sP)
            hxr = [S.sb("hxr%d" % i, [128, D], BF16, sP) for i in range(2)]
            S.op("pool", lambda e: e.memset(ones256.t[:], 1.0), [], [ones256])
            S.op("dve", lambda e: e.tensor_scalar(out=padded.t[:], in0=carry.t[:], scalar1=127.0, scalar2=None, op0=ALU.add), [carry], [padded])
            S.op("dve", lambda e: e.tensor_copy(out=pad_i.t[:], in_=padded.t[:]), [padded], [pad_i])
            S.op("dve", lambda e: e.tensor_scalar(out=pad_i.t[:], in0=pad_i.t[:], scalar1=7, scalar2=7, op0=ALU.arith_shift_right, op1=ALU.logical_shift_left), [pad_i], [pad_i])
            S.op("dve", lambda e: e.tensor_copy(out=padded.t[:], in_=pad_i.t[:]), [pad_i], [padded])
            S.op("dve", lambda e: e.tensor_tensor_scan(out=pends.t[:], data0=ones256.t[:], data1=padded.t[:], initial=0.0, op0=ALU.mult, op1=ALU.add), [ones256, padded], [pends])
            S.op("dve", lambda e: e.tensor_tensor(out=pstart.t[:], in0=pends.t[:], in1=padded.t[:], op=ALU.subtract), [pends, padded], [pstart])
            dump("pstart", pstart, pstart.t[:], [128, NE])
            slots_tb = [TB(slots_all.t, "slots_g%d" % g_) for g_ in range(32)]
            xs_parts = [TB(xs_pad.t, "xs_part%d" % k_) for k_ in range(8)]
            hxr.append(S.sb("hxr2", [128, D], BF16, sP))
            ps8s = [ps8, S.sb("ps8b", [128, 8], F32, sP)]
            for gi in range(32):
                hx = hxr[gi % 3]; ps8 = ps8s[gi % 2]
                S.dma("sp", hx, hx2d, out=hx.t[:], in_=hx2d.t.ap()[gi * 128:(gi + 1) * 128, :])
                S.op("dve", lambda e: e.memset(ps8.t[:], 0.0), [], [ps8])
                for k in range(8):
                    S.op("dve", lambda e: e.scalar_tensor_tensor(out=junk256.t[:], in0=iota_f.t[:], scalar=eidx_all.t[:, gi * 8 + k:gi * 8 + k + 1], in1=pstart.t[:], op0=ALU.is_equal, op1=ALU.mult, accum_out=ps8.t[:, k:k + 1]), [iota_f, eidx_all, pstart], [ps8], cols=[ps8])
                S.op("dve", lambda e: e.tensor_tensor(out=ps8.t[:], in0=ps8.t[:], in1=pos_all.t[:, gi * 8:gi * 8 + 8], op=ALU.add), [ps8, pos_all], [ps8])
                S.op("dve", lambda e: e.tensor_copy(out=slots_all.t[:, gi * 8:gi * 8 + 8], in_=ps8.t[:]), [ps8], [slots_tb[gi]])
                for k in range(8):
                    S.dma("pool", xs_parts[k], hx, out=xs_pad.t.ap(), in_=hx.t[:], extra_reads=[slots_tb[gi]],
                          indirect=dict(out_offset=bass.IndirectOffsetOnAxis(ap=slots_all.t[:, gi * 8 + k:gi * 8 + k + 1], axis=0), in_offset=None, bounds_check=NSLOT - 1, oob_is_err=False))
            S.op("pool", lambda e: e.memset(E_all.t[:], 0.0), [], [E_all])
            for j in range(NBLK):
                S.op("dve", lambda e: e.tensor_scalar(out=junk256.t[:], in0=pends.t[:], scalar1=float(128 * j), scalar2=0.0, op0=ALU.is_le, op1=ALU.add, accum_out=E_all.t[:, j:j + 1]), [pends], [E_all], cols=[E_all])
            dump("E_all", E_all, E_all.t[:], [128, NBLK])
        S.barrier()
        dump("slots_all", slots_all, slots_all.t[:], [128, 256], I32)
        if stop_after == "SC":
            S.finish()
            return nc, dbg_outs

        with S.scope() as sE:
            pidx_i = S.sb("pidx_i", [128, 1], I32, sE); pidx = S.sb("pidx", [128, 1], F32, sE)
            S.op("pool", lambda e: e.iota(pidx_i.t[:], pattern=[[0, 1]], base=0, channel_multiplier=1), [], [pidx_i])
            S.op("dve", lambda e: e.tensor_copy(out=pidx.t[:], in_=pidx_i.t[:]), [pidx_i], [pidx])
            NBUF = 4
            w13f = [S.sb("w13f%d" % i, [128, 8, 512], F32, sE) for i in range(NBUF)]
            w2f = [S.sb("w2f%d" % i, [128, 2, D], F32, sE) for i in range(NBUF)]
            w13b = [S.sb("w13b%d" % i, [128, 8, 512], BF16, sE) for i in range(2)]
            w2b = [S.sb("w2b%d" % i, [128, 2, D], BF16, sE) for i in range(2)]
            wix_f = [S.sb("wixf%d" % i, [128, 1], F32, sE) for i in range(NBUF)]
            wix = [S.sb("wix%d" % i, [128, 1], I32, sE) for i in range(NBUF)]
            xr = [S.sb("e_xr%d" % i, [128, D], BF16, sE) for i in range(2)]
            xTe = [S.sb("e_xT%d" % i, [128, 8, 128], BF16, sE) for i in range(2)]
            usil = S.sb("e_usil", [128, 256], F32, sE); hb = S.sb("e_hb", [128, 256], BF16, sE)
            hTe = S.sb("e_hT", [128, 2, 128], BF16, sE)
            yo = [S.sb("e_yo%d" % i, [128, D], F32, sE) for i in range(2)]
            ptx = [S.ps("e_ptx%d" % i, [128, 8, 128], BF16, sE) for i in range(2)]
            pu_ = [S.ps("e_pu%d" % i, [128, 512], F32, sE) for i in range(2)]
            py2 = [[S.ps("e_py%d_%d" % (i, h), [128, 512], F32, sE) for h in range(2)] for i in range(2)]
            nblk_run = int(os.environ.get("MOE_NBLK", NBLK))
            WB = ne_decl * 128 - 1

            def load_w(j):
                i = j % NBUF
                if os.environ.get("MOE_NOLOAD"):
                    return
                S.op("dve", lambda e: e.tensor_scalar(out=wix_f[i].t[:], in0=E_all.t[:, j:j + 1], scalar1=128.0, scalar2=pidx.t[:, 0:1], op0=ALU.mult, op1=ALU.add), [E_all, pidx], [wix_f[i]])
                S.op("dve", lambda e: e.tensor_copy(out=wix[i].t[:], in_=wix_f[i].t[:]), [wix_f[i]], [wix[i]])
                S.dma("pool", w13f[i], None, out=w13f[i].t[:].rearrange("p k n -> p (k n)"), in_=w13, extra_reads=[wix[i]],
                      indirect=dict(out_offset=None, in_offset=bass.IndirectOffsetOnAxis(ap=wix[i].t[:, 0:1], axis=0), bounds_check=WB, oob_is_err=False))
                S.dma("pool", w2f[i], None, out=w2f[i].t[:].rearrange("p k n -> p (k n)"), in_=w2, extra_reads=[wix[i]],
                      indirect=dict(out_offset=None, in_offset=bass.IndirectOffsetOnAxis(ap=wix[i].t[:, 0:1], axis=0), bounds_check=WB, oob_is_err=False))

            def cast_w(j):
                i = j % NBUF; o = j % 2
                if os.environ.get("MOE_NOCAST"):
                    return
                S.op("act", lambda e: e.activation(out=w13b[o].t[:, 0:3, :], in_=w13f[i].t[:, 0:3, :], func=AF.Copy), [w13f[i]], [w13b[o]])
                S.op("dve", lambda e: e.tensor_scalar(out=w13b[o].t[:, 3:8, :], in0=w13f[i].t[:, 3:8, :], scalar1=1.0, scalar2=None, op0=ALU.mult), [w13f[i]], [w13b[o]])
                S.op("act", lambda e: e.activation(out=w2b[o].t[:, 0, :], in_=w2f[i].t[:, 0, :], func=AF.Copy), [w2f[i]], [w2b[o]])
                S.op("dve", lambda e: e.tensor_scalar(out=w2b[o].t[:, 1, :], in0=w2f[i].t[:, 1, :], scalar1=1.0, scalar2=None, op0=ALU.mult), [w2f[i]], [w2b[o]])

            xr3 = xr + [S.sb("e_xr2", [128, D], BF16, sE)]
            usil2 = [usil, S.sb("e_usil1", [128, 256], F32, sE)]
            hb2 = [hb, S.sb("e_hb1", [128, 256], BF16, sE)]
            hTe2 = [hTe, S.sb("e_hT1", [128, 2, 128], BF16, sE)]

            def s_load_x(j):
                S.dma("sp", xr3[j % 3], xs_pad, out=xr3[j % 3].t[:], in_=xs_pad.t.ap()[j * 128:(j + 1) * 128, :])

            def s_Tx(j):
                x_ = xr3[j % 3]; p_ = ptx[j % 2]
                for k in range(8):
                    S.op("pe", lambda e: e.transpose(out=p_.t[:, k, :], in_=x_.t[:, k * 128:(k + 1) * 128], identity=ident.t[:]), [x_, ident], [p_])
                S.op("act", lambda e: e.activation(out=xTe[j % 2].t[:], in_=p_.t[:], func=AF.Copy), [p_], [xTe[j % 2]])

            def s_up(j):
                o = j % 2
                for k in range(8):
                    S.op("pe", lambda e: e.matmul(pu_[o].t[:], lhsT=xTe[o].t[:, k, :], rhs=w13b[o].t[:, k, :], start=(k == 0), stop=(k == 7)), [xTe[o], w13b[o]], [pu_[o]])
                S.op("act", lambda e: e.activation(out=usil2[o].t[:], in_=pu_[o].t[:, 0:256], func=AF.Silu), [pu_[o]], [usil2[o]])
                S.op("dve", lambda e: e.tensor_tensor(out=hb2[o].t[:], in0=usil2[o].t[:], in1=pu_[o].t[:, 256:512], op=ALU.mult), [usil2[o], pu_[o]], [hb2[o]])

            def s_Th(j):
                o = j % 2; p_ = ptx[o]
                for c2 in range(2):
                    S.op("pe", lambda e: e.transpose(out=p_.t[:, c2, :], in_=hb2[o].t[:, c2 * 128:(c2 + 1) * 128], identity=ident.t[:]), [hb2[o], ident], [p_])
                S.op("act", lambda e: e.activation(out=hTe2[o].t[:], in_=p_.t[:, 0:2, :], func=AF.Copy), [p_], [hTe2[o]])

            def s_down(j):
                o = j % 2
                for half in range(2):
                    pp = py2[o][half]
                    for c2 in range(2):
                        S.op("pe", lambda e: e.matmul(pp.t[:], lhsT=hTe2[o].t[:, c2, :], rhs=w2b[o].t[:, c2, half * 512:(half + 1) * 512], start=(c2 == 0), stop=(c2 == 1)), [hTe2[o], w2b[o]], [pp])
                    if half == 0:
                        S.op("act", lambda e: e.activation(out=yo[o].t[:, 0:512], in_=pp.t[:], func=AF.Copy), [pp], [yo[o]])
                    else:
                        S.op("dve", lambda e: e.tensor_copy(out=yo[o].t[:, 512:1024], in_=pp.t[:]), [pp], [yo[o]])
                S.dma("sp", ys_pad, yo[o], out=ys_pad.t.ap()[j * 128:(j + 1) * 128, :], in_=yo[o].t[:])

            n_ = nblk_run
            load_w(0)
            if n_ > 1:
                load_w(1)
            if n_ > 2:
                load_w(2)
            s_load_x(0)
            if n_ > 1:
                s_load_x(1)
            cast_w(0); s_Tx(0); s_up(0)
            for j in range(n_):
                if j + 3 < n_:
                    load_w(j + 3)
                if j + 2 < n_:
                    s_load_x(j + 2)
                if j + 1 < n_:
                    cast_w(j + 1); s_Tx(j + 1)
                s_Th(j)
                if j + 1 < n_:
                    s_up(j + 1)
                s_down(j)
        if stop_after == "EXP":
            S.finish()
            return nc, dbg_outs

        with S.scope() as sF:
            g2_b = [S.sb("g2_b%d" % b, [128, D], F32, sF) for b in range(NB)]
            pbf = S.ps("pbf", [128, 512], F32, sF)
            for b in range(NB):
                row_bcast(g2_b[b], lambda k: modc.t[:, 40 + k, b:b + 1], modc, pbf)
            accs = [S.sb("f_acc%d" % i, [128, D], F32, sF) for i in range(2)]
            gat = [S.sb("f_gat%d" % i, [128, D], F32, sF) for i in range(12)]
            x1rs = [S.sb("f_x1r%d" % i, [128, D], F32, sF) for i in range(2)]; fo = [S.sb("f_o%d" % i, [128, D], F32, sF) for i in range(2)]
            fst = S.sb("f_st", [128, 4], F32, sF); fjunk = S.sb("f_junk", [128, D], BF16, sF)
            for gi in range(32):
                b = gi // 16
                acc = accs[gi % 2]; x1r = x1rs[gi % 2]
                S.dma("sp", acc, yshd, out=acc.t[:], in_=yshd.t.ap()[gi * 128:(gi + 1) * 128, :])
                S.dma("sp", x1r, x1d, out=x1r.t[:], in_=x1d.t.ap()[gi * 128:(gi + 1) * 128, :])
                for k in range(8):
                    g = gat[(gi * 8 + k) % 12]
                    S.dma("pool", g, ys_pad, out=g.t[:], in_=ys_pad.t.ap(), extra_reads=[slots_all],
                          indirect=dict(out_offset=None, in_offset=bass.IndirectOffsetOnAxis(ap=slots_all.t[:, gi * 8 + k:gi * 8 + k + 1], axis=0)))
                    S.op("dve", lambda e: e.scalar_tensor_tensor(out=acc.t[:], in0=g.t[:], scalar=gates_all.t[:, gi, k:k + 1], in1=acc.t[:], op0=ALU.mult, op1=ALU.add), [g, gates_all, acc], [acc])
                S.op("dve", lambda e: e.tensor_tensor(out=acc.t[:], in0=acc.t[:], in1=g2_b[b].t[:], op=ALU.mult), [acc, g2_b[b]], [acc])
                S.op("dve", lambda e: e.tensor_tensor(out=acc.t[:], in0=acc.t[:], in1=x1r.t[:], op=ALU.add), [acc, x1r], [acc])
                S.op("dve", lambda e: e.memset(fst.t[:, 0:1], 0.0), [], [fst])
                S.op("act", lambda e: e.activation(out=fjunk.t[:], in_=acc.t[:], func=AF.Square, accum_out=fst.t[:, 0:1]), [acc], [fst])
                S.op("dve", lambda e: e.tensor_scalar(out=fst.t[:, 1:2], in0=fst.t[:, 0:1], scalar1=1.0 / D, scalar2=EPS, op0=ALU.mult, op1=ALU.add), [fst], [fst])
                S.op("act", lambda e: e.activation(out=fst.t[:, 1:2], in_=fst.t[:, 1:2], func=AF.Sqrt), [fst], [fst])
                S.op("dve", lambda e: e.reciprocal(out=fst.t[:, 2:3], in_=fst.t[:, 1:2]), [fst], [fst])
                o_ = fo[gi % 2]
                S.op("dve", lambda e: e.scalar_tensor_tensor(out=o_.t[:], in0=acc.t[:], scalar=fst.t[:, 2:3], in1=nfin_b.t[:], op0=ALU.mult, op1=ALU.mult), [acc, fst, nfin_b], [o_])
                S.dma("sp", None, o_, out=out[b, (gi % 16) * 128:(gi % 16 + 1) * 128, :], in_=o_.t[:], final=True)
        S.finish()
    return nc, dbg_outs


def prep_inputs(inp, cores=range(8), ne_decl=256):
    f = lambda a: np.ascontiguousarray(np.asarray(a, dtype=np.float32))
    col8 = lambda v: f(np.asarray(v).reshape(8, 128).T)
    ada_bc = f(np.asarray(inp["ada_b"])[0].reshape(48, 128).T)
    conv_w = np.asarray(inp["lru_conv_w"])[0]
    convw = f(conv_w.T.reshape(4, 128, 4).transpose(1, 0, 2).reshape(128, 16))
    convb = f(np.asarray(inp["lru_conv_b"])[0].reshape(4, 128).T)

    def blockdiag(w):
        w = np.asarray(w)[0]
        o = np.zeros((128, 8, 128), np.float32)
        for d in range(2):
            for c in range(4):
                for hh in range(2):
                    o[hh * 64:(hh + 1) * 64, d * 4 + c, hh * 64:(hh + 1) * 64] = w[d, 2 * c + hh]
        return o

    def col_dc(v):
        return f(np.asarray(v)[0].reshape(2, 4, 128).transpose(2, 0, 1).reshape(128, 8))

    shared = {
        "ada_w": f(np.asarray(inp["ada_w"])[0]), "ada_bc": ada_bc,
        "nmix": col8(np.asarray(inp["norm_mix"])[0]), "nffn": col8(np.asarray(inp["norm_ffn"])[0]),
        "nfin": f(np.asarray(inp["norm_final"]).reshape(1, 1024)),
        "w_in": f(np.asarray(inp["w_in"])[0]), "w_out": f(np.asarray(inp["w_out"])[0]),
        "convw": convw, "convb": convb,
        "wa_bd": blockdiag(inp["lru_wa"]), "wx_bd": blockdiag(inp["lru_wx"]),
        "ba_c": col_dc(inp["lru_ba"]), "bx_c": col_dc(inp["lru_bx"]), "lam_c": col_dc(inp["lru_lambda"]),
        "lbl": f(np.asarray(inp["hgrn_lb_logits"]).reshape(1, 2048)),
        "hnorm": f(np.asarray(inp["hgrn_norm"]).reshape(1, 512)),
        "router_w": f(np.asarray(inp["router_w"])[0]), "router_b": f(np.asarray(inp["router_b"]).reshape(1, 256)),
        "w13": f(np.asarray(inp["exp_w13"])[0][:ne_decl].reshape(ne_decl, 8, 128, 512).transpose(0, 2, 1, 3).reshape(ne_decl * 128, 4096)),
        "w2": f(np.asarray(inp["exp_w2"])[0][:ne_decl].reshape(ne_decl, 2, 128, 1024).transpose(0, 2, 1, 3).reshape(ne_decl * 128, 2048)),
        "sw13": f(np.asarray(inp["shared_w13"])[0]), "sw2": f(np.asarray(inp["shared_w2"])[0]),
    }
    x = np.asarray(inp["x"]); c = np.asarray(inp["c"]); ctx = np.asarray(inp["ctx"]); c_ctx = np.asarray(inp["c_ctx"])
    maps = []
    for i in cores:
        m = dict(shared)
        m["x2"] = f(x[2 * i:2 * i + 2]); m["ctx2"] = f(ctx[2 * i:2 * i + 2])
        m["cT"] = f(np.stack([c[2 * i], c[2 * i + 1], c_ctx], axis=1))
        maps.append(m)
    return maps


_NC_CACHE = {}


def kernel(**inputs):
    if "nc" not in _NC_CACHE:
        _NC_CACHE["nc"] = build_nc()[0]
    nc = _NC_CACHE["nc"]
    maps = prep_inputs(inputs)
    res = run_bass_kernel_spmd(nc, maps, core_ids=list(range(8)))
    return np.concatenate([r["out"] for r in res.results], axis=0).astype(np.float32)
```
